# Optimizing a Trainium2 kernel written in Bass

```python
import math
import jax, jax.numpy as jnp
from jax import lax
import numpy as np

D_MODEL = 1024
BATCH = 8
SEQ = 4096
DEPTH = 4

CHUNK = 64
Q_BLOCK = 128
EPS = 1e-6
NEG_INF = -1e30
D_MIX = D_MODEL
HEAD_DIM = 64
W_SSM = D_MIX // 4
W_SB = D_MIX // 4
W_CH = D_MIX // 4
W_DF = D_MIX - W_SSM - W_SB - W_CH
SSM_GROUP = 16
SSM_GROUPS = W_SSM // SSM_GROUP
SSM_STATE = 64
DT_MIN = 1e-3
DT_MAX = 1e-1
H_SB = W_SB // HEAD_DIM
H_CH = W_CH // HEAD_DIM
H_DF = W_DF // HEAD_DIM
DF_QK_DIM = HEAD_DIM // 2
CH_LEFT_CHUNKS = 8
CH_BAND = CH_LEFT_CHUNKS + 1
REL_CLIP = 128
N_MIX_HEADS = D_MIX // HEAD_DIM
IN_COLS = W_SSM + 3 * W_SB + 3 * W_CH + 3 * W_DF
D_FF = 11 * D_MODEL // 4
N_EXPERTS = 8
TOP_K = 2
D_FF_EXPERT = 7 * D_MODEL // 2
N_DENSE = (DEPTH + 1) // 2
N_MOE = DEPTH // 2

kernel_name = 'hybrid_parallel_heads_streaming_block'


def rms_norm(x, gain):
    xf = x.astype(jnp.float32)
    y = xf * lax.rsqrt(jnp.mean(xf * xf, axis=-1, keepdims=True) + EPS)
    return (y * gain.astype(jnp.float32)).astype(x.dtype)


def split_columns(proj):
    sizes = (W_SSM, W_SB, W_SB, W_SB, W_CH, W_CH, W_CH, W_DF, W_DF, W_DF)
    parts, off = [], 0
    for w in sizes:
        parts.append(proj[..., off:off + w])
        off += w
    return parts


def s5_mixer(u, lam_re, lam_im, log_dt, b_re, b_im, c_re, c_im, d_skip, w_glu, b_glu):
    bsz, seq, _ = u.shape
    f32 = jnp.float32
    uf = u.astype(f32).reshape(bsz, seq, SSM_GROUPS, SSM_GROUP)
    lam = lax.complex(lam_re.astype(f32), lam_im.astype(f32))
    dt = jnp.exp(log_dt.astype(f32))[:, None]
    lam_bar = jnp.exp(lam * dt)
    b_mat = lax.complex(b_re.astype(f32), b_im.astype(f32))
    c_mat = lax.complex(c_re.astype(f32), c_im.astype(f32))
    b_bar = ((lam_bar - 1.0) / lam)[..., None] * b_mat
    bu = jnp.einsum('bsgp,gnp->sbgn', uf.astype(jnp.complex64), b_bar)
    a = jnp.broadcast_to(lam_bar, (seq, 1) + lam_bar.shape)

    def combine(e1, e2):
        a1, b1 = e1
        a2, b2 = e2
        return a1 * a2, a2 * b1 + b2

    _, states = lax.associative_scan(combine, (a, bu), axis=0)
    y = jnp.einsum('sbgn,gpn->bsgp', states, c_mat).real + d_skip.astype(f32).reshape(SSM_GROUPS, SSM_GROUP) * uf
    y = jax.nn.gelu(y.reshape(bsz, seq, W_SSM))
    y = y * jax.nn.sigmoid(y @ w_glu + b_glu)
    return y.astype(u.dtype)


def stick_breaking_attention(q, k, v):
    bsz, seq, h, d = q.shape
    nb = seq // Q_BLOCK
    scale = d ** -0.5
    q_blocks = jnp.moveaxis(q.reshape(bsz, nb, Q_BLOCK, h, d), 1, 0)
    key_pos = jnp.arange(seq)

    def block(args):
        qb, bi = args
        q_pos = bi * Q_BLOCK + jnp.arange(Q_BLOCK)
        z = jnp.einsum('bqhd,bkhd->bhqk', qb, k).astype(jnp.float32) * scale
        before = key_pos[None, :] < q_pos[:, None]
        log_1m = jnp.where(before, jax.nn.log_sigmoid(-z), 0.0)
        between = lax.cumsum(log_1m, axis=3, reverse=True) - log_1m
        w = jnp.where(before, jnp.exp(jax.nn.log_sigmoid(z) + between), 0.0)
        return jnp.einsum('bhqk,bkhd->bqhd', w.astype(v.dtype), v)

    out = lax.map(block, (q_blocks, jnp.arange(nb)))
    return jnp.moveaxis(out, 0, 1).reshape(bsz, seq, h * d)


def chunked_band_attention(q, k, v, rel_bias):
    bsz, seq, h, d = q.shape
    nc = seq // CHUNK
    scale = d ** -0.5
    qc = q.reshape(bsz, nc, CHUNK, h, d)
    pad = ((0, 0), (CH_LEFT_CHUNKS * CHUNK, 0), (0, 0), (0, 0))
    kp = jnp.pad(k, pad).reshape(bsz, nc + CH_LEFT_CHUNKS, CHUNK, h, d)
    vp = jnp.pad(v, pad).reshape(bsz, nc + CH_LEFT_CHUNKS, CHUNK, h, d)
    band_idx = jnp.arange(nc)[:, None] + jnp.arange(CH_BAND)[None, :]
    kb = kp[:, band_idx].reshape(bsz, nc, CH_BAND * CHUNK, h, d)
    vb = vp[:, band_idx].reshape(bsz, nc, CH_BAND * CHUNK, h, d)
    key_valid = jnp.repeat(band_idx >= CH_LEFT_CHUNKS, CHUNK, axis=1)
    rel = CH_LEFT_CHUNKS * CHUNK + jnp.arange(CHUNK)[:, None] - jnp.arange(CH_BAND * CHUNK)[None, :]
    bias = rel_bias.astype(jnp.float32)[:, jnp.clip(rel, -REL_CLIP, REL_CLIP) + REL_CLIP]
    s = jnp.einsum('bcqhd,bckhd->bhcqk', qc, kb).astype(jnp.float32) * scale + bias[:, None]
    s = jnp.where(key_valid[:, None, :], s, NEG_INF)
    p = jax.nn.softmax(s, axis=-1)
    out = jnp.einsum('bhcqk,bckhd->bcqhd', p.astype(v.dtype), vb)
    return out.reshape(bsz, seq, h * d)


def differential_attention(q, k, v, lam, slopes):
    bsz, seq, h, _, dq = q.shape
    nb = seq // Q_BLOCK
    scale = dq ** -0.5
    q_blocks = jnp.moveaxis(q.reshape(bsz, nb, Q_BLOCK, h, 2, dq), 1, 0)
    key_pos = jnp.arange(seq)

    def block(args):
        qb, bi = args
        q_pos = bi * Q_BLOCK + jnp.arange(Q_BLOCK)
        s = jnp.einsum('bqhid,bkhid->ibhqk', qb, k).astype(jnp.float32) * scale
        dist = jnp.abs(q_pos[:, None] - key_pos[None, :]).astype(jnp.float32)
        s = s - slopes[:, None, None] * dist
        allowed = (key_pos[None, :] // CHUNK) <= (q_pos[:, None] // CHUNK)
        p = jax.nn.softmax(jnp.where(allowed, s, NEG_INF), axis=-1)
        w = p[0] - lam * p[1]
        return jnp.einsum('bhqk,bkhd->bqhd', w.astype(v.dtype), v)

    out = lax.map(block, (q_blocks, jnp.arange(nb)))
    return jnp.moveaxis(out, 0, 1).reshape(bsz, seq, h * v.shape[-1])


def swiglu(h, w1, w3, w2):
    return (jax.nn.silu(h @ w1) * (h @ w3)) @ w2


def moe_ffn(h, w_router, w1, w3, w2):
    bsz, seq, dm = h.shape
    tok = h.reshape(bsz * seq, dm)
    logits = (tok @ w_router).astype(jnp.float32)
    top_vals, top_idx = lax.top_k(logits, TOP_K)
    gates = jax.nn.softmax(top_vals, axis=-1)
    dense_gate = jnp.sum(jax.nn.one_hot(top_idx, N_EXPERTS, dtype=jnp.float32) * gates[..., None], axis=1)
    out = jnp.zeros_like(tok)
    for e in range(N_EXPERTS):
        out = out + dense_gate[:, e:e + 1].astype(tok.dtype) * swiglu(tok, w1[e], w3[e], w2[e])
    return out.reshape(bsz, seq, dm)


def setup_inputs(seed: int = 0) -> dict:
    key = jax.random.key(seed)
    ks = iter(jax.random.split(key, 40))
    f32 = jnp.float32

    def nrm(shape, scale):
        return scale * jax.random.normal(next(ks), shape, f32)

    def gain(shape):
        return 1.0 + 0.05 * jax.random.normal(next(ks), shape, f32)

    res_scale = (2.0 * DEPTH) ** -0.5
    return {
        'x': nrm((BATCH, SEQ, D_MODEL), 1.0),
        'norm_mix_g': gain((DEPTH, D_MODEL)),
        'w_in': nrm((DEPTH, D_MODEL, IN_COLS), D_MODEL ** -0.5),
        'ssm_lam_re': -0.5 + nrm((DEPTH, SSM_GROUPS, SSM_STATE), 0.01),
        'ssm_lam_im': jnp.pi * jnp.arange(SSM_STATE, dtype=f32) + nrm((DEPTH, SSM_GROUPS, SSM_STATE), 0.01),
        'ssm_log_dt': jax.random.uniform(next(ks), (DEPTH, SSM_GROUPS), f32, math.log(DT_MIN), math.log(DT_MAX)),
        'ssm_b_re': nrm((DEPTH, SSM_GROUPS, SSM_STATE, SSM_GROUP), (2.0 * SSM_GROUP) ** -0.5),
        'ssm_b_im': nrm((DEPTH, SSM_GROUPS, SSM_STATE, SSM_GROUP), (2.0 * SSM_GROUP) ** -0.5),
        'ssm_c_re': nrm((DEPTH, SSM_GROUPS, SSM_GROUP, SSM_STATE), (2.0 * SSM_STATE) ** -0.5),
        'ssm_c_im': nrm((DEPTH, SSM_GROUPS, SSM_GROUP, SSM_STATE), (2.0 * SSM_STATE) ** -0.5),
        'ssm_d': nrm((DEPTH, W_SSM), 1.0),
        'ssm_w_glu': nrm((DEPTH, W_SSM, W_SSM), W_SSM ** -0.5),
        'ssm_b_glu': nrm((DEPTH, W_SSM), 0.02),
        'ch_q_norm_g': gain((DEPTH, HEAD_DIM)),
        'ch_k_norm_g': gain((DEPTH, HEAD_DIM)),
        'ch_rel_bias': nrm((DEPTH, H_CH, 2 * REL_CLIP + 1), 0.1),
        'df_q_norm_g': gain((DEPTH, 2, DF_QK_DIM)),
        'df_k_norm_g': gain((DEPTH, 2, DF_QK_DIM)),
        'df_lambda': nrm((DEPTH, 4, DF_QK_DIM), 0.1),
        'out_norm_g': gain((DEPTH, D_MIX)),
        'w_out': nrm((DEPTH, D_MIX, D_MODEL), D_MIX ** -0.5 * res_scale),
        'norm_ffn_g': gain((DEPTH, D_MODEL)),
        'ffn_w1': nrm((N_DENSE, D_MODEL, D_FF), D_MODEL ** -0.5),
        'ffn_w3': nrm((N_DENSE, D_MODEL, D_FF), D_MODEL ** -0.5),
        'ffn_w2': nrm((N_DENSE, D_FF, D_MODEL), D_FF ** -0.5 * res_scale),
        'moe_router': nrm((N_MOE, D_MODEL, N_EXPERTS), D_MODEL ** -0.5),
        'moe_w1': nrm((N_MOE, N_EXPERTS, D_MODEL, D_FF_EXPERT), D_MODEL ** -0.5),
        'moe_w3': nrm((N_MOE, N_EXPERTS, D_MODEL, D_FF_EXPERT), D_MODEL ** -0.5),
        'moe_w2': nrm((N_MOE, N_EXPERTS, D_FF_EXPERT, D_MODEL), D_FF_EXPERT ** -0.5 * res_scale),
    }


def reference(x, norm_mix_g, w_in, ssm_lam_re, ssm_lam_im, ssm_log_dt, ssm_b_re, ssm_b_im, ssm_c_re, ssm_c_im,
              ssm_d, ssm_w_glu, ssm_b_glu, ch_q_norm_g, ch_k_norm_g, ch_rel_bias, df_q_norm_g, df_k_norm_g,
              df_lambda, out_norm_g, w_out, norm_ffn_g, ffn_w1, ffn_w3, ffn_w2, moe_router, moe_w1, moe_w3, moe_w2):
    bsz, seq, _ = x.shape
    slopes = jnp.exp2(-8.0 * jnp.arange(1, H_DF + 1, dtype=jnp.float32) / H_DF)

    def heads(t, n):
        return t.reshape(bsz, seq, n, HEAD_DIM)

    for layer in range(DEPTH):
        h = rms_norm(x, norm_mix_g[layer])
        proj = h @ w_in[layer]
        u, q_sb, k_sb, v_sb, q_ch, k_ch, v_ch, q_df, k_df, v_df = split_columns(proj)

        y_ssm = s5_mixer(u, ssm_lam_re[layer], ssm_lam_im[layer], ssm_log_dt[layer], ssm_b_re[layer],
                         ssm_b_im[layer], ssm_c_re[layer], ssm_c_im[layer], ssm_d[layer],
                         ssm_w_glu[layer], ssm_b_glu[layer])

        y_sb = stick_breaking_attention(heads(q_sb, H_SB), heads(k_sb, H_SB), heads(v_sb, H_SB))

        q_c = rms_norm(heads(q_ch, H_CH), ch_q_norm_g[layer])
        k_c = rms_norm(heads(k_ch, H_CH), ch_k_norm_g[layer])
        y_ch = chunked_band_attention(q_c, k_c, heads(v_ch, H_CH), ch_rel_bias[layer])

        q_d = rms_norm(q_df.reshape(bsz, seq, H_DF, 2, DF_QK_DIM), df_q_norm_g[layer])
        k_d = rms_norm(k_df.reshape(bsz, seq, H_DF, 2, DF_QK_DIM), df_k_norm_g[layer])
        lambda_init = 0.8 - 0.6 * math.exp(-0.3 * layer)
        lam_p = df_lambda[layer].astype(jnp.float32)
        lam = jnp.exp(jnp.sum(lam_p[0] * lam_p[1])) - jnp.exp(jnp.sum(lam_p[2] * lam_p[3])) + lambda_init
        y_df = differential_attention(q_d, k_d, heads(v_df, H_DF), lam, slopes)

        mix = jnp.concatenate([y_ssm, y_sb, y_ch, y_df], axis=-1).reshape(bsz, seq, N_MIX_HEADS, HEAD_DIM)
        head_scale = jnp.concatenate([jnp.ones((N_MIX_HEADS - H_DF,), jnp.float32),
                                      jnp.full((H_DF,), 1.0 - lambda_init, jnp.float32)])
        mix = rms_norm(mix, out_norm_g[layer].reshape(N_MIX_HEADS, HEAD_DIM)) * head_scale[:, None].astype(x.dtype)
        x = x + mix.reshape(bsz, seq, D_MIX) @ w_out[layer]

        h2 = rms_norm(x, norm_ffn_g[layer])
        idx = layer // 2
        if layer % 2 == 0:
            x = x + swiglu(h2, ffn_w1[idx], ffn_w3[idx], ffn_w2[idx])
        else:
            x = x + moe_ffn(h2, moe_router[idx], moe_w1[idx], moe_w3[idx], moe_w2[idx])
    return x
```

```python
import math
import numpy as np
import ml_dtypes
import concourse.bass as bass
import concourse.mybir as mybir
from concourse.bass_utils import run_bass_kernel_spmd

F32 = mybir.dt.float32
BF16 = mybir.dt.bfloat16
AF = mybir.ActivationFunctionType
ALU = mybir.AluOpType

D = 1024
DEPTH = 4
NCORES = 8
EPS = 1e-6
IN_COLS = 2560
D_FF = 2816
D_FFE = 3584
NEXP = 8
GATE_ENG = "dve"
H_DF = 4
SLOPES = [2.0 ** (-8.0 * (h + 1) / H_DF) for h in range(H_DF)]


class Buf:
    __slots__ = ("h", "lw", "rd", "name")

    def __init__(self, h, name=""):
        self.h = h
        self.lw = None
        self.rd = {}
        self.name = name

    def __getitem__(self, idx):
        return self.h[idx]

    def view(self, idx):
        return Buf(self.h[idx], self.name)


class Prog:
    SEM_ROT = 30000

    def __init__(self, nc):
        self.nc = nc
        self.eng = {"pe": nc.tensor, "act": nc.scalar, "dve": nc.vector, "pool": nc.gpsimd, "sp": nc.sync}
        self.sem = {}
        self.cnt = {}
        self.nsem = 0
        for e in self.eng:
            self._newsem(e)
        self.seen = {e: {} for e in self.eng}
        self.ndsem = 16
        self.dsem = [nc.alloc_semaphore(f"dq{i}") for i in range(self.ndsem)]
        self.dcnt = [0] * self.ndsem
        self.dnext = 0
        self.ninst = 0
        self._uid = 0
        self.pending = {}

    def _newsem(self, e):
        self.nsem += 1
        self.sem[e] = self.nc.alloc_semaphore(f"s{e}{self.nsem}")
        self.cnt[e] = 0

    def uid(self, p="t"):
        self._uid += 1
        return f"{p}{self._uid}"

    def sb(self, shape, dt=F32, name=None):
        return Buf(self.nc.alloc_sbuf_tensor(name or self.uid("sb"), list(shape), dt), name or "")

    def ps(self, shape, dt=F32, name=None):
        return Buf(self.nc.alloc_psum_tensor(name or self.uid("ps"), list(shape), dt), name or "")

    def dram(self, name, shape, dt, kind="Internal"):
        return Buf(self.nc.dram_tensor(name, list(shape), dt, kind=kind).ap(), name)

    def _collect(self, reads, writes):
        t = {}

        def add(k, v):
            if t.get(k, 0) < v:
                t[k] = v

        for b in reads:
            if b.lw is not None:
                add(*b.lw)
        for b in writes:
            if b.lw is not None:
                add(*b.lw)
            for k, v in b.rd.items():
                add(k, v)
        return t

    def _wait(self, e, tickets, skip_own=False):
        own = self.sem[e]
        seen = self.seen[e]
        for s, v in tickets.items():
            if skip_own and s is own:
                continue
            if seen.get(s, 0) < v:
                self.eng[e].wait_ge(s, v)
                seen[s] = v

    def _mark(self, reads, writes, tk):
        s, v = tk
        for b in reads:
            if b.rd.get(s, 0) < v:
                b.rd[s] = v
        for b in writes:
            b.lw = tk
            b.rd = {}

    def op(self, e, fn, reads=(), writes=(), sig=True):
        self._wait(e, self._collect(reads, writes), skip_own=(e == "pe"))
        inst = fn(self.eng[e])
        self.ninst += 1
        if sig:
            if self.cnt[e] >= self.SEM_ROT:
                self._newsem(e)
            self.cnt[e] += 1
            inst.then_inc(self.sem[e], 1)
            tk = (self.sem[e], self.cnt[e])
        else:
            assert self.cnt[e] < self.SEM_ROT + 10000
            tk = (self.sem[e], self.cnt[e] + 1)
        self._mark(reads, writes, tk)
        return inst

    def dma(self, q, out_ap, in_ap, reads=(), writes=(), **kw):
        t = self._collect(reads, writes)
        k = self.dnext
        self.dnext = (self.dnext + 1) % self.ndsem
        if self.dcnt[k] > 0:
            s = self.dsem[k]
            if t.get(s, 0) < self.dcnt[k]:
                t[s] = self.dcnt[k]
        self._wait(q, t)
        inst = self.eng[q].dma_start(out=out_ap, in_=in_ap, **kw)
        self.ninst += 1
        self.dcnt[k] += 16
        inst.then_inc(self.dsem[k], 16)
        self._mark(reads, writes, (self.dsem[k], self.dcnt[k]))
        return inst

    def finish(self):
        t = {}
        for k in range(self.ndsem):
            if self.dcnt[k] > 0:
                t[self.dsem[k]] = self.dcnt[k]
        for e in ("pe", "act", "dve", "pool"):
            if self.cnt[e] > 0:
                t[self.sem[e]] = self.cnt[e]
        self._wait("sp", t)

    def mm(self, out_b, out_ap, l_b, l_ap, r_b, r_ap, start, stop, sig=None):
        if sig is None:
            sig = stop
        return self.op("pe", lambda en: en.matmul(out_ap, lhsT=l_ap, rhs=r_ap, start=start, stop=stop),
                       reads=(l_b, r_b) if start else (l_b, r_b), writes=(out_b,), sig=sig)

    def actf(self, out_b, out_ap, in_b, in_ap, func, bias=None, scale=None, extra_reads=()):
        kw = {}
        if bias is not None:
            kw["bias"] = bias
        if scale is not None:
            kw["scale"] = scale
        return self.op("act", lambda en: en.activation(out=out_ap, in_=in_ap, func=func, **kw),
                       reads=(in_b,) + tuple(extra_reads), writes=(out_b,))

    def tt(self, e, out_b, out_ap, a_b, a_ap, b_b, b_ap, op):
        return self.op(e, lambda en: en.tensor_tensor(out=out_ap, in0=a_ap, in1=b_ap, op=op),
                       reads=(a_b, b_b), writes=(out_b,))

    def ts(self, e, out_b, out_ap, a_b, a_ap, s1, op0, s2=None, op1=None, extra_reads=()):
        if op1 is None:
            return self.op(e, lambda en: en.tensor_scalar(out=out_ap, in0=a_ap, scalar1=s1, scalar2=None, op0=op0),
                           reads=(a_b,) + tuple(extra_reads), writes=(out_b,))
        return self.op(e, lambda en: en.tensor_scalar(out=out_ap, in0=a_ap, scalar1=s1, scalar2=s2, op0=op0, op1=op1),
                       reads=(a_b,) + tuple(extra_reads), writes=(out_b,))

    def stt(self, out_b, out_ap, a_b, a_ap, scalar, b_b, b_ap, op0, op1, extra_reads=()):
        return self.op("dve", lambda en: en.scalar_tensor_tensor(out=out_ap, in0=a_ap, scalar=scalar, in1=b_ap,
                                                                 op0=op0, op1=op1),
                       reads=(a_b, b_b) + tuple(extra_reads), writes=(out_b,))

    def copy(self, e, out_b, out_ap, in_b, in_ap):
        if e == "act":
            return self.op("act", lambda en: en.copy(out=out_ap, in_=in_ap), reads=(in_b,), writes=(out_b,))
        return self.op(e, lambda en: en.tensor_copy(out=out_ap, in_=in_ap), reads=(in_b,), writes=(out_b,))

    def memset(self, e, out_b, out_ap, val):
        return self.op(e, lambda en: en.memset(out_ap, val), reads=(), writes=(out_b,))

    def recip(self, out_b, out_ap, in_b, in_ap):
        return self.op("dve", lambda en: en.reciprocal(out=out_ap, in_=in_ap), reads=(in_b,), writes=(out_b,))


def _prog_patch():
    def op(self, e, fn, reads=(), writes=(), sig=True):
        self._wait(e, self._collect(reads, writes), skip_own=(e == "pe"))
        inst = fn(self.eng[e])
        self.ninst += 1
        pend = self.pending.get(e, False)
        if sig:
            if self.cnt[e] >= self.SEM_ROT and not pend:
                self._newsem(e)
            self.cnt[e] += 1
            inst.then_inc(self.sem[e], 1)
            tk = (self.sem[e], self.cnt[e])
            self.pending[e] = False
        else:
            tk = (self.sem[e], self.cnt[e] + 1)
            self.pending[e] = True
        self._mark(reads, writes, tk)
        return inst

    def barrier(self):
        t = {}
        for k in range(self.ndsem):
            if self.dcnt[k] > 0:
                t[self.dsem[k]] = self.dcnt[k]
        for e in ("pe", "act", "dve", "pool", "sp"):
            assert not self.pending.get(e, False)
            if self.cnt[e] > 0:
                t[self.sem[e]] = self.cnt[e]
        for e in ("pe", "act", "dve", "pool", "sp"):
            self._wait(e, t)

    Prog.op = op
    Prog.barrier = barrier


_prog_patch()


CB = {}
CF = {}


def _layout_consts():
    off = 0
    for name, w in (("ones", 128), ("ident", 128), ("bd64", 128), ("bd32", 128), ("tincl", 128),
                    ("m0", 512), ("m1", 512), ("m2", 512), ("m3", 512)):
        CB[name] = (off, w)
        off += w
    CB["_n"] = off
    off = 0
    for name, w in (("ident", 128), ("chm0", 128), ("chm4", 128), ("dfd0", 128), ("dfd1", 128), ("dfd2", 128),
                    ("dfd3", 128), ("dfb0", 36), ("dfb1", 36), ("dfb2", 36), ("dfb3", 36), ("sel", 8 * 128),
                    ("sclrow", 1), ("ones", 128)):
        CF[name] = (off, w)
        off += w
    CF["_n"] = off


_layout_consts()


def make_consts():
    cb = np.zeros((128, CB["_n"]), np.float32)
    cf = np.zeros((128, CF["_n"]), np.float32)
    i = np.arange(128)

    def setb(n, a):
        o, w = CB[n]
        cb[:, o:o + w] = a

    def setf(n, a):
        o, w = CF[n]
        cf[:a.shape[0], o:o + w] = a

    setb("ones", np.ones((128, 128)))
    setb("ident", np.eye(128))
    setb("bd64", (i[:, None] // 64 == i[None, :] // 64).astype(np.float32))
    setb("bd32", (i[:, None] // 32 == i[None, :] // 32).astype(np.float32))
    setb("tincl", (i[:, None] >= i[None, :]).astype(np.float32))
    q = np.arange(512)
    for m in range(4):
        setb(f"m{m}", ((128 * m + i[:, None]) < q[None, :]).astype(np.float32))
    setf("ident", np.eye(128, dtype=np.float32))
    qq = i[:, None]
    kk = i[None, :]
    setf("chm0", np.where((kk >= 64) & (qq < 64), -1e30, 0.0).astype(np.float32))
    setf("chm4", np.where((kk < 64) & (qq >= 64), -1e30, 0.0).astype(np.float32))
    kk2 = i[:, None]
    qq2 = i[None, :]
    for h in range(4):
        sl = SLOPES[h]
        t = -sl * np.abs(qq2 - kk2) + sl * qq2
        t = np.where((kk2 // 64) <= (qq2 // 64), t, -1e30)
        setf(f"dfd{h}", t.astype(np.float32))
        setf(f"dfb{h}", (sl * (i[:, None] - 128.0 * (np.arange(36)[None, :] - 3.0))).astype(np.float32))
    sel = np.zeros((128, 8, 128), np.float32)
    for e in range(8):
        sel[e, e, :] = 1.0
    setf("sel", sel.reshape(128, 8 * 128))
    scl = np.ones((128, 1), np.float32)
    scl[64, 0] = math.sqrt(64.0 * EPS)
    setf("sclrow", scl)
    setf("ones", np.ones((128, 128), np.float32))
    return cb.astype(ml_dtypes.bfloat16), cf


SP = {"g1": (0, 8), "g2": (8, 8), "go": (16, 8), "d": (24, 2), "bglu": (26, 2), "chq": (28, 1), "chk": (29, 1),
      "dfq": (30, 1), "dfk": (31, 1), "lre": (32, 8), "lim": (40, 8), "ldt": (48, 8), "dfl": (56, 128), "goh": (184, 16)}
SPN = 200


def pack_small(inp, l):
    a = np.zeros((128, SPN), np.float32)

    def fm(v):
        return np.ascontiguousarray(v.reshape(-1, 128).T)

    a[:, 0:8] = fm(inp["norm_mix_g"][l])
    a[:, 8:16] = fm(inp["norm_ffn_g"][l])
    a[:, 16:24] = fm(inp["out_norm_g"][l])
    a[:, 24:26] = fm(inp["ssm_d"][l])
    a[:, 26:28] = fm(inp["ssm_b_glu"][l])
    a[:, 28] = np.tile(inp["ch_q_norm_g"][l], 2)
    a[:, 29] = np.tile(inp["ch_k_norm_g"][l], 2)
    a[:, 30] = np.tile(inp["df_q_norm_g"][l].reshape(-1), 2)
    a[:, 31] = np.tile(inp["df_k_norm_g"][l].reshape(-1), 2)
    for nm, key in (("lre", "ssm_lam_re"), ("lim", "ssm_lam_im")):
        o = SP[nm][0]
        v = inp[key][l].reshape(8, 2, 64)
        a[:, o:o + 8] = v.transpose(1, 2, 0).reshape(128, 8)
    o = SP["ldt"][0]
    v = np.repeat(inp["ssm_log_dt"][l].reshape(8, 2, 1), 64, axis=2)
    a[:, o:o + 8] = v.transpose(1, 2, 0).reshape(128, 8)
    o = SP["dfl"][0]
    a[:, o:o + 128] = inp["df_lambda"][l].reshape(1, 128)
    a[0:64, 184:200] = inp["out_norm_g"][l].reshape(16, 64).T
    return a


def pack_ssm_bc(inp, l):
    b_re, b_im = inp["ssm_b_re"][l], inp["ssm_b_im"][l]
    c_re, c_im = inp["ssm_c_re"][l], inp["ssm_c_im"][l]
    outs = []
    for b in (b_re, b_im):
        pad = np.zeros((8, 128, 128), np.float32)
        for g in range(16):
            j, g2 = g // 2, g % 2
            k0 = (g % 8) * 16
            pad[j, k0:k0 + 16, g2 * 64:(g2 + 1) * 64] = b[g].T
        outs.append(np.ascontiguousarray(pad.transpose(1, 0, 2)))
    for c in (c_re, c_im):
        pad = np.zeros((8, 128, 128), np.float32)
        for g in range(16):
            j, g2 = g // 2, g % 2
            m0 = (g % 8) * 16
            pad[j, g2 * 64:(g2 + 1) * 64, m0:m0 + 16] = c[g].T
        outs.append(np.ascontiguousarray(pad.transpose(1, 0, 2)))
    return outs


def pack_chb(inp, l):
    rb = inp["ch_rel_bias"][l]
    q = np.arange(128)[:, None]
    k = np.arange(128)[None, :]
    out = np.zeros((128, 4, 5, 128), np.float32)
    for d in range(5):
        idx = np.clip(128 * d + q - k, -128, 128) + 128
        out[:, :, d, :] = rb[:, idx].transpose(1, 0, 2)
    return out


from contextlib import ExitStack

ALL_PHASES = ("n1", "ssm", "sb", "ch", "df", "op", "ffn")


def build(S, layer_list, phases=ALL_PHASES, io=None, first_layer_from_x=True, last_to_y=True):
    nc = bass.Bass("TRN2", target_bir_lowering=False)
    p = Prog(nc)
    NB = S // 512
    NKT = S // 128
    io = io or {}
    used_inputs = []

    def dr(name, shape, dt, kind="Internal"):
        if name in io:
            kind = "ExternalInput" if io[name] == "in" else "ExternalOutput"
        if kind == "ExternalInput":
            used_inputs.append(name)
        return p.dram(name, shape, dt, kind)

    xT = dr("xT", [1024, S], F32, "ExternalInput")
    yT = dr("yT", [1024, S], F32, "ExternalOutput")
    cb_d = dr("cb", [128, CB["_n"]], BF16, "ExternalInput")
    cf_d = dr("cf", [128, CF["_n"]], F32, "ExternalInput")
    XR = dr("XR", [1024, S], F32)
    HT = dr("HT", [1024, S], BF16)
    UT = dr("UT", [256, S], F32)
    QT = {g: dr(f"QT{g}", [256, S], BF16) for g in ("sb", "ch", "df")}
    KT = {g: dr(f"KT{g}", [256, S], BF16) for g in ("sb", "ch", "df")}
    VV = {g: dr(f"VV{g}", [S, 256], BF16) for g in ("sb", "ch", "df")}
    MIX = dr("MIX", [1024, S], BF16)
    GT = dr("GT", [8, S], F32)

    PSALL = nc.alloc_psum_tensor("psall", [128, 8, 512], F32)
    PS = [Buf(PSALL[:, i, :], f"psb{i}") for i in range(8)]

    def ps2(i):
        return PSALL[:, i:i + 2, :]
    cbt = p.sb([128, CB["_n"]], BF16, name="cbt")
    cft = p.sb([128, CF["_n"]], F32, name="cft")
    p.dma("sp", cbt[:], cb_d[:], reads=(cb_d,), writes=(cbt,))
    p.dma("sp", cft[:], cf_d[:], reads=(cf_d,), writes=(cft,))

    def cbs(name, rows=128, c0=0, c1=None):
        o, w = CB[name]
        c1 = w if c1 is None else c1
        return cbt[0:rows, o + c0:o + c1]

    def cfs(name, r0=0, r1=128, c0=0, c1=None):
        o, w = CF[name]
        c1 = w if c1 is None else c1
        return cft[r0:r1, o + c0:o + c1]

    bct = p.sb([128, 16], F32, name="bct")
    _bias_cols = {}
    for _h in range(4):
        for _o in range(4):
            _v = float(SLOPES[_h] * 128 * _o)
            p.memset("pool", bct, bct[:, _h * 4 + _o:_h * 4 + _o + 1], _v)
            _bias_cols[round(_v, 6)] = _h * 4 + _o

    def bias_const(v):
        c = _bias_cols[round(float(v), 6)]
        return bct[:, c:c + 1]

    class Phase:
        def __init__(self):
            self.stk = ExitStack()

        def sb(self, shape, dt=F32):
            h = self.stk.enter_context(nc.sbuf_tensor(p.uid("t"), list(shape), dt))
            return Buf(h)

        def close(self):
            p.barrier()
            self.stk.close()

    def ld_w(ph, name, shape_dram, pattern, sb_shape, **kw):
        d = dr(name, shape_dram, F32, "ExternalInput")
        t = ph.sb(sb_shape, BF16)
        return d, t

    wdecl = {}

    def wd(name, shape):
        if name not in wdecl:
            wdecl[name] = dr(name, shape, F32, "ExternalInput")
        return wdecl[name]

    def cast_load(dst_b, dst_ap, src_b, src_ap):
        p.dma("pool", dst_ap, src_ap, reads=(src_b,), writes=(dst_b,), max_dma_last_dim=4096)

    def rms_block(ph, xt, spk, gofs, hT, tmp_sq, tmp_r, psb, h32=None):
        p.actf(tmp_sq, tmp_sq[:], xt, xt[:], AF.Square)
        for c in range(8):
            p.mm(psb, psb[:], cbt, cbs("ones"), tmp_sq, tmp_sq[:, c, :], start=(c == 0), stop=(c == 7))
        p.actf(tmp_r, tmp_r[:], psb, psb[:], AF.Ln, bias=EPS, scale=1.0 / 1024.0)
        p.actf(tmp_r, tmp_r[:], tmp_r, tmp_r[:], AF.Exp, scale=-0.5)
        for c in range(8):
            p.stt(hT, hT[:, c, :], xt, xt[:, c, :], spk[:, gofs + c:gofs + c + 1], tmp_r, tmp_r[:],
                  ALU.mult, ALU.mult, extra_reads=(spk,))
            if h32 is not None:
                p.stt(h32, h32[:, c, :], xt, xt[:, c, :], spk[:, gofs + c:gofs + c + 1], tmp_r, tmp_r[:],
                      ALU.mult, ALU.mult, extra_reads=(spk,))

    def group_norm(src_b, src_ap, rows, N, bdname, gsz, gain_ap, gain_b, out_b, out_ap, sq, rr, psb, gscale=None):
        p.actf(sq, sq[0:rows, 0:N], src_b, src_ap, AF.Square)
        p.mm(psb, psb[0:rows, 0:N], cbt, cbs(bdname, rows=rows, c1=rows), sq, sq[0:rows, 0:N], start=True, stop=True)
        p.actf(rr, rr[0:rows, 0:N], psb, psb[0:rows, 0:N], AF.Ln, bias=EPS, scale=1.0 / gsz)
        p.actf(rr, rr[0:rows, 0:N], rr, rr[0:rows, 0:N], AF.Exp, scale=-0.5)
        p.stt(out_b, out_ap, src_b, src_ap, gain_ap, rr, rr[0:rows, 0:N], ALU.mult, ALU.mult, extra_reads=(gain_b,))

    def phase_n1(l, xsrc):
        ph = Phase()
        spk = ph.sb([128, SPN])
        spd = wd(f"sp{l}", [128, SPN])
        p.dma("sp", spk[:], spd[:], reads=(spd,), writes=(spk,))
        wind = wd(f"w_in{l}", [1024, IN_COLS])
        win = ph.sb([128, 8, IN_COLS], BF16)
        wv = wind[:].rearrange("(c p) n -> p c n", p=128)
        for c in range(8):
            cast_load(win, win[:, c, :], wind, wv[:, c, :])
        gq = ph.sb([128, 4])
        p.ts("dve", gq, gq[:, 0:1], spk, spk[:, 28:29], 0.125, ALU.mult)
        p.copy("dve", gq, gq[:, 1:2], spk, spk[:, 29:30])
        p.ts("dve", gq, gq[:, 2:3], spk, spk[:, 30:31], 32.0 ** -0.5, ALU.mult)
        p.copy("dve", gq, gq[:, 3:4], spk, spk[:, 31:32])
        xts = [ph.sb([128, 8, 512]) for _ in range(3)]
        sqs = [ph.sb([128, 8, 512], BF16) for _ in range(2)]
        hTs = [ph.sb([128, 8, 512], BF16) for _ in range(2)]
        rrs = [ph.sb([128, 512]) for _ in range(2)]
        evs = [ph.sb([128, 512], BF16) for _ in range(4)]
        evf = [ph.sb([128, 512]) for _ in range(2)]
        sq2 = [ph.sb([128, 512], BF16) for _ in range(2)]
        rr2 = [ph.sb([128, 512]) for _ in range(2)]
        vts = [ph.sb([128, 768], BF16) for _ in range(2)]
        xv = xsrc[:].rearrange("(c p) t -> p c t", p=128)
        cnt = {"ev": 0, "ps": 0, "nm": 0}
        fm = [("u", 0, 0), ("u", 128, 1), ("qsb", 256, 0), ("qsb", 384, 1), ("ksb", 512, 0), ("ksb", 640, 1),
              ("qch", 1024, 0), ("qch", 1152, 1), ("kch", 1280, 0), ("kch", 1408, 1),
              ("qdf", 1792, 0), ("qdf", 1920, 1), ("kdf", 2048, 0), ("kdf", 2176, 1)]

        def stageL(tb):
            xt = xts[tb % 3]
            p.dma("sp", xt[:], xv[:, :, tb * 512:(tb + 1) * 512], reads=(xsrc,), writes=(xt,))

        def stageA(tb):
            rms_block(ph, xts[tb % 3], spk, SP["g1"][0], hTs[tb % 2], sqs[tb % 2], rrs[tb % 2], PS[7])

        def stageB(tb):
            hT = hTs[tb % 2]
            tsl = slice(tb * 512, (tb + 1) * 512)
            pending = [None]

            def finish_norm():
                if pending[0] is None:
                    return
                kind, rows, psb, ev, sq_, rr_, pn = pending[0]
                pending[0] = None
                gi = {"qch": 0, "kch": 1, "qdf": 2, "kdf": 3}[kind]
                ch = kind.endswith("ch")
                p.mm(pn, pn[:], cbt, cbs("bd64" if ch else "bd32"), sq_, sq_[:], start=True, stop=True)
                p.actf(rr_, rr_[:], pn, pn[:], AF.Ln, bias=EPS, scale=1.0 / (64.0 if ch else 32.0))
                p.actf(rr_, rr_[:], rr_, rr_[:], AF.Exp, scale=-0.5)
                p.stt(ev, ev[:], psb, psb[:], gq[:, gi:gi + 1], rr_, rr_[:], ALU.mult, ALU.mult, extra_reads=(gq,))
                dst = (QT if kind[0] == "q" else KT)["ch" if ch else "df"]
                p.dma("sp", dst[rows, tsl], ev[:], reads=(ev,), writes=(dst,))

            for kind, col, half in fm:
                psb = PS[cnt["ps"] % 4]
                cnt["ps"] += 1
                for c in range(8):
                    p.mm(psb, psb[:], win, win[:, c, col:col + 128], hT, hT[:, c, :], start=(c == 0), stop=(c == 7))
                finish_norm()
                rows = slice(half * 128, (half + 1) * 128)
                if kind == "u":
                    ev = evf[cnt["ev"] % 2]
                    cnt["ev"] += 1
                    p.copy("act", ev, ev[:], psb, psb[:])
                    p.dma("sp", UT[rows, tsl], ev[:], reads=(ev,), writes=(UT,))
                elif kind == "qsb":
                    ev = evs[cnt["ev"] % 4]
                    cnt["ev"] += 1
                    p.actf(ev, ev[:], psb, psb[:], AF.Copy, scale=0.125)
                    p.dma("sp", QT["sb"][rows, tsl], ev[:], reads=(ev,), writes=(QT["sb"],))
                elif kind == "ksb":
                    ev = evs[cnt["ev"] % 4]
                    cnt["ev"] += 1
                    p.copy("act", ev, ev[:], psb, psb[:])
                    p.dma("sp", KT["sb"][rows, tsl], ev[:], reads=(ev,), writes=(KT["sb"],))
                else:
                    ev = evs[cnt["ev"] % 4]
                    cnt["ev"] += 1
                    k2 = cnt["nm"] % 2
                    cnt["nm"] += 1
                    p.actf(sq2[k2], sq2[k2][:], psb, psb[:], AF.Square)
                    pending[0] = (kind, rows, psb, ev, sq2[k2], rr2[k2], PS[4 + k2])
            for tt_ in range(4):
                vt = vts[tt_ % 2]
                tok = slice(tt_ * 128, (tt_ + 1) * 128)
                for gi, (g, col) in enumerate((("sb", 768), ("ch", 1536), ("df", 2304))):
                    psb = PS[cnt["ps"] % 4]
                    cnt["ps"] += 1
                    for c in range(8):
                        p.mm(psb, psb[:, 0:256], hT, hT[:, c, tok], win, win[:, c, col:col + 256],
                             start=(c == 0), stop=(c == 7))
                    if tt_ == 0 and gi == 0:
                        finish_norm()
                    p.copy("act" if gi != 1 else "dve", vt, vt[:, gi * 256:(gi + 1) * 256], psb, psb[:, 0:256])
                t0 = tb * 512 + tt_ * 128
                for gi, g in enumerate(("sb", "ch", "df")):
                    p.dma("sp", VV[g][t0:t0 + 128, :], vt[:, gi * 256:(gi + 1) * 256], reads=(vt,), writes=(VV[g],))

        stageL(0)
        if NB > 1:
            stageL(1)
        stageA(0)
        for tb in range(NB):
            if tb + 2 < NB:
                stageL(tb + 2)
            if tb + 1 < NB:
                stageA(tb + 1)
            stageB(tb)
        ph.close()

    def phase_op(l, xsrc, moe_idx):
        ph = Phase()
        spk = ph.sb([128, SPN])
        spd = wd(f"sp{l}", [128, SPN])
        p.dma("sp", spk[:], spd[:], reads=(spd,), writes=(spk,))
        wod = wd(f"w_out{l}", [1024, 1024])
        wo = ph.sb([128, 8, 1024], BF16)
        wv = wod[:].rearrange("(c p) n -> p c n", p=128)
        for c in range(8):
            cast_load(wo, wo[:, c, :], wod, wv[:, c, :])
        if moe_idx is not None:
            wrd = wd(f"mr{moe_idx}", [1024, 8])
            wr = ph.sb([128, 8, 8])
            p.dma("sp", wr[:], wrd[:].rearrange("(c p) e -> p c e", p=128), reads=(wrd,), writes=(wr,))
            h32s = [ph.sb([128, 8, 512]) for _ in range(2)]
            lg = ph.sb([128, 4, 8])
            wk = [ph.sb([128, 4, 8]) for _ in range(4)]
            mx = [ph.sb([128, 4, 1]) for _ in range(3)]
            gts = ph.sb([8, 512])
        xts = [ph.sb([128, 8, 512]) for _ in range(2)]
        xns = [ph.sb([128, 8, 512]) for _ in range(2)]
        mxs = [ph.sb([128, 8, 512], BF16) for _ in range(2)]
        sqs = [ph.sb([128, 8, 512], BF16) for _ in range(2)]
        hTs = [ph.sb([128, 8, 512], BF16) for _ in range(2)]
        rrs = [ph.sb([128, 512]) for _ in range(2)]
        xv = xsrc[:].rearrange("(c p) t -> p c t", p=128)
        xo = XR[:].rearrange("(c p) t -> p c t", p=128)
        mv = MIX[:].rearrange("(c p) t -> p c t", p=128)
        hv = HT[:].rearrange("(c p) t -> p c t", p=128)
        cnt = {"ps": 0}

        def stageL(tb):
            tsl = slice(tb * 512, (tb + 1) * 512)
            xt, mt = xts[tb % 2], mxs[tb % 2]
            p.dma("sp", xt[:], xv[:, :, tsl], reads=(xsrc,), writes=(xt,))
            p.dma("sp", mt[:], mv[:, :, tsl], reads=(MIX,), writes=(mt,))

        def stageA(tb):
            xt, xn, mt, sq = xts[tb % 2], xns[tb % 2], mxs[tb % 2], sqs[tb % 2]
            tsl = slice(tb * 512, (tb + 1) * 512)
            for ft in range(8):
                psb = PS[cnt["ps"] % 4]
                cnt["ps"] += 1
                for c in range(8):
                    p.mm(psb, psb[:], wo, wo[:, c, ft * 128:(ft + 1) * 128], mt, mt[:, c, :],
                         start=(c == 0), stop=(c == 7))
                p.tt("dve", xn, xn[:, ft, :], xt, xt[:, ft, :], psb, psb[:], ALU.add)
            p.dma("sp", xo[:, :, tsl], xn[:], reads=(xn,), writes=(XR,))
            p.actf(sq, sq[:], xn, xn[:], AF.Square)

        def stageB(tb):
            xn, sq, hT, rr = xns[tb % 2], sqs[tb % 2], hTs[tb % 2], rrs[tb % 2]
            tsl = slice(tb * 512, (tb + 1) * 512)
            h32 = h32s[tb % 2] if moe_idx is not None else None
            psb = PS[7]
            gofs = SP["g2"][0]
            for c in range(8):
                p.mm(psb, psb[:], cbt, cbs("ones"), sq, sq[:, c, :], start=(c == 0), stop=(c == 7))
            p.actf(rr, rr[:], psb, psb[:], AF.Ln, bias=EPS, scale=1.0 / 1024.0)
            p.actf(rr, rr[:], rr, rr[:], AF.Exp, scale=-0.5)
            for c in range(8):
                p.stt(hT, hT[:, c, :], xn, xn[:, c, :], spk[:, gofs + c:gofs + c + 1], rr, rr[:],
                      ALU.mult, ALU.mult, extra_reads=(spk,))
                if h32 is not None:
                    p.stt(h32, h32[:, c, :], xn, xn[:, c, :], spk[:, gofs + c:gofs + c + 1], rr, rr[:],
                          ALU.mult, ALU.mult, extra_reads=(spk,))
            p.dma("sp", hv[:, :, tsl], hT[:], reads=(hT,), writes=(HT,))
            if moe_idx is not None:
                pl = PS[6]
                for tt_ in range(4):
                    for c in range(8):
                        p.mm(pl, pl[:, tt_ * 8:(tt_ + 1) * 8], h32, h32[:, c, tt_ * 128:(tt_ + 1) * 128],
                             wr, wr[:, c, :], start=(c == 0), stop=(c == 7))
                p.copy("dve", lg, lg[:], pl, pl[:, 0:32].rearrange("p (a e) -> p a e", e=8))
                m1, m2, ssum = mx
                AX = mybir.AxisListType.X
                p.op("dve", lambda en: en.tensor_reduce(out=m1[:], in_=lg[:], axis=AX, op=ALU.max),
                     reads=(lg,), writes=(m1,))
                p.tt("dve", wk[0], wk[0][:], lg, lg[:], m1, m1[:].broadcast_to([128, 4, 8]), ALU.is_equal)
                p.stt(wk[1], wk[1][:].rearrange("p a e -> p (a e)"), wk[0], wk[0][:].rearrange("p a e -> p (a e)"),
                      -1e30, lg, lg[:].rearrange("p a e -> p (a e)"), ALU.mult, ALU.add)
                p.op("dve", lambda en: en.tensor_reduce(out=m2[:], in_=wk[1][:], axis=AX, op=ALU.max),
                     reads=(wk[1],), writes=(m2,))
                p.tt("dve", wk[0], wk[0][:], lg, lg[:], m2, m2[:].broadcast_to([128, 4, 8]), ALU.is_ge)
                p.tt("dve", wk[1], wk[1][:], lg, lg[:], m1, m1[:].broadcast_to([128, 4, 8]), ALU.subtract)
                p.actf(wk[2], wk[2][:], wk[1], wk[1][:], AF.Exp)
                p.tt("dve", wk[2], wk[2][:], wk[2], wk[2][:], wk[0], wk[0][:], ALU.mult)
                p.op("dve", lambda en: en.tensor_reduce(out=ssum[:], in_=wk[2][:], axis=AX, op=ALU.add),
                     reads=(wk[2],), writes=(ssum,))
                p.recip(ssum, ssum[:], ssum, ssum[:])
                p.tt("dve", wk[3], wk[3][:], wk[2], wk[2][:], ssum, ssum[:].broadcast_to([128, 4, 8]), ALU.mult)
                pt = PS[5]
                for tt_ in range(4):
                    p.op("pe", lambda en: en.transpose(pt[0:8, tt_ * 128:(tt_ + 1) * 128], wk[3][:, tt_, :],
                                                       cfs("ident")),
                         reads=(wk[3], cft), writes=(pt,), sig=(tt_ == 3))
                p.copy("act", gts, gts[:], pt, pt[0:8, :])
                p.dma("sp", GT[:, tsl], gts[:], reads=(gts,), writes=(GT,))

        stageL(0)
        if NB > 1:
            stageL(1)
        stageA(0)
        for tb in range(NB):
            if tb + 1 < NB:
                stageA(tb + 1)
            if tb + 2 < NB:
                stageL(tb + 2)
            stageB(tb)
        ph.close()

    def phase_ffn(l, moe_idx, dense_idx, dst):
        ph = Phase()
        moe = moe_idx is not None
        FC = 512 if moe else 256
        nfi = FC // 128
        dff = D_FFE if moe else D_FF
        nchunk = dff // FC
        nexp = NEXP if moe else 1
        if moe:
            w1d = wd(f"mw1_{moe_idx}", [NEXP, 1024, D_FFE])
            w3d = wd(f"mw3_{moe_idx}", [NEXP, 1024, D_FFE])
            w2d = wd(f"mw2_{moe_idx}", [NEXP, D_FFE, 1024])
        else:
            w1d = wd(f"w1_{dense_idx}", [1024, D_FF])
            w3d = wd(f"w3_{dense_idx}", [1024, D_FF])
            w2d = wd(f"w2_{dense_idx}", [D_FF, 1024])
        HTOK = min(2048, S)
        nhalf = S // HTOK
        ntb = HTOK // 512
        acc = ph.sb([128, 8, HTOK])
        h2 = ph.sb([128, 8, HTOK], BF16)
        w1s = [ph.sb([128, 8, FC], BF16) for _ in range(2)]
        w3s = [ph.sb([128, 8, FC], BF16) for _ in range(2)]
        w2s = [ph.sb([128, nfi, 1024], BF16) for _ in range(2)]
        sas = [ph.sb([128, 512]) for _ in range(3)]
        gs = [ph.sb([128, nfi, 512], BF16) for _ in range(2)]
        if moe:
            gtile = ph.sb([8, HTOK])
            gbhs = [ph.sb([128, ntb, 512]) for _ in range(2)]
        xo = XR[:].rearrange("(c p) t -> p c t", p=128)
        do = dst[:].rearrange("(c p) t -> p c t", p=128)
        hv = HT[:].rearrange("(c p) t -> p c t", p=128)
        accv = [[acc.view((slice(None), ft, slice(tb * 512, (tb + 1) * 512))) for tb in range(ntb)] for ft in range(8)]
        chunks = [(e, fc) for e in range(nexp) for fc in range(nchunk)]
        nck = len(chunks)

        def load_chunk(ci):
            e, fc = chunks[ci]
            w1, w3, w2 = w1s[ci % 2], w3s[ci % 2], w2s[ci % 2]
            fsl = slice(fc * FC, (fc + 1) * FC)
            if moe:
                s1 = w1d[e].rearrange("(c p) n -> p c n", p=128)
                s3 = w3d[e].rearrange("(c p) n -> p c n", p=128)
                s2 = w2d[e, fsl, :].rearrange("(i p) n -> p i n", p=128)
            else:
                s1 = w1d[:].rearrange("(c p) n -> p c n", p=128)
                s3 = w3d[:].rearrange("(c p) n -> p c n", p=128)
                s2 = w2d[fsl, :].rearrange("(i p) n -> p i n", p=128)
            cast_load(w1, w1[:], w1d, s1[:, :, fsl])
            cast_load(w3, w3[:], w3d, s3[:, :, fsl])
            cast_load(w2, w2[:], w2d, s2)

        nsa = [0]

        def stage1(u):
            ci, tb = divmod(u, ntb)
            e, fc = chunks[ci]
            w1, w3 = w1s[ci % 2], w3s[ci % 2]
            tsl = slice(tb * 512, (tb + 1) * 512)
            g = gs[u % 2]
            if moe:
                gbh = gbhs[e % 2]
                if fc == 0 and tb == 0:
                    for tb2 in range(ntb):
                        pg = PS[6 + tb2 % 2]
                        p.mm(pg, pg[:], cft, cfs("sel", r0=0, r1=8, c0=e * 128, c1=(e + 1) * 128),
                             gtile, gtile[:, tb2 * 512:(tb2 + 1) * 512], start=True, stop=True)
                        p.copy("act", gbh, gbh[:, tb2, :], pg, pg[:])
                gb_ap = gbh[:, tb, :]
            for i in range(nfi):
                pa, pb = PS[(2 * i) % 4], PS[(2 * i + 1) % 4]
                for c in range(8):
                    p.mm(pa, pa[:], w1, w1[:, c, i * 128:(i + 1) * 128], h2, h2[:, c, tsl],
                         start=(c == 0), stop=(c == 7))
                for c in range(8):
                    p.mm(pb, pb[:], w3, w3[:, c, i * 128:(i + 1) * 128], h2, h2[:, c, tsl],
                         start=(c == 0), stop=(c == 7))
                sa = sas[nsa[0] % 3]
                nsa[0] += 1
                p.actf(sa, sa[:], pa, pa[:], AF.Silu)
                if moe:
                    p.tt(GATE_ENG, sa, sa[:], sa, sa[:], gbh, gb_ap, ALU.mult)
                p.tt("dve", g, g[:, i, :], sa, sa[:], pb, pb[:], ALU.mult)

        def stage2(u):
            ci, tb = divmod(u, ntb)
            w2 = w2s[ci % 2]
            g = gs[u % 2]
            for ft in range(8):
                po = PS[4 + ft % (2 if moe else 4)]
                for i in range(nfi):
                    p.mm(po, po[:], w2, w2[:, i, ft * 128:(ft + 1) * 128], g, g[:, i, :],
                         start=(i == 0), stop=(i == nfi - 1))
                av = accv[ft][tb]
                p.tt("dve", av, av[:], av, av[:], po, po[:], ALU.add)

        for hf in range(nhalf):
            hsl = slice(hf * HTOK, (hf + 1) * HTOK)
            for c in range(8):
                p.dma("sp", acc[:, c, :], xo[:, c, hsl], reads=(XR,), writes=tuple(accv[c]))
            p.dma("sp", h2[:], hv[:, :, hsl], reads=(HT,), writes=(h2,))
            if moe:
                p.dma("sp", gtile[:], GT[:, hsl], reads=(GT,), writes=(gtile,))
            load_chunk(0)
            if nck > 1:
                load_chunk(1)
            nu = nck * ntb
            for step in range(nu + 1):
                if step < nu:
                    stage1(step)
                if step >= 1:
                    stage2(step - 1)
                    ci, tb = divmod(step, ntb)
                    if step < nu and tb == 0 and ci >= 1 and ci + 1 < nck:
                        load_chunk(ci + 1)
            for c in range(8):
                p.dma("sp", do[:, c, hsl], acc[:, c, :], reads=tuple(accv[c]), writes=(dst,))
        ph.close()

    def phase_ssm(l):
        ph = Phase()
        T = 512
        spk = ph.sb([128, SPN])
        spd = wd(f"sp{l}", [128, SPN])
        p.dma("sp", spk[:], spd[:], reads=(spd,), writes=(spk,))
        bc = {}
        for nm in ("bpr", "bpi", "cpr", "cpi"):
            d = wd(f"{nm}{l}", [128, 8, 128])
            t = ph.sb([128, 8, 128], BF16)
            cast_load(t, t[:], d, d[:])
            bc[nm] = t
        p.ts("dve", bc["cpi"], bc["cpi"][:], bc["cpi"], bc["cpi"][:], -1.0, ALU.mult)
        wgd = wd(f"wglu{l}", [256, 256])
        wg = ph.sb([128, 2, 256], BF16)
        cast_load(wg, wg[:], wgd, wgd[:].rearrange("(c p) n -> p c n", p=128))
        sm = ph.sb([128, 24, 8])
        SM = {n: i for i, n in enumerate(("dt", "a", "th", "r", "c", "s", "t0", "t1", "t2", "cr", "ci", "den",
                                           "cor", "coi", "pr", "pi", "etr", "eti", "zir", "zii", "u0", "u1"))}

        def sv(n):
            return sm[:, SM[n], :]

        lre = spk[:, 32:40]
        lim = spk[:, 40:48]
        p.actf(sm, sv("dt"), spk, spk[:, 48:56], AF.Exp)
        p.tt("dve", sm, sv("a"), sm, sv("dt"), spk, lre, ALU.mult)
        p.tt("dve", sm, sv("th"), sm, sv("dt"), spk, lim, ALU.mult)
        p.actf(sm, sv("r"), sm, sv("a"), AF.Exp)
        hp = ph.sb([128, 1])
        p.memset("dve", hp, hp[:], math.pi / 2)
        p.actf(sm, sv("s"), sm, sv("th"), AF.Sin, scale=1.0 / 32.0)
        p.actf(sm, sv("c"), sm, sv("th"), AF.Sin, scale=1.0 / 32.0, bias=hp[:], extra_reads=(hp,))

        def csquare(cn, sn):
            p.tt("dve", sm, sv("t0"), sm, sv(cn), sm, sv(cn), ALU.mult)
            p.tt("dve", sm, sv("t1"), sm, sv(sn), sm, sv(sn), ALU.mult)
            p.tt("dve", sm, sv("t2"), sm, sv(cn), sm, sv(sn), ALU.mult)
            p.tt("dve", sm, sv(cn), sm, sv("t0"), sm, sv("t1"), ALU.subtract)
            p.ts("dve", sm, sv(sn), sm, sv("t2"), 2.0, ALU.mult)

        for _ in range(5):
            csquare("c", "s")
        p.tt("dve", sm, sv("cr"), sm, sv("r"), sm, sv("c"), ALU.mult)
        p.ts("dve", sm, sv("cr"), sm, sv("cr"), -1.0, ALU.add)
        p.tt("dve", sm, sv("ci"), sm, sv("r"), sm, sv("s"), ALU.mult)
        p.tt("dve", sm, sv("t0"), spk, lre, spk, lre, ALU.mult)
        p.tt("dve", sm, sv("t1"), spk, lim, spk, lim, ALU.mult)
        p.tt("dve", sm, sv("den"), sm, sv("t0"), sm, sv("t1"), ALU.add)
        p.recip(sm, sv("den"), sm, sv("den"))
        p.tt("dve", sm, sv("t0"), sm, sv("cr"), spk, lre, ALU.mult)
        p.tt("dve", sm, sv("t1"), sm, sv("ci"), spk, lim, ALU.mult)
        p.tt("dve", sm, sv("t0"), sm, sv("t0"), sm, sv("t1"), ALU.add)
        p.tt("dve", sm, sv("cor"), sm, sv("t0"), sm, sv("den"), ALU.mult)
        p.tt("dve", sm, sv("t0"), sm, sv("ci"), spk, lre, ALU.mult)
        p.tt("dve", sm, sv("t1"), sm, sv("cr"), spk, lim, ALU.mult)
        p.tt("dve", sm, sv("t0"), sm, sv("t0"), sm, sv("t1"), ALU.subtract)
        p.tt("dve", sm, sv("coi"), sm, sv("t0"), sm, sv("den"), ALU.mult)
        Er = ph.sb([128, 8, T])
        Ei = ph.sb([128, 8, T])
        Fr = ph.sb([128, 8, T], BF16)
        Fi = ph.sb([128, 8, T], BF16)
        Rt = ph.sb([128, 8, T])
        tA = ph.sb([128, 8, T])
        tB = ph.sb([128, 8, T])
        p.memset("dve", Er, Er[:, :, 0:1], 1.0)
        p.memset("dve", Ei, Ei[:, :, 0:1], 0.0)
        p.copy("dve", sm, sv("pr"), sm, sv("c"))
        p.copy("dve", sm, sv("pi"), sm, sv("s"))
        w = 1
        while w < T:
            prb = sm[:, SM["pr"], :].rearrange("p (j o) -> p j o", o=1).broadcast_to([128, 8, w])
            pib = sm[:, SM["pi"], :].rearrange("p (j o) -> p j o", o=1).broadcast_to([128, 8, w])
            p.tt("dve", tA, tA[:, :, 0:w], Er, Er[:, :, 0:w], sm, prb, ALU.mult)
            p.tt("dve", tB, tB[:, :, 0:w], Ei, Ei[:, :, 0:w], sm, pib, ALU.mult)
            p.tt("dve", Er, Er[:, :, w:2 * w], tA, tA[:, :, 0:w], tB, tB[:, :, 0:w], ALU.subtract)
            p.tt("dve", tA, tA[:, :, 0:w], Er, Er[:, :, 0:w], sm, pib, ALU.mult)
            p.tt("dve", tB, tB[:, :, 0:w], Ei, Ei[:, :, 0:w], sm, prb, ALU.mult)
            p.tt("dve", Ei, Ei[:, :, w:2 * w], tA, tA[:, :, 0:w], tB, tB[:, :, 0:w], ALU.add)
            csquare("pr", "pi")
            w *= 2
        p.copy("dve", sm, sv("etr"), sm, sv("pr"))
        p.copy("dve", sm, sv("eti"), sm, sv("pi"))
        corb = sm[:, SM["cor"], :].rearrange("p (j o) -> p j o", o=1).broadcast_to([128, 8, T])
        coib = sm[:, SM["coi"], :].rearrange("p (j o) -> p j o", o=1).broadcast_to([128, 8, T])
        rb = sm[:, SM["r"], :].rearrange("p (j o) -> p j o", o=1).broadcast_to([128, 8, T])
        p.tt("dve", tA, tA[:], Er, Er[:], sm, corb, ALU.mult)
        p.tt("dve", tB, tB[:], Ei, Ei[:], sm, coib, ALU.mult)
        p.tt("dve", Fr, Fr[:], tA, tA[:], tB, tB[:], ALU.add)
        p.tt("dve", tA, tA[:], Er, Er[:], sm, coib, ALU.mult)
        p.tt("dve", tB, tB[:], Ei, Ei[:], sm, corb, ALU.mult)
        p.tt("dve", Fi, Fi[:], tA, tA[:], tB, tB[:], ALU.subtract)
        p.copy("dve", Rt, Rt[:], sm, rb)
        p.memset("dve", sm, sv("zir"), 0.0)
        p.memset("dve", sm, sv("zii"), 0.0)

        u32s = [ph.sb([128, 2, T]) for _ in range(2)]
        ubs = [ph.sb([128, 2, T], BF16) for _ in range(2)]
        wk = [ph.sb([128, T], BF16) for _ in range(10)]
        abs_ = [ph.sb([128, T], BF16) for _ in range(4)]
        Ecb = ph.sb([128, 8, T], BF16)
        Esb = ph.sb([128, 8, T], BF16)
        p.copy("act", Ecb, Ecb[:], Er, Er[:])
        p.copy("act", Esb, Esb[:], Ei, Ei[:])
        xrs = [ph.sb([128, T], BF16) for _ in range(2)]
        xis = [ph.sb([128, T], BF16) for _ in range(2)]
        yv = [ph.sb([128, T]) for _ in range(4)]
        gl32 = ph.sb([128, 2, T])
        glb = ph.sb([128, 2, T], BF16)
        sq = ph.sb([128, T], BF16)
        rr = ph.sb([128, T])
        outb = [ph.sb([128, T], BF16) for _ in range(2)]
        uv = UT[:].rearrange("(c p) t -> p c t", p=128)
        for tb in range(S // T):
            tsl = slice(tb * T, (tb + 1) * T)
            u32, ub = u32s[tb % 2], ubs[tb % 2]
            p.dma("sp", u32[:], uv[:, :, tsl], reads=(UT,), writes=(u32,))
            cast_load(ub, ub[:], UT, uv[:, :, tsl])
            for j in range(8):
                A, B = PS[(2 * j) % 4], PS[(2 * j + 1) % 4]
                p.mm(A, A[:], bc["bpr"], bc["bpr"][:, j, :], ub, ub[:, j // 4, :], start=True, stop=True)
                p.mm(B, B[:], bc["bpi"], bc["bpi"][:, j, :], ub, ub[:, j // 4, :], start=True, stop=True)
                t1, t2, t3, t4, wre, wim, zre, zim, t5, t6 = wk
                Ab, Bb = abs_[(2 * j) % 4], abs_[(2 * j + 1) % 4]
                p.copy("act", Ab, Ab[:], A, A[:])
                p.copy("act", Bb, Bb[:], B, B[:])
                p.tt("dve", t1, t1[:], Fr, Fr[:, j, :], Ab, Ab[:], ALU.mult)
                p.tt("dve", t2, t2[:], Fi, Fi[:, j, :], Bb, Bb[:], ALU.mult)
                p.tt("dve", wre, wre[:], t1, t1[:], t2, t2[:], ALU.subtract)
                p.tt("dve", t3, t3[:], Fr, Fr[:, j, :], Bb, Bb[:], ALU.mult)
                p.tt("dve", t4, t4[:], Fi, Fi[:, j, :], Ab, Ab[:], ALU.mult)
                p.tt("dve", wim, wim[:], t3, t3[:], t4, t4[:], ALU.add)
                p.op("dve", lambda en: en.tensor_tensor_scan(out=zre[:], data0=Rt[:, j, :], data1=wre[:],
                                                             initial=sm[:, SM["zir"], j:j + 1],
                                                             op0=ALU.mult, op1=ALU.add),
                     reads=(Rt, wre, sm), writes=(zre,))
                p.op("dve", lambda en: en.tensor_tensor_scan(out=zim[:], data0=Rt[:, j, :], data1=wim[:],
                                                             initial=sm[:, SM["zii"], j:j + 1],
                                                             op0=ALU.mult, op1=ALU.add),
                     reads=(Rt, wim, sm), writes=(zim,))
                xr, xi = xrs[j % 2], xis[j % 2]
                p.tt("dve", t1, t1[:], Ecb, Ecb[:, j, :], zre, zre[:], ALU.mult)
                p.tt("dve", t2, t2[:], Esb, Esb[:, j, :], zim, zim[:], ALU.mult)
                p.tt("dve", xr, xr[:], t1, t1[:], t2, t2[:], ALU.subtract)
                p.tt("dve", t5, t5[:], Ecb, Ecb[:, j, :], zim, zim[:], ALU.mult)
                p.tt("dve", t6, t6[:], Esb, Esb[:, j, :], zre, zre[:], ALU.mult)
                p.tt("dve", xi, xi[:], t5, t5[:], t6, t6[:], ALU.add)
                p.tt("dve", sm, sm[:, SM["u0"], j:j + 1], sm, sm[:, SM["eti"], j:j + 1], zim, zim[:, T - 1:T], ALU.mult)
                p.tt("dve", sm, sm[:, SM["u1"], j:j + 1], sm, sm[:, SM["eti"], j:j + 1], zre, zre[:, T - 1:T], ALU.mult)
                p.stt(sm, sm[:, SM["zir"], j:j + 1], zre, zre[:, T - 1:T], sm[:, SM["etr"], j:j + 1],
                      sm, sm[:, SM["u0"], j:j + 1], ALU.mult, ALU.subtract)
                p.stt(sm, sm[:, SM["zii"], j:j + 1], zim, zim[:, T - 1:T], sm[:, SM["etr"], j:j + 1],
                      sm, sm[:, SM["u1"], j:j + 1], ALU.mult, ALU.add)
                Y = PS[4 + j // 4]
                p.mm(Y, Y[:], bc["cpr"], bc["cpr"][:, j, :], xr, xr[:], start=(j % 4 == 0), stop=False)
                p.mm(Y, Y[:], bc["cpi"], bc["cpi"][:, j, :], xi, xi[:], start=False, stop=(j % 4 == 3))
            for ft in range(2):
                Y = PS[4 + ft]
                y0, y1, y2, y3 = yv
                p.stt(y0, y0[:], u32, u32[:, ft, :], spk[:, 24 + ft:25 + ft], Y, Y[:], ALU.mult, ALU.add,
                      extra_reads=(spk,))
                p.tt("dve", y1, y1[:], y0, y0[:], y0, y0[:], ALU.mult)
                p.ts("dve", y1, y1[:], y1, y1[:], 0.044715, ALU.mult, 1.0, ALU.add)
                p.tt("dve", y2, y2[:], y1, y1[:], y0, y0[:], ALU.mult)
                p.actf(y3, y3[:], y2, y2[:], AF.Sigmoid, scale=2.0 * math.sqrt(2.0 / math.pi))
                p.tt("dve", gl32, gl32[:, ft, :], y0, y0[:], y3, y3[:], ALU.mult)
                p.copy("act", glb, glb[:, ft, :], gl32, gl32[:, ft, :])
            for ft in range(2):
                G = PS[6]
                for k2 in range(2):
                    p.mm(G, G[:], wg, wg[:, k2, ft * 128:(ft + 1) * 128], glb, glb[:, k2, :],
                         start=(k2 == 0), stop=(k2 == 1))
                y0, y1, y2, y3 = yv
                p.actf(y0, y0[:], G, G[:], AF.Sigmoid, bias=spk[:, 26 + ft:27 + ft], extra_reads=(spk,))
                p.tt("dve", y1, y1[:], gl32, gl32[:, ft, :], y0, y0[:], ALU.mult)
                ob = outb[ft]
                group_norm(y1, y1[:], 128, T, "bd64", 64.0, spk[:, 16 + ft:17 + ft], spk, ob, ob[:], sq, rr, PS[7])
                p.dma("sp", MIX[ft * 128:(ft + 1) * 128, tsl], ob[:], reads=(ob,), writes=(MIX,))
        ph.close()

    def phase_sb(l):
        ph = Phase()
        spk = ph.sb([128, SPN])
        spd = wd(f"sp{l}", [128, SPN])
        p.dma("sp", spk[:], spd[:], reads=(spd,), writes=(spk,))
        qTs = [ph.sb([64, S], BF16) for _ in range(2)]
        kTs = [ph.sb([64, S], BF16) for _ in range(2)]
        nkTs = [ph.sb([64, S], BF16) for _ in range(2)]
        vvs = [ph.sb([128, NKT, 64], BF16) for _ in range(2)]
        Es = [ph.sb([128, 2, 512]) for _ in range(2)]
        SPb = [ph.sb([128, 2, 512], BF16) for _ in range(3)]
        Wb = [ph.sb([128, 2, 512], BF16) for _ in range(2)]
        SPsum = ph.sb([128, 512], BF16)
        sq = ph.sb([128, 512], BF16)
        rr = ph.sb([128, 512])
        ob = [ph.sb([64, 512], BF16) for _ in range(2)]
        iters = []
        for h in range(4):
            for qb in range(NB):
                for a in range(4 * qb + 3, 0, -2):
                    iters.append((h, qb, a))
        n = len(iters)

        def load_head(h):
            hs = slice(h * 64, (h + 1) * 64)
            qT, kT, nkT, vv = qTs[h % 2], kTs[h % 2], nkTs[h % 2], vvs[h % 2]
            p.dma("sp", qT[:], QT["sb"][hs, :], reads=(QT["sb"],), writes=(qT,))
            p.dma("sp", kT[:], KT["sb"][hs, :], reads=(KT["sb"],), writes=(kT,))
            p.dma("sp", vv[:], VV["sb"][:, hs].rearrange("(a p) d -> p a d", p=128), reads=(VV["sb"],), writes=(vv,))
            p.actf(nkT, nkT[:], kT, kT[:], AF.Copy, scale=-1.0)

        def stage1(it):
            h, qb, a = iters[it]
            if qb == 0 and a == 3 and h == 0:
                load_head(0)
            if qb == 1 and a == 7 and h + 1 < 4:
                load_head(h + 1)
            qT, kT = qTs[h % 2], kTs[h % 2]
            qsl = slice(qb * 512, (qb + 1) * 512)
            b0i = 2 * (it % 2)
            E, sp_ = Es[it % 2], SPb[it % 3]
            for t in range(2):
                A = PS[b0i + t]
                ksl = slice((a - t) * 128, (a - t + 1) * 128)
                p.mm(A, A[:], kT, kT[:, ksl], qT, qT[:, qsl], start=True, stop=True)
            p.actf(E, E[:], PS[b0i], ps2(b0i), AF.Exp, extra_reads=(PS[b0i + 1],))
            p.actf(sp_, sp_[:], E, E[:], AF.Ln, bias=1.0)
            for t in range(2):
                diag = a - t - 4 * qb
                if diag >= 0:
                    p.tt("dve", sp_, sp_[:, t, :], sp_, sp_[:, t, :], cbt, cbs(f"m{diag}"), ALU.mult)

        def stage2(it):
            h, qb, a = iters[it]
            qT, nkT = qTs[h % 2], nkTs[h % 2]
            qsl = slice(qb * 512, (qb + 1) * 512)
            sp_, W = SPb[it % 3], Wb[it % 2]
            first = (a == 4 * qb + 3)
            for t in range(2):
                Bp = PS[4 + t]
                ksl = slice((a - t) * 128, (a - t + 1) * 128)
                p.mm(Bp, Bp[:], nkT, nkT[:, ksl], qT, qT[:, qsl], start=True, stop=False)
                lastmm = first and t == 0
                p.mm(Bp, Bp[:], cbt, cbs("tincl"), sp_, sp_[:, t, :], start=False, stop=lastmm)
                if t == 1:
                    p.mm(Bp, Bp[:], cbt, cbs("ones"), sp_, sp_[:, 0, :], start=False, stop=first)
                if not first:
                    p.mm(Bp, Bp[:], cbt, cbs("ones"), SPsum, SPsum[:], start=False, stop=True)
            p.actf(W, W[:], PS[4], ps2(4), AF.Exp, scale=-1.0, extra_reads=(PS[5],))
            for t in range(2):
                diag = a - t - 4 * qb
                if diag >= 0:
                    p.tt("dve", W, W[:, t, :], W, W[:, t, :], cbt, cbs(f"m{diag}"), ALU.mult)
            if a - 1 > 0:
                if first:
                    p.tt("dve", SPsum, SPsum[:], sp_, sp_[:, 0, :], sp_, sp_[:, 1, :], ALU.add)
                else:
                    p.tt("dve", SPsum, SPsum[:], SPsum, SPsum[:], sp_, sp_[:, 0, :], ALU.add)
                    p.tt("dve", SPsum, SPsum[:], SPsum, SPsum[:], sp_, sp_[:, 1, :], ALU.add)

        def stage3(it):
            h, qb, a = iters[it]
            vv, W = vvs[h % 2], Wb[it % 2]
            O = PS[6 + qb % 2]
            for t in range(2):
                p.mm(O, O[0:64, :], vv, vv[:, a - t, :], W, W[:, t, :], start=(a == 4 * qb + 3 and t == 0),
                     stop=(a - t == 0))
            if a - 1 == 0:
                qsl = slice(qb * 512, (qb + 1) * 512)

                def finA(O=O):
                    p.actf(sq, sq[0:64, :], O, O[0:64, :], AF.Square)

                def finB(O=O, h=h, qb=qb, qsl=qsl):
                    o_ = ob[qb % 2]
                    G = PS[4]
                    p.mm(G, G[0:64, :], cbt, cbs("bd64", rows=64, c1=64), sq, sq[0:64, :], start=True, stop=True)
                    p.actf(rr, rr[0:64, :], G, G[0:64, :], AF.Ln, bias=EPS, scale=1.0 / 64.0)
                    p.actf(rr, rr[0:64, :], rr, rr[0:64, :], AF.Exp, scale=-0.5)
                    p.stt(o_, o_[:], O, O[0:64, :], spk[0:64, 188 + h:189 + h], rr, rr[0:64, :], ALU.mult, ALU.mult,
                          extra_reads=(spk,))
                    p.dma("sp", MIX[256 + h * 64:256 + (h + 1) * 64, qsl], o_[:], reads=(o_,), writes=(MIX,))

                pend.append((cur_step[0] + 1, finA))
                pend.append((cur_step[0] + 2, finB))

        pend = []
        cur_step = [0]
        step = 0
        while step < n + 2 or pend:
            cur_step[0] = step
            if step < n:
                stage1(step)
            if 0 <= step - 1 < n:
                stage2(step - 1)
            if 0 <= step - 2 < n:
                stage3(step - 2)
            due = [f for (d, f) in pend if d <= step]
            pend[:] = [(d, f) for (d, f) in pend if d > step]
            for f in due:
                f()
            step += 1
        ph.close()

    def phase_ch(l):
        ph = Phase()
        spk = ph.sb([128, SPN])
        spd = wd(f"sp{l}", [128, SPN])
        p.dma("sp", spk[:], spd[:], reads=(spd,), writes=(spk,))
        chd = wd(f"chb{l}", [128, 4, 5, 128])
        bt32 = ph.sb([128, 4, 5, 128])
        btb = ph.sb([128, 4, 5, 128], BF16)
        p.dma("sp", bt32[:], chd[:], reads=(chd,), writes=(bt32,))
        for h in range(4):
            p.tt("dve", bt32, bt32[:, h, 0, :], bt32, bt32[:, h, 0, :], cft, cfs("chm0"), ALU.add)
            p.tt("dve", bt32, bt32[:, h, 4, :], bt32, bt32[:, h, 4, :], cft, cfs("chm4"), ALU.add)
        p.copy("dve", btb, btb[:], bt32, bt32[:])
        qTs = [ph.sb([64, S], BF16) for _ in range(2)]
        kTs = [ph.sb([64, S], BF16) for _ in range(2)]
        vas = [ph.sb([128, NKT, 65], BF16) for _ in range(2)]
        for va in vas:
            p.memset("dve", va, va[:, :, 64:65], 1.0)
        Pt = [ph.sb([128, 640], BF16) for _ in range(2)]
        sq = ph.sb([65, 512], BF16)
        rr = ph.sb([64, 512])
        ob = [ph.sb([64, 512], BF16) for _ in range(2)]
        iters = [(h, qt) for h in range(4) for qt in range(NKT)]
        n = len(iters)

        def load_head(h):
            hs = slice(h * 64, (h + 1) * 64)
            p.dma("sp", qTs[h % 2][:], QT["ch"][hs, :], reads=(QT["ch"],), writes=(qTs[h % 2],))
            p.dma("sp", kTs[h % 2][:], KT["ch"][hs, :], reads=(KT["ch"],), writes=(kTs[h % 2],))
            p.dma("sp", vas[h % 2][:, :, 0:64], VV["ch"][:, hs].rearrange("(a p) d -> p a d", p=128),
                  reads=(VV["ch"],), writes=(vas[h % 2],))

        def stage1(it):
            h, qt = iters[it]
            if h == 0 and qt == 0:
                load_head(0)
            if qt == 2 and h + 1 < 4:
                load_head(h + 1)
            qT, kT = qTs[h % 2], kTs[h % 2]
            q128 = slice(qt * 128, (qt + 1) * 128)
            nd = min(4, qt) + 1
            X, Y = PS[it % 2], PS[2 + it % 2]
            for d in range(nd):
                a = qt - d
                dst_b = X if d < 4 else Y
                dst = X[:, d * 128:(d + 1) * 128] if d < 4 else Y[:, 0:128]
                p.mm(dst_b, dst, kT, kT[:, a * 128:(a + 1) * 128], qT, qT[:, q128], start=True, stop=False)
                p.mm(dst_b, dst, btb, btb[:, h, d, :], cbt, cbs("ident"), start=False, stop=True)

        def stage2(it):
            h, qt = iters[it]
            va = vas[h % 2]
            nd = min(4, qt) + 1
            X, Y = PS[it % 2], PS[2 + it % 2]
            P_ = Pt[it % 2]
            O = PS[4 + (qt // 4) % 2]
            ocol = slice((qt % 4) * 128, (qt % 4 + 1) * 128)
            n4 = min(nd, 4)
            p.actf(P_, P_[:, 0:n4 * 128], X, X[:, 0:n4 * 128], AF.Exp)
            if nd == 5:
                p.actf(P_, P_[:, 512:640], Y, Y[:, 0:128], AF.Exp)
            for d in range(nd):
                a = qt - d
                p.mm(O, O[0:65, ocol], va, va[:, a, :], P_, P_[:, d * 128:(d + 1) * 128],
                     start=(d == 0), stop=(d == nd - 1))
            if qt % 4 == 3:
                qb = qt // 4
                qsl = slice(qb * 512, (qb + 1) * 512)
                o_ = ob[qb % 2]
                p.actf(sq, sq[:], O, O[0:65, :], AF.Square, scale=cfs("sclrow", r0=0, r1=65), extra_reads=(cft,))
                SSB = PS[6]
                p.mm(SSB, SSB[0:64, :], cbt, cbs("ones", rows=65, c1=64), sq, sq[:], start=True, stop=True)
                p.actf(rr, rr[:], SSB, SSB[0:64, :], AF.Ln, scale=1.0 / 64.0)
                p.actf(rr, rr[:], rr, rr[:], AF.Exp, scale=-0.5)
                p.stt(o_, o_[:], O, O[0:64, :], spk[0:64, 192 + h:193 + h], rr, rr[:], ALU.mult, ALU.mult,
                      extra_reads=(spk,))
                p.dma("sp", MIX[512 + h * 64:512 + (h + 1) * 64, qsl], o_[:], reads=(o_,), writes=(MIX,))

        for step in range(n + 1):
            if step < n:
                stage1(step)
            if 0 <= step - 1 < n:
                stage2(step - 1)
        ph.close()

    def phase_df(l):
        ph = Phase()
        lam_init = 0.8 - 0.6 * math.exp(-0.3 * l)
        spk = ph.sb([128, SPN])
        spd = wd(f"sp{l}", [128, SPN])
        p.dma("sp", spk[:], spd[:], reads=(spd,), writes=(spk,))
        AX = mybir.AxisListType.X
        sc = ph.sb([128, 8])
        pr = ph.sb([128, 2, 32])
        p.tt("dve", pr, pr[:, 0, :], spk, spk[:, 56:88], spk, spk[:, 88:120], ALU.mult)
        p.tt("dve", pr, pr[:, 1, :], spk, spk[:, 120:152], spk, spk[:, 152:184], ALU.mult)
        p.op("dve", lambda en: en.tensor_reduce(out=sc[:, 0:2], in_=pr[:], axis=AX, op=ALU.add), reads=(pr,), writes=(sc,))
        p.actf(sc, sc[:, 2:4], sc, sc[:, 0:2], AF.Exp)
        p.tt("dve", sc, sc[:, 4:5], sc, sc[:, 3:4], sc, sc[:, 2:3], ALU.subtract)
        p.ts("dve", sc, sc[:, 5:6], sc, sc[:, 4:5], -lam_init, ALU.add)
        lrow = ph.sb([128, 64])
        p.ts("dve", lrow, lrow[:], cft, cfs("ones", c1=64), sc[:, 5:6], ALU.mult, extra_reads=(sc,))
        gdf = ph.sb([64, 4])
        p.ts("dve", gdf, gdf[:], spk, spk[0:64, 196:200], 1.0 - lam_init, ALU.mult)
        qT = ph.sb([64, S], BF16)
        kT = ph.sb([64, S], BF16)
        va = ph.sb([128, NKT, 65], BF16)
        p.memset("dve", va, va[:, :, 64:65], 1.0)
        Pt2 = [ph.sb([128, 2, 512], BF16) for _ in range(2)]
        sd2 = [ph.sb([128, 2, 128]) for _ in range(2)]
        rc = ph.sb([128, 1024])
        b0 = ph.sb([64, 512])
        b1 = ph.sb([64, 512])
        t0 = ph.sb([64, 512])
        t1 = ph.sb([64, 512])
        sq = ph.sb([64, 512], BF16)
        rr = ph.sb([64, 512])
        ob = [ph.sb([64, 512], BF16) for _ in range(2)]
        qTs = [qT, ph.sb([64, S], BF16)]
        kTs = [kT, ph.sb([64, S], BF16)]
        vas = [va, ph.sb([128, NKT, 65], BF16)]
        p.memset("dve", vas[1], vas[1][:, :, 64:65], 1.0)
        iters = []
        for h in range(4):
            for qb in range(NB):
                for a in range(4 * qb + 4):
                    iters.append((h, qb, a))
        n = len(iters)

        def load_head(h):
            hs = slice(h * 64, (h + 1) * 64)
            p.dma("sp", qTs[h % 2][:], QT["df"][hs, :], reads=(QT["df"],), writes=(qTs[h % 2],))
            p.dma("sp", kTs[h % 2][:], KT["df"][hs, :], reads=(KT["df"],), writes=(kTs[h % 2],))
            p.dma("sp", vas[h % 2][:, :, 0:64], VV["df"][:, hs].rearrange("(a p) d -> p a d", p=128),
                  reads=(VV["df"],), writes=(vas[h % 2],))

        def stage1(it):
            h, qb, a = iters[it]
            if qb == 0 and a == 0 and h == 0:
                load_head(0)
            if qb == 0 and a == 2 and h + 1 < 4:
                load_head(h + 1)
            qT_, kT_ = qTs[h % 2], kTs[h % 2]
            qsl = slice(qb * 512, (qb + 1) * 512)
            ksl = slice(a * 128, (a + 1) * 128)
            for c in range(2):
                Sc = PS[c + 2 * (it % 2)]
                p.mm(Sc, Sc[:], kT_, kT_[32 * c:32 * c + 32, ksl], qT_, qT_[32 * c:32 * c + 32, qsl],
                     start=True, stop=True)

        def stage2(it):
            h, qb, a = iters[it]
            sl = SLOPES[h]
            nsub = 2 if h == 0 else 1
            SW = 512 // nsub
            va_ = vas[h % 2]
            qsl = slice(qb * 512, (qb + 1) * 512)
            Oc = (PS[4 + 2 * (qb % 2)], PS[5 + 2 * (qb % 2)])
            amax = 4 * qb + 3
            i = a - 4 * qb
            b0i = 2 * (it % 2)
            S0, S1 = PS[b0i], PS[b0i + 1]
            S2 = ps2(b0i)
            P2 = Pt2[it % 2]
            c0 = 0
            if i >= 0:
                j = i
                r = (128 * j) // SW
                off = 128 * j - r * SW
                sdt = sd2[it % 2]
                for c in range(2):
                    Sc = PS[b0i + c]
                    p.tt("dve", sdt, sdt[:, c, :], Sc, Sc[:, 128 * j:128 * j + 128], cft, cfs(f"dfd{h}"), ALU.add)
                p.actf(P2, P2[:, :, 128 * j:128 * j + 128], sdt, sdt[:], AF.Exp, bias=bias_const(sl * off),
                       extra_reads=(bct,))
                c0 = 128 * (i + 1)
            for r in range(nsub):
                lo = max(r * SW, c0)
                hi = (r + 1) * SW
                if lo >= hi:
                    continue
                m = (qb * 512 + r * SW - 128 * a) // 128
                p.actf(P2, P2[:, :, lo:hi], S0, S2[:, :, lo:hi], AF.Exp,
                       bias=cfs(f"dfb{h}", c0=m + 3, c1=m + 4), extra_reads=(cft, S1))
            w0 = 128 * max(i, 0)
            for c in range(2):
                p.mm(Oc[c], Oc[c][0:65, w0:512], va_, va_[:, a, :], P2, P2[:, c, w0:512],
                     start=(a == 0), stop=(a == amax))
            c = 1
            if a == amax:
                rcq = rcs[qb % 2]

                def finA(Oc=Oc, rcq=rcq):
                    p.copy("act", rcq, rcq[64:65, 0:512], Oc[0], Oc[0][64:65, :])
                    p.copy("act", rcq, rcq[64:65, 512:1024], Oc[1], Oc[1][64:65, :])
                    p.recip(rcq, rcq[64:65, :], rcq, rcq[64:65, :])

                def finB(Oc=Oc, rcq=rcq, h=h, qb=qb, qsl=qsl):
                    bi = 2 * ((cur_step[0] + 1) % 2)
                    B0, B1 = PS[bi], PS[bi + 1]
                    p.mm(B0, B0[0:64, :], cft, cfs("ones", r0=64, r1=65, c1=64), rcq, rcq[64:65, 0:512],
                         start=True, stop=True)
                    p.mm(B1, B1[0:64, :], lrow, lrow[64:65, :], rcq, rcq[64:65, 512:1024], start=True, stop=True)
                    p.copy("act", b0, b0[:], B0, B0[0:64, :])
                    p.copy("act", b1, b1[:], B1, B1[0:64, :])
                    p.tt("dve", t0, t0[:], Oc[0], Oc[0][0:64, :], b0, b0[:], ALU.mult)
                    p.tt("dve", t1, t1[:], Oc[1], Oc[1][0:64, :], b1, b1[:], ALU.mult)
                    p.tt("dve", t0, t0[:], t0, t0[:], t1, t1[:], ALU.add)

                def finC(h=h, qb=qb, qsl=qsl):
                    bi = 2 * ((cur_step[0] + 1) % 2)
                    o_ = ob[qb % 2]
                    group_norm(t0, t0[:], 64, 512, "bd64", 64.0, gdf[:, h:h + 1], gdf, o_, o_[:], sq, rr, PS[bi])
                    p.dma("sp", MIX[768 + h * 64:768 + (h + 1) * 64, qsl], o_[:], reads=(o_,), writes=(MIX,))

                pend.append((cur_step[0] + 1, finA))
                pend.append((cur_step[0] + 2, finB))
                pend.append((cur_step[0] + 3, finC))

        rcs = [rc, ph.sb([128, 1024])]
        pend = []
        cur_step = [0]
        step = 0
        while step < n + 1 or pend:
            cur_step[0] = step
            if step < n:
                stage1(step)
            if 0 <= step - 1 < n:
                stage2(step - 1)
            due = [f for (d, f) in pend if d <= step]
            pend[:] = [(d, f) for (d, f) in pend if d > step]
            for f in due:
                f()
            step += 1
        ph.close()

    mixers_local = {"ssm": phase_ssm, "sb": phase_sb, "ch": phase_ch, "df": phase_df}

    mixers = mixers_local

    def run_layers():
        nl = len(layer_list)
        for li, l in enumerate(layer_list):
            xsrc = xT if (li == 0 and first_layer_from_x) else XR
            moe = (l % 2 == 1)
            moe_idx = l // 2 if moe else None
            dense_idx = l // 2 if not moe else None
            if "n1" in phases:
                phase_n1(l, xsrc)
            for m in ("ssm", "sb", "ch", "df"):
                if m in phases:
                    mixers[m](l)
            if "op" in phases:
                phase_op(l, xsrc, moe_idx)
            if "ffn" in phases:
                dst = yT if (li == nl - 1 and last_to_y) else XR
                phase_ffn(l, moe_idx, dense_idx, dst)

    return nc, p, run_layers, mixers, locals()


S_FULL = 4096
LAUNCH_GROUPS = [[0, 1, 2, 3]]


def _layer_inputs(inp, l):
    m = {f"sp{l}": pack_small(inp, l),
         f"w_in{l}": np.ascontiguousarray(inp["w_in"][l]),
         f"w_out{l}": np.ascontiguousarray(inp["w_out"][l]),
         f"wglu{l}": np.ascontiguousarray(inp["ssm_w_glu"][l]),
         f"chb{l}": pack_chb(inp, l)}
    bpr, bpi, cpr, cpi = pack_ssm_bc(inp, l)
    m.update({f"bpr{l}": bpr, f"bpi{l}": bpi, f"cpr{l}": cpr, f"cpi{l}": cpi})
    i = l // 2
    if l % 2 == 0:
        m.update({f"w1_{i}": np.ascontiguousarray(inp["ffn_w1"][i]), f"w3_{i}": np.ascontiguousarray(inp["ffn_w3"][i]),
                  f"w2_{i}": np.ascontiguousarray(inp["ffn_w2"][i])})
    else:
        m.update({f"mw1_{i}": np.ascontiguousarray(inp["moe_w1"][i]), f"mw3_{i}": np.ascontiguousarray(inp["moe_w3"][i]),
                  f"mw2_{i}": np.ascontiguousarray(inp["moe_w2"][i]), f"mr{i}": np.ascontiguousarray(inp["moe_router"][i])})
    return m


def kernel(**inputs):
    inp = {k: np.asarray(v) for k, v in inputs.items()}
    x = inp["x"].astype(np.float32, copy=False)
    B, S, _ = x.shape
    cb, cf = make_consts()
    cur = [np.ascontiguousarray(x[b].T) for b in range(B)]
    for grp in LAUNCH_GROUPS:
        nc, p, run_layers, mixers, loc = build(S, grp)
        run_layers()
        p.finish()
        used = set(loc["used_inputs"])
        shared = {"cb": cb, "cf": cf}
        for l in grp:
            shared.update(_layer_inputs(inp, l))
        shared = {k: v for k, v in shared.items() if k in used}
        in_maps = []
        for b in range(B):
            m = dict(shared)
            m["xT"] = cur[b]
            in_maps.append(m)
        res = run_bass_kernel_spmd(nc, in_maps, core_ids=list(range(B)))
        cur = [np.ascontiguousarray(res.results[b]["yT"]) for b in range(B)]
    out = np.stack([cur[b].T for b in range(B)], axis=0)
    return np.ascontiguousarray(out.astype(np.float32, copy=False))
```

```python
import math
import numpy as np
import ml_dtypes
import concourse.bass as bass
import concourse.mybir as mybir
from concourse.bass_utils import run_bass_kernel_spmd

F32 = mybir.dt.float32
BF16 = mybir.dt.bfloat16
AF = mybir.ActivationFunctionType
ALU = mybir.AluOpType

D = 1024
DEPTH = 4
NCORES = 8
EPS = 1e-6
IN_COLS = 2560
D_FF = 2816
D_FFE = 3584
NEXP = 8
GATE_ENG = "dve"
H_DF = 4
SLOPES = [2.0 ** (-8.0 * (h + 1) / H_DF) for h in range(H_DF)]


class Buf:
    __slots__ = ("h", "lw", "rd", "name")

    def __init__(self, h, name=""):
        self.h = h
        self.lw = None
        self.rd = {}
        self.name = name

    def __getitem__(self, idx):
        return self.h[idx]

    def view(self, idx):
        return Buf(self.h[idx], self.name)


class Prog:
    SEM_ROT = 30000

    def __init__(self, nc):
        self.nc = nc
        self.eng = {"pe": nc.tensor, "act": nc.scalar, "dve": nc.vector, "pool": nc.gpsimd, "sp": nc.sync}
        self.sem = {}
        self.cnt = {}
        self.nsem = 0
        for e in self.eng:
            self._newsem(e)
        self.seen = {e: {} for e in self.eng}
        self.ndsem = 16
        self.dsem = [nc.alloc_semaphore(f"dq{i}") for i in range(self.ndsem)]
        self.dcnt = [0] * self.ndsem
        self.dnext = 0
        self.ninst = 0
        self._uid = 0
        self.pending = {}

    def _newsem(self, e):
        self.nsem += 1
        self.sem[e] = self.nc.alloc_semaphore(f"s{e}{self.nsem}")
        self.cnt[e] = 0

    def uid(self, p="t"):
        self._uid += 1
        return f"{p}{self._uid}"

    def sb(self, shape, dt=F32, name=None):
        return Buf(self.nc.alloc_sbuf_tensor(name or self.uid("sb"), list(shape), dt), name or "")

    def ps(self, shape, dt=F32, name=None):
        return Buf(self.nc.alloc_psum_tensor(name or self.uid("ps"), list(shape), dt), name or "")

    def dram(self, name, shape, dt, kind="Internal"):
        return Buf(self.nc.dram_tensor(name, list(shape), dt, kind=kind).ap(), name)

    def _collect(self, reads, writes):
        t = {}

        def add(k, v):
            if t.get(k, 0) < v:
                t[k] = v

        for b in reads:
            if b.lw is not None:
                add(*b.lw)
        for b in writes:
            if b.lw is not None:
                add(*b.lw)
            for k, v in b.rd.items():
                add(k, v)
        return t

    def _wait(self, e, tickets, skip_own=False):
        own = self.sem[e]
        seen = self.seen[e]
        for s, v in tickets.items():
            if skip_own and s is own:
                continue
            if seen.get(s, 0) < v:
                self.eng[e].wait_ge(s, v)
                seen[s] = v

    def _mark(self, reads, writes, tk):
        s, v = tk
        for b in reads:
            if b.rd.get(s, 0) < v:
                b.rd[s] = v
        for b in writes:
            b.lw = tk
            b.rd = {}

    def op(self, e, fn, reads=(), writes=(), sig=True):
        self._wait(e, self._collect(reads, writes), skip_own=(e == "pe"))
        inst = fn(self.eng[e])
        self.ninst += 1
        if sig:
            if self.cnt[e] >= self.SEM_ROT:
                self._newsem(e)
            self.cnt[e] += 1
            inst.then_inc(self.sem[e], 1)
            tk = (self.sem[e], self.cnt[e])
        else:
            assert self.cnt[e] < self.SEM_ROT + 10000
            tk = (self.sem[e], self.cnt[e] + 1)
        self._mark(reads, writes, tk)
        return inst

    def dma(self, q, out_ap, in_ap, reads=(), writes=(), **kw):
        t = self._collect(reads, writes)
        k = self.dnext
        self.dnext = (self.dnext + 1) % self.ndsem
        if self.dcnt[k] > 0:
            s = self.dsem[k]
            if t.get(s, 0) < self.dcnt[k]:
                t[s] = self.dcnt[k]
        self._wait(q, t)
        inst = self.eng[q].dma_start(out=out_ap, in_=in_ap, **kw)
        self.ninst += 1
        self.dcnt[k] += 16
        inst.then_inc(self.dsem[k], 16)
        self._mark(reads, writes, (self.dsem[k], self.dcnt[k]))
        return inst

    def finish(self):
        t = {}
        for k in range(self.ndsem):
            if self.dcnt[k] > 0:
                t[self.dsem[k]] = self.dcnt[k]
        for e in ("pe", "act", "dve", "pool"):
            if self.cnt[e] > 0:
                t[self.sem[e]] = self.cnt[e]
        self._wait("sp", t)

    def mm(self, out_b, out_ap, l_b, l_ap, r_b, r_ap, start, stop, sig=None):
        if sig is None:
            sig = stop
        return self.op("pe", lambda en: en.matmul(out_ap, lhsT=l_ap, rhs=r_ap, start=start, stop=stop),
                       reads=(l_b, r_b) if start else (l_b, r_b), writes=(out_b,), sig=sig)

    def actf(self, out_b, out_ap, in_b, in_ap, func, bias=None, scale=None, extra_reads=()):
        kw = {}
        if bias is not None:
            kw["bias"] = bias
        if scale is not None:
            kw["scale"] = scale
        return self.op("act", lambda en: en.activation(out=out_ap, in_=in_ap, func=func, **kw),
                       reads=(in_b,) + tuple(extra_reads), writes=(out_b,))

    def tt(self, e, out_b, out_ap, a_b, a_ap, b_b, b_ap, op):
        return self.op(e, lambda en: en.tensor_tensor(out=out_ap, in0=a_ap, in1=b_ap, op=op),
                       reads=(a_b, b_b), writes=(out_b,))

    def ts(self, e, out_b, out_ap, a_b, a_ap, s1, op0, s2=None, op1=None, extra_reads=()):
        if op1 is None:
            return self.op(e, lambda en: en.tensor_scalar(out=out_ap, in0=a_ap, scalar1=s1, scalar2=None, op0=op0),
                           reads=(a_b,) + tuple(extra_reads), writes=(out_b,))
        return self.op(e, lambda en: en.tensor_scalar(out=out_ap, in0=a_ap, scalar1=s1, scalar2=s2, op0=op0, op1=op1),
                       reads=(a_b,) + tuple(extra_reads), writes=(out_b,))

    def stt(self, out_b, out_ap, a_b, a_ap, scalar, b_b, b_ap, op0, op1, extra_reads=()):
        return self.op("dve", lambda en: en.scalar_tensor_tensor(out=out_ap, in0=a_ap, scalar=scalar, in1=b_ap,
                                                                 op0=op0, op1=op1),
                       reads=(a_b, b_b) + tuple(extra_reads), writes=(out_b,))

    def copy(self, e, out_b, out_ap, in_b, in_ap):
        if e == "act":
            return self.op("act", lambda en: en.copy(out=out_ap, in_=in_ap), reads=(in_b,), writes=(out_b,))
        return self.op(e, lambda en: en.tensor_copy(out=out_ap, in_=in_ap), reads=(in_b,), writes=(out_b,))

    def memset(self, e, out_b, out_ap, val):
        return self.op(e, lambda en: en.memset(out_ap, val), reads=(), writes=(out_b,))

    def recip(self, out_b, out_ap, in_b, in_ap):
        return self.op("dve", lambda en: en.reciprocal(out=out_ap, in_=in_ap), reads=(in_b,), writes=(out_b,))


def _prog_patch():
    def op(self, e, fn, reads=(), writes=(), sig=True):
        self._wait(e, self._collect(reads, writes), skip_own=(e == "pe"))
        inst = fn(self.eng[e])
        self.ninst += 1
        pend = self.pending.get(e, False)
        if sig:
            if self.cnt[e] >= self.SEM_ROT and not pend:
                self._newsem(e)
            self.cnt[e] += 1
            inst.then_inc(self.sem[e], 1)
            tk = (self.sem[e], self.cnt[e])
            self.pending[e] = False
        else:
            tk = (self.sem[e], self.cnt[e] + 1)
            self.pending[e] = True
        self._mark(reads, writes, tk)
        return inst

    def barrier(self):
        t = {}
        for k in range(self.ndsem):
            if self.dcnt[k] > 0:
                t[self.dsem[k]] = self.dcnt[k]
        for e in ("pe", "act", "dve", "pool", "sp"):
            assert not self.pending.get(e, False)
            if self.cnt[e] > 0:
                t[self.sem[e]] = self.cnt[e]
        for e in ("pe", "act", "dve", "pool", "sp"):
            self._wait(e, t)

    Prog.op = op
    Prog.barrier = barrier


_prog_patch()


CB = {}
CF = {}


def _layout_consts():
    off = 0
    for name, w in (("ones", 128), ("ident", 128), ("bd64", 128), ("bd32", 128), ("tincl", 128),
                    ("m0", 512), ("m1", 512), ("m2", 512), ("m3", 512)):
        CB[name] = (off, w)
        off += w
    CB["_n"] = off
    off = 0
    for name, w in (("ident", 128), ("chm0", 128), ("chm4", 128), ("dfd0", 128), ("dfd1", 128), ("dfd2", 128),
                    ("dfd3", 128), ("dfb0", 36), ("dfb1", 36), ("dfb2", 36), ("dfb3", 36), ("sel", 8 * 128),
                    ("sclrow", 1), ("ones", 128)):
        CF[name] = (off, w)
        off += w
    CF["_n"] = off


_layout_consts()


def make_consts():
    cb = np.zeros((128, CB["_n"]), np.float32)
    cf = np.zeros((128, CF["_n"]), np.float32)
    i = np.arange(128)

    def setb(n, a):
        o, w = CB[n]
        cb[:, o:o + w] = a

    def setf(n, a):
        o, w = CF[n]
        cf[:a.shape[0], o:o + w] = a

    setb("ones", np.ones((128, 128)))
    setb("ident", np.eye(128))
    setb("bd64", (i[:, None] // 64 == i[None, :] // 64).astype(np.float32))
    setb("bd32", (i[:, None] // 32 == i[None, :] // 32).astype(np.float32))
    setb("tincl", (i[:, None] >= i[None, :]).astype(np.float32))
    q = np.arange(512)
    for m in range(4):
        setb(f"m{m}", ((128 * m + i[:, None]) < q[None, :]).astype(np.float32))
    setf("ident", np.eye(128, dtype=np.float32))
    qq = i[:, None]
    kk = i[None, :]
    setf("chm0", np.where((kk >= 64) & (qq < 64), -1e30, 0.0).astype(np.float32))
    setf("chm4", np.where((kk < 64) & (qq >= 64), -1e30, 0.0).astype(np.float32))
    kk2 = i[:, None]
    qq2 = i[None, :]
    for h in range(4):
        sl = SLOPES[h]
        t = -sl * np.abs(qq2 - kk2) + sl * qq2
        t = np.where((kk2 // 64) <= (qq2 // 64), t, -1e30)
        setf(f"dfd{h}", t.astype(np.float32))
        setf(f"dfb{h}", (sl * (i[:, None] - 128.0 * (np.arange(36)[None, :] - 3.0))).astype(np.float32))
    sel = np.zeros((128, 8, 128), np.float32)
    for e in range(8):
        sel[e, e, :] = 1.0
    setf("sel", sel.reshape(128, 8 * 128))
    scl = np.ones((128, 1), np.float32)
    scl[64, 0] = math.sqrt(64.0 * EPS)
    setf("sclrow", scl)
    setf("ones", np.ones((128, 128), np.float32))
    return cb.astype(ml_dtypes.bfloat16), cf


SP = {"g1": (0, 8), "g2": (8, 8), "go": (16, 8), "d": (24, 2), "bglu": (26, 2), "chq": (28, 1), "chk": (29, 1),
      "dfq": (30, 1), "dfk": (31, 1), "lre": (32, 8), "lim": (40, 8), "ldt": (48, 8), "dfl": (56, 128), "goh": (184, 16)}
SPN = 200


def pack_small(inp, l):
    a = np.zeros((128, SPN), np.float32)

    def fm(v):
        return np.ascontiguousarray(v.reshape(-1, 128).T)

    a[:, 0:8] = fm(inp["norm_mix_g"][l])
    a[:, 8:16] = fm(inp["norm_ffn_g"][l])
    a[:, 16:24] = fm(inp["out_norm_g"][l])
    a[:, 24:26] = fm(inp["ssm_d"][l])
    a[:, 26:28] = fm(inp["ssm_b_glu"][l])
    a[:, 28] = np.tile(inp["ch_q_norm_g"][l], 2)
    a[:, 29] = np.tile(inp["ch_k_norm_g"][l], 2)
    a[:, 30] = np.tile(inp["df_q_norm_g"][l].reshape(-1), 2)
    a[:, 31] = np.tile(inp["df_k_norm_g"][l].reshape(-1), 2)
    for nm, key in (("lre", "ssm_lam_re"), ("lim", "ssm_lam_im")):
        o = SP[nm][0]
        v = inp[key][l].reshape(8, 2, 64)
        a[:, o:o + 8] = v.transpose(1, 2, 0).reshape(128, 8)
    o = SP["ldt"][0]
    v = np.repeat(inp["ssm_log_dt"][l].reshape(8, 2, 1), 64, axis=2)
    a[:, o:o + 8] = v.transpose(1, 2, 0).reshape(128, 8)
    o = SP["dfl"][0]
    a[:, o:o + 128] = inp["df_lambda"][l].reshape(1, 128)
    a[0:64, 184:200] = inp["out_norm_g"][l].reshape(16, 64).T
    return a


def pack_ssm_bc(inp, l):
    b_re, b_im = inp["ssm_b_re"][l], inp["ssm_b_im"][l]
    c_re, c_im = inp["ssm_c_re"][l], inp["ssm_c_im"][l]
    outs = []
    for b in (b_re, b_im):
        pad = np.zeros((8, 128, 128), np.float32)
        for g in range(16):
            j, g2 = g // 2, g % 2
            k0 = (g % 8) * 16
            pad[j, k0:k0 + 16, g2 * 64:(g2 + 1) * 64] = b[g].T
        outs.append(np.ascontiguousarray(pad.transpose(1, 0, 2)))
    for c in (c_re, c_im):
        pad = np.zeros((8, 128, 128), np.float32)
        for g in range(16):
            j, g2 = g // 2, g % 2
            m0 = (g % 8) * 16
            pad[j, g2 * 64:(g2 + 1) * 64, m0:m0 + 16] = c[g].T
        outs.append(np.ascontiguousarray(pad.transpose(1, 0, 2)))
    return outs


def pack_chb(inp, l):
    rb = inp["ch_rel_bias"][l]
    q = np.arange(128)[:, None]
    k = np.arange(128)[None, :]
    out = np.zeros((128, 4, 5, 128), np.float32)
    for d in range(5):
        idx = np.clip(128 * d + q - k, -128, 128) + 128
        out[:, :, d, :] = rb[:, idx].transpose(1, 0, 2)
    return out


from contextlib import ExitStack

ALL_PHASES = ("n1", "ssm", "sb", "ch", "df", "op", "ffn")


def build(S, layer_list, phases=ALL_PHASES, io=None, first_layer_from_x=True, last_to_y=True):
    nc = bass.Bass("TRN2", target_bir_lowering=False)
    p = Prog(nc)
    NB = S // 512
    NKT = S // 128
    io = io or {}
    used_inputs = []

    def dr(name, shape, dt, kind="Internal"):
        if name in io:
            kind = "ExternalInput" if io[name] == "in" else "ExternalOutput"
        if kind == "ExternalInput":
            used_inputs.append(name)
        return p.dram(name, shape, dt, kind)

    xT = dr("xT", [1024, S], F32, "ExternalInput")
    yT = dr("yT", [1024, S], F32, "ExternalOutput")
    cb_d = dr("cb", [128, CB["_n"]], BF16, "ExternalInput")
    cf_d = dr("cf", [128, CF["_n"]], F32, "ExternalInput")
    XR = dr("XR", [1024, S], F32)
    HT = dr("HT", [1024, S], BF16)
    UT = dr("UT", [256, S], F32)
    QT = {g: dr(f"QT{g}", [256, S], BF16) for g in ("sb", "ch", "df")}
    KT = {g: dr(f"KT{g}", [256, S], BF16) for g in ("sb", "ch", "df")}
    VV = {g: dr(f"VV{g}", [S, 256], BF16) for g in ("sb", "ch", "df")}
    MIX = dr("MIX", [1024, S], BF16)
    GT = dr("GT", [8, S], F32)

    PSALL = nc.alloc_psum_tensor("psall", [128, 8, 512], F32)
    PS = [Buf(PSALL[:, i, :], f"psb{i}") for i in range(8)]

    def ps2(i):
        return PSALL[:, i:i + 2, :]
    cbt = p.sb([128, CB["_n"]], BF16, name="cbt")
    cft = p.sb([128, CF["_n"]], F32, name="cft")
    p.dma("sp", cbt[:], cb_d[:], reads=(cb_d,), writes=(cbt,))
    p.dma("sp", cft[:], cf_d[:], reads=(cf_d,), writes=(cft,))

    def cbs(name, rows=128, c0=0, c1=None):
        o, w = CB[name]
        c1 = w if c1 is None else c1
        return cbt[0:rows, o + c0:o + c1]

    def cfs(name, r0=0, r1=128, c0=0, c1=None):
        o, w = CF[name]
        c1 = w if c1 is None else c1
        return cft[r0:r1, o + c0:o + c1]

    bct = p.sb([128, 16], F32, name="bct")
    _bias_cols = {}
    for _h in range(4):
        for _o in range(4):
            _v = float(SLOPES[_h] * 128 * _o)
            p.memset("pool", bct, bct[:, _h * 4 + _o:_h * 4 + _o + 1], _v)
            _bias_cols[round(_v, 6)] = _h * 4 + _o

    def bias_const(v):
        c = _bias_cols[round(float(v), 6)]
        return bct[:, c:c + 1]

    class Phase:
        def __init__(self):
            self.stk = ExitStack()

        def sb(self, shape, dt=F32):
            h = self.stk.enter_context(nc.sbuf_tensor(p.uid("t"), list(shape), dt))
            return Buf(h)

        def close(self):
            p.barrier()
            self.stk.close()

    def ld_w(ph, name, shape_dram, pattern, sb_shape, **kw):
        d = dr(name, shape_dram, F32, "ExternalInput")
        t = ph.sb(sb_shape, BF16)
        return d, t

    wdecl = {}

    def wd(name, shape):
        if name not in wdecl:
            wdecl[name] = dr(name, shape, F32, "ExternalInput")
        return wdecl[name]

    def cast_load(dst_b, dst_ap, src_b, src_ap):
        p.dma("pool", dst_ap, src_ap, reads=(src_b,), writes=(dst_b,), max_dma_last_dim=4096)

    def rms_block(ph, xt, spk, gofs, hT, tmp_sq, tmp_r, psb, h32=None):
        p.actf(tmp_sq, tmp_sq[:], xt, xt[:], AF.Square)
        for c in range(8):
            p.mm(psb, psb[:], cbt, cbs("ones"), tmp_sq, tmp_sq[:, c, :], start=(c == 0), stop=(c == 7))
        p.actf(tmp_r, tmp_r[:], psb, psb[:], AF.Ln, bias=EPS, scale=1.0 / 1024.0)
        p.actf(tmp_r, tmp_r[:], tmp_r, tmp_r[:], AF.Exp, scale=-0.5)
        for c in range(8):
            p.stt(hT, hT[:, c, :], xt, xt[:, c, :], spk[:, gofs + c:gofs + c + 1], tmp_r, tmp_r[:],
                  ALU.mult, ALU.mult, extra_reads=(spk,))
            if h32 is not None:
                p.stt(h32, h32[:, c, :], xt, xt[:, c, :], spk[:, gofs + c:gofs + c + 1], tmp_r, tmp_r[:],
                      ALU.mult, ALU.mult, extra_reads=(spk,))

    def group_norm(src_b, src_ap, rows, N, bdname, gsz, gain_ap, gain_b, out_b, out_ap, sq, rr, psb, gscale=None):
        p.actf(sq, sq[0:rows, 0:N], src_b, src_ap, AF.Square)
        p.mm(psb, psb[0:rows, 0:N], cbt, cbs(bdname, rows=rows, c1=rows), sq, sq[0:rows, 0:N], start=True, stop=True)
        p.actf(rr, rr[0:rows, 0:N], psb, psb[0:rows, 0:N], AF.Ln, bias=EPS, scale=1.0 / gsz)
        p.actf(rr, rr[0:rows, 0:N], rr, rr[0:rows, 0:N], AF.Exp, scale=-0.5)
        p.stt(out_b, out_ap, src_b, src_ap, gain_ap, rr, rr[0:rows, 0:N], ALU.mult, ALU.mult, extra_reads=(gain_b,))

    def phase_n1(l, xsrc):
        ph = Phase()
        spk = ph.sb([128, SPN])
        spd = wd(f"sp{l}", [128, SPN])
        p.dma("sp", spk[:], spd[:], reads=(spd,), writes=(spk,))
        wind = wd(f"w_in{l}", [1024, IN_COLS])
        win = ph.sb([128, 8, IN_COLS], BF16)
        wv = wind[:].rearrange("(c p) n -> p c n", p=128)
        for c in range(8):
            cast_load(win, win[:, c, :], wind, wv[:, c, :])
        gq = ph.sb([128, 4])
        p.ts("dve", gq, gq[:, 0:1], spk, spk[:, 28:29], 0.125, ALU.mult)
        p.copy("dve", gq, gq[:, 1:2], spk, spk[:, 29:30])
        p.ts("dve", gq, gq[:, 2:3], spk, spk[:, 30:31], 32.0 ** -0.5, ALU.mult)
        p.copy("dve", gq, gq[:, 3:4], spk, spk[:, 31:32])
        xts = [ph.sb([128, 8, 512]) for _ in range(3)]
        sqs = [ph.sb([128, 8, 512], BF16) for _ in range(2)]
        hTs = [ph.sb([128, 8, 512], BF16) for _ in range(2)]
        rrs = [ph.sb([128, 512]) for _ in range(2)]
        evs = [ph.sb([128, 512], BF16) for _ in range(4)]
        evf = [ph.sb([128, 512]) for _ in range(2)]
        sq2 = [ph.sb([128, 512], BF16) for _ in range(2)]
        rr2 = [ph.sb([128, 512]) for _ in range(2)]
        vts = [ph.sb([128, 768], BF16) for _ in range(2)]
        xv = xsrc[:].rearrange("(c p) t -> p c t", p=128)
        cnt = {"ev": 0, "ps": 0, "nm": 0}
        fm = [("u", 0, 0), ("u", 128, 1), ("qsb", 256, 0), ("qsb", 384, 1), ("ksb", 512, 0), ("ksb", 640, 1),
              ("qch", 1024, 0), ("qch", 1152, 1), ("kch", 1280, 0), ("kch", 1408, 1),
              ("qdf", 1792, 0), ("qdf", 1920, 1), ("kdf", 2048, 0), ("kdf", 2176, 1)]

        def stageL(tb):
            xt = xts[tb % 3]
            p.dma("sp", xt[:], xv[:, :, tb * 512:(tb + 1) * 512], reads=(xsrc,), writes=(xt,))

        def stageA(tb):
            rms_block(ph, xts[tb % 3], spk, SP["g1"][0], hTs[tb % 2], sqs[tb % 2], rrs[tb % 2], PS[7])

        def stageB(tb):
            hT = hTs[tb % 2]
            tsl = slice(tb * 512, (tb + 1) * 512)
            pending = [None]

            def finish_norm():
                if pending[0] is None:
                    return
                kind, rows, psb, ev, sq_, rr_, pn = pending[0]
                pending[0] = None
                gi = {"qch": 0, "kch": 1, "qdf": 2, "kdf": 3}[kind]
                ch = kind.endswith("ch")
                p.mm(pn, pn[:], cbt, cbs("bd64" if ch else "bd32"), sq_, sq_[:], start=True, stop=True)
                p.actf(rr_, rr_[:], pn, pn[:], AF.Ln, bias=EPS, scale=1.0 / (64.0 if ch else 32.0))
                p.actf(rr_, rr_[:], rr_, rr_[:], AF.Exp, scale=-0.5)
                p.stt(ev, ev[:], psb, psb[:], gq[:, gi:gi + 1], rr_, rr_[:], ALU.mult, ALU.mult, extra_reads=(gq,))
                dst = (QT if kind[0] == "q" else KT)["ch" if ch else "df"]
                p.dma("sp", dst[rows, tsl], ev[:], reads=(ev,), writes=(dst,))

            for kind, col, half in fm:
                psb = PS[cnt["ps"] % 4]
                cnt["ps"] += 1
                for c in range(8):
                    p.mm(psb, psb[:], win, win[:, c, col:col + 128], hT, hT[:, c, :], start=(c == 0), stop=(c == 7))
                finish_norm()
                rows = slice(half * 128, (half + 1) * 128)
                if kind == "u":
                    ev = evf[cnt["ev"] % 2]
                    cnt["ev"] += 1
                    p.copy("act", ev, ev[:], psb, psb[:])
                    p.dma("sp", UT[rows, tsl], ev[:], reads=(ev,), writes=(UT,))
                elif kind == "qsb":
                    ev = evs[cnt["ev"] % 4]
                    cnt["ev"] += 1
                    p.actf(ev, ev[:], psb, psb[:], AF.Copy, scale=0.125)
                    p.dma("sp", QT["sb"][rows, tsl], ev[:], reads=(ev,), writes=(QT["sb"],))
                elif kind == "ksb":
                    ev = evs[cnt["ev"] % 4]
                    cnt["ev"] += 1
                    p.copy("act", ev, ev[:], psb, psb[:])
                    p.dma("sp", KT["sb"][rows, tsl], ev[:], reads=(ev,), writes=(KT["sb"],))
                else:
                    ev = evs[cnt["ev"] % 4]
                    cnt["ev"] += 1
                    k2 = cnt["nm"] % 2
                    cnt["nm"] += 1
                    p.actf(sq2[k2], sq2[k2][:], psb, psb[:], AF.Square)
                    pending[0] = (kind, rows, psb, ev, sq2[k2], rr2[k2], PS[4 + k2])
            for tt_ in range(4):
                vt = vts[tt_ % 2]
                tok = slice(tt_ * 128, (tt_ + 1) * 128)
                for gi, (g, col) in enumerate((("sb", 768), ("ch", 1536), ("df", 2304))):
                    psb = PS[cnt["ps"] % 4]
                    cnt["ps"] += 1
                    for c in range(8):
                        p.mm(psb, psb[:, 0:256], hT, hT[:, c, tok], win, win[:, c, col:col + 256],
                             start=(c == 0), stop=(c == 7))
                    if tt_ == 0 and gi == 0:
                        finish_norm()
                    p.copy("act" if gi != 1 else "dve", vt, vt[:, gi * 256:(gi + 1) * 256], psb, psb[:, 0:256])
                t0 = tb * 512 + tt_ * 128
                for gi, g in enumerate(("sb", "ch", "df")):
                    p.dma("sp", VV[g][t0:t0 + 128, :], vt[:, gi * 256:(gi + 1) * 256], reads=(vt,), writes=(VV[g],))

        stageL(0)
        if NB > 1:
            stageL(1)
        stageA(0)
        for tb in range(NB):
            if tb + 2 < NB:
                stageL(tb + 2)
            if tb + 1 < NB:
                stageA(tb + 1)
            stageB(tb)
        ph.close()

    def phase_op(l, xsrc, moe_idx):
        ph = Phase()
        spk = ph.sb([128, SPN])
        spd = wd(f"sp{l}", [128, SPN])
        p.dma("sp", spk[:], spd[:], reads=(spd,), writes=(spk,))
        wod = wd(f"w_out{l}", [1024, 1024])
        wo = ph.sb([128, 8, 1024], BF16)
        wv = wod[:].rearrange("(c p) n -> p c n", p=128)
        for c in range(8):
            cast_load(wo, wo[:, c, :], wod, wv[:, c, :])
        if moe_idx is not None:
            wrd = wd(f"mr{moe_idx}", [1024, 8])
            wr = ph.sb([128, 8, 8])
            p.dma("sp", wr[:], wrd[:].rearrange("(c p) e -> p c e", p=128), reads=(wrd,), writes=(wr,))
            h32s = [ph.sb([128, 8, 512]) for _ in range(2)]
            lg = ph.sb([128, 4, 8])
            wk = [ph.sb([128, 4, 8]) for _ in range(4)]
            mx = [ph.sb([128, 4, 1]) for _ in range(3)]
            gts = ph.sb([8, 512])
        xts = [ph.sb([128, 8, 512]) for _ in range(2)]
        xns = [ph.sb([128, 8, 512]) for _ in range(2)]
        mxs = [ph.sb([128, 8, 512], BF16) for _ in range(2)]
        sqs = [ph.sb([128, 8, 512], BF16) for _ in range(2)]
        hTs = [ph.sb([128, 8, 512], BF16) for _ in range(2)]
        rrs = [ph.sb([128, 512]) for _ in range(2)]
        xv = xsrc[:].rearrange("(c p) t -> p c t", p=128)
        xo = XR[:].rearrange("(c p) t -> p c t", p=128)
        mv = MIX[:].rearrange("(c p) t -> p c t", p=128)
        hv = HT[:].rearrange("(c p) t -> p c t", p=128)
        cnt = {"ps": 0}

        def stageL(tb):
            tsl = slice(tb * 512, (tb + 1) * 512)
            xt, mt = xts[tb % 2], mxs[tb % 2]
            p.dma("sp", xt[:], xv[:, :, tsl], reads=(xsrc,), writes=(xt,))
            p.dma("sp", mt[:], mv[:, :, tsl], reads=(MIX,), writes=(mt,))

        def stageA(tb):
            xt, xn, mt, sq = xts[tb % 2], xns[tb % 2], mxs[tb % 2], sqs[tb % 2]
            tsl = slice(tb * 512, (tb + 1) * 512)
            for ft in range(8):
                psb = PS[cnt["ps"] % 4]
                cnt["ps"] += 1
                for c in range(8):
                    p.mm(psb, psb[:], wo, wo[:, c, ft * 128:(ft + 1) * 128], mt, mt[:, c, :],
                         start=(c == 0), stop=(c == 7))
                p.tt("dve", xn, xn[:, ft, :], xt, xt[:, ft, :], psb, psb[:], ALU.add)
            p.dma("sp", xo[:, :, tsl], xn[:], reads=(xn,), writes=(XR,))
            p.actf(sq, sq[:], xn, xn[:], AF.Square)

        def stageB(tb):
            xn, sq, hT, rr = xns[tb % 2], sqs[tb % 2], hTs[tb % 2], rrs[tb % 2]
            tsl = slice(tb * 512, (tb + 1) * 512)
            h32 = h32s[tb % 2] if moe_idx is not None else None
            psb = PS[7]
            gofs = SP["g2"][0]
            for c in range(8):
                p.mm(psb, psb[:], cbt, cbs("ones"), sq, sq[:, c, :], start=(c == 0), stop=(c == 7))
            p.actf(rr, rr[:], psb, psb[:], AF.Ln, bias=EPS, scale=1.0 / 1024.0)
            p.actf(rr, rr[:], rr, rr[:], AF.Exp, scale=-0.5)
            for c in range(8):
                p.stt(hT, hT[:, c, :], xn, xn[:, c, :], spk[:, gofs + c:gofs + c + 1], rr, rr[:],
                      ALU.mult, ALU.mult, extra_reads=(spk,))
                if h32 is not None:
                    p.stt(h32, h32[:, c, :], xn, xn[:, c, :], spk[:, gofs + c:gofs + c + 1], rr, rr[:],
                          ALU.mult, ALU.mult, extra_reads=(spk,))
            p.dma("sp", hv[:, :, tsl], hT[:], reads=(hT,), writes=(HT,))
            if moe_idx is not None:
                pl = PS[6]
                for tt_ in range(4):
                    for c in range(8):
                        p.mm(pl, pl[:, tt_ * 8:(tt_ + 1) * 8], h32, h32[:, c, tt_ * 128:(tt_ + 1) * 128],
                             wr, wr[:, c, :], start=(c == 0), stop=(c == 7))
                p.copy("dve", lg, lg[:], pl, pl[:, 0:32].rearrange("p (a e) -> p a e", e=8))
                m1, m2, ssum = mx
                AX = mybir.AxisListType.X
                p.op("dve", lambda en: en.tensor_reduce(out=m1[:], in_=lg[:], axis=AX, op=ALU.max),
                     reads=(lg,), writes=(m1,))
                p.tt("dve", wk[0], wk[0][:], lg, lg[:], m1, m1[:].broadcast_to([128, 4, 8]), ALU.is_equal)
                p.stt(wk[1], wk[1][:].rearrange("p a e -> p (a e)"), wk[0], wk[0][:].rearrange("p a e -> p (a e)"),
                      -1e30, lg, lg[:].rearrange("p a e -> p (a e)"), ALU.mult, ALU.add)
                p.op("dve", lambda en: en.tensor_reduce(out=m2[:], in_=wk[1][:], axis=AX, op=ALU.max),
                     reads=(wk[1],), writes=(m2,))
                p.tt("dve", wk[0], wk[0][:], lg, lg[:], m2, m2[:].broadcast_to([128, 4, 8]), ALU.is_ge)
                p.tt("dve", wk[1], wk[1][:], lg, lg[:], m1, m1[:].broadcast_to([128, 4, 8]), ALU.subtract)
                p.actf(wk[2], wk[2][:], wk[1], wk[1][:], AF.Exp)
                p.tt("dve", wk[2], wk[2][:], wk[2], wk[2][:], wk[0], wk[0][:], ALU.mult)
                p.op("dve", lambda en: en.tensor_reduce(out=ssum[:], in_=wk[2][:], axis=AX, op=ALU.add),
                     reads=(wk[2],), writes=(ssum,))
                p.recip(ssum, ssum[:], ssum, ssum[:])
                p.tt("dve", wk[3], wk[3][:], wk[2], wk[2][:], ssum, ssum[:].broadcast_to([128, 4, 8]), ALU.mult)
                pt = PS[5]
                for tt_ in range(4):
                    p.op("pe", lambda en: en.transpose(pt[0:8, tt_ * 128:(tt_ + 1) * 128], wk[3][:, tt_, :],
                                                       cfs("ident")),
                         reads=(wk[3], cft), writes=(pt,), sig=(tt_ == 3))
                p.copy("act", gts, gts[:], pt, pt[0:8, :])
                p.dma("sp", GT[:, tsl], gts[:], reads=(gts,), writes=(GT,))

        stageL(0)
        if NB > 1:
            stageL(1)
        stageA(0)
        for tb in range(NB):
            if tb + 1 < NB:
                stageA(tb + 1)
            if tb + 2 < NB:
                stageL(tb + 2)
            stageB(tb)
        ph.close()

    def phase_ffn(l, moe_idx, dense_idx, dst):
        ph = Phase()
        moe = moe_idx is not None
        FC = 512 if moe else 256
        nfi = FC // 128
        dff = D_FFE if moe else D_FF
        nchunk = dff // FC
        nexp = NEXP if moe else 1
        if moe:
            w1d = wd(f"mw1_{moe_idx}", [NEXP, 1024, D_FFE])
            w3d = wd(f"mw3_{moe_idx}", [NEXP, 1024, D_FFE])
            w2d = wd(f"mw2_{moe_idx}", [NEXP, D_FFE, 1024])
        else:
            w1d = wd(f"w1_{dense_idx}", [1024, D_FF])
            w3d = wd(f"w3_{dense_idx}", [1024, D_FF])
            w2d = wd(f"w2_{dense_idx}", [D_FF, 1024])
        HTOK = min(2048, S)
        nhalf = S // HTOK
        ntb = HTOK // 512
        acc = ph.sb([128, 8, HTOK])
        h2 = ph.sb([128, 8, HTOK], BF16)
        w1s = [ph.sb([128, 8, FC], BF16) for _ in range(2)]
        w3s = [ph.sb([128, 8, FC], BF16) for _ in range(2)]
        w2s = [ph.sb([128, nfi, 1024], BF16) for _ in range(2)]
        sas = [ph.sb([128, 512]) for _ in range(3)]
        gs = [ph.sb([128, nfi, 512], BF16) for _ in range(2)]
        if moe:
            gtile = ph.sb([8, HTOK])
            gbhs = [ph.sb([128, ntb, 512]) for _ in range(2)]
        xo = XR[:].rearrange("(c p) t -> p c t", p=128)
        do = dst[:].rearrange("(c p) t -> p c t", p=128)
        hv = HT[:].rearrange("(c p) t -> p c t", p=128)
        accv = [[acc.view((slice(None), ft, slice(tb * 512, (tb + 1) * 512))) for tb in range(ntb)] for ft in range(8)]
        chunks = [(e, fc) for e in range(nexp) for fc in range(nchunk)]
        nck = len(chunks)

        def load_chunk(ci):
            e, fc = chunks[ci]
            w1, w3, w2 = w1s[ci % 2], w3s[ci % 2], w2s[ci % 2]
            fsl = slice(fc * FC, (fc + 1) * FC)
            if moe:
                s1 = w1d[e].rearrange("(c p) n -> p c n", p=128)
                s3 = w3d[e].rearrange("(c p) n -> p c n", p=128)
                s2 = w2d[e, fsl, :].rearrange("(i p) n -> p i n", p=128)
            else:
                s1 = w1d[:].rearrange("(c p) n -> p c n", p=128)
                s3 = w3d[:].rearrange("(c p) n -> p c n", p=128)
                s2 = w2d[fsl, :].rearrange("(i p) n -> p i n", p=128)
            cast_load(w1, w1[:], w1d, s1[:, :, fsl])
            cast_load(w3, w3[:], w3d, s3[:, :, fsl])
            cast_load(w2, w2[:], w2d, s2)

        nsa = [0]

        def stage1(u):
            ci, tb = divmod(u, ntb)
            e, fc = chunks[ci]
            w1, w3 = w1s[ci % 2], w3s[ci % 2]
            tsl = slice(tb * 512, (tb + 1) * 512)
            g = gs[u % 2]
            if moe:
                gbh = gbhs[e % 2]
                if fc == 0 and tb == 0:
                    for tb2 in range(ntb):
                        pg = PS[6 + tb2 % 2]
                        p.mm(pg, pg[:], cft, cfs("sel", r0=0, r1=8, c0=e * 128, c1=(e + 1) * 128),
                             gtile, gtile[:, tb2 * 512:(tb2 + 1) * 512], start=True, stop=True)
                        p.copy("act", gbh, gbh[:, tb2, :], pg, pg[:])
                gb_ap = gbh[:, tb, :]
            for i in range(nfi):
                pa, pb = PS[(2 * i) % 4], PS[(2 * i + 1) % 4]
                for c in range(8):
                    p.mm(pa, pa[:], w1, w1[:, c, i * 128:(i + 1) * 128], h2, h2[:, c, tsl],
                         start=(c == 0), stop=(c == 7))
                for c in range(8):
                    p.mm(pb, pb[:], w3, w3[:, c, i * 128:(i + 1) * 128], h2, h2[:, c, tsl],
                         start=(c == 0), stop=(c == 7))
                sa = sas[nsa[0] % 3]
                nsa[0] += 1
                p.actf(sa, sa[:], pa, pa[:], AF.Silu)
                if moe:
                    p.tt(GATE_ENG, sa, sa[:], sa, sa[:], gbh, gb_ap, ALU.mult)
                p.tt("dve", g, g[:, i, :], sa, sa[:], pb, pb[:], ALU.mult)

        def stage2(u):
            ci, tb = divmod(u, ntb)
            w2 = w2s[ci % 2]
            g = gs[u % 2]
            for ft in range(8):
                po = PS[4 + ft % (2 if moe else 4)]
                for i in range(nfi):
                    p.mm(po, po[:], w2, w2[:, i, ft * 128:(ft + 1) * 128], g, g[:, i, :],
                         start=(i == 0), stop=(i == nfi - 1))
                av = accv[ft][tb]
                p.tt("dve", av, av[:], av, av[:], po, po[:], ALU.add)

        for hf in range(nhalf):
            hsl = slice(hf * HTOK, (hf + 1) * HTOK)
            for c in range(8):
                p.dma("sp", acc[:, c, :], xo[:, c, hsl], reads=(XR,), writes=tuple(accv[c]))
            p.dma("sp", h2[:], hv[:, :, hsl], reads=(HT,), writes=(h2,))
            if moe:
                p.dma("sp", gtile[:], GT[:, hsl], reads=(GT,), writes=(gtile,))
            load_chunk(0)
            if nck > 1:
                load_chunk(1)
            nu = nck * ntb
            for step in range(nu + 1):
                if step < nu:
                    stage1(step)
                if step >= 1:
                    stage2(step - 1)
                    ci, tb = divmod(step, ntb)
                    if step < nu and tb == 0 and ci >= 1 and ci + 1 < nck:
                        load_chunk(ci + 1)
            for c in range(8):
                p.dma("sp", do[:, c, hsl], acc[:, c, :], reads=tuple(accv[c]), writes=(dst,))
        ph.close()

    def phase_ssm(l):
        ph = Phase()
        T = 512
        spk = ph.sb([128, SPN])
        spd = wd(f"sp{l}", [128, SPN])
        p.dma("sp", spk[:], spd[:], reads=(spd,), writes=(spk,))
        bc = {}
        for nm in ("bpr", "bpi", "cpr", "cpi"):
            d = wd(f"{nm}{l}", [128, 8, 128])
            t = ph.sb([128, 8, 128], BF16)
            cast_load(t, t[:], d, d[:])
            bc[nm] = t
        p.ts("dve", bc["cpi"], bc["cpi"][:], bc["cpi"], bc["cpi"][:], -1.0, ALU.mult)
        wgd = wd(f"wglu{l}", [256, 256])
        wg = ph.sb([128, 2, 256], BF16)
        cast_load(wg, wg[:], wgd, wgd[:].rearrange("(c p) n -> p c n", p=128))
        sm = ph.sb([128, 24, 8])
        SM = {n: i for i, n in enumerate(("dt", "a", "th", "r", "c", "s", "t0", "t1", "t2", "cr", "ci", "den",
                                           "cor", "coi", "pr", "pi", "etr", "eti", "zir", "zii", "u0", "u1"))}

        def sv(n):
            return sm[:, SM[n], :]

        lre = spk[:, 32:40]
        lim = spk[:, 40:48]
        p.actf(sm, sv("dt"), spk, spk[:, 48:56], AF.Exp)
        p.tt("dve", sm, sv("a"), sm, sv("dt"), spk, lre, ALU.mult)
        p.tt("dve", sm, sv("th"), sm, sv("dt"), spk, lim, ALU.mult)
        p.actf(sm, sv("r"), sm, sv("a"), AF.Exp)
        hp = ph.sb([128, 1])
        p.memset("dve", hp, hp[:], math.pi / 2)
        p.actf(sm, sv("s"), sm, sv("th"), AF.Sin, scale=1.0 / 32.0)
        p.actf(sm, sv("c"), sm, sv("th"), AF.Sin, scale=1.0 / 32.0, bias=hp[:], extra_reads=(hp,))

        def csquare(cn, sn):
            p.tt("dve", sm, sv("t0"), sm, sv(cn), sm, sv(cn), ALU.mult)
            p.tt("dve", sm, sv("t1"), sm, sv(sn), sm, sv(sn), ALU.mult)
            p.tt("dve", sm, sv("t2"), sm, sv(cn), sm, sv(sn), ALU.mult)
            p.tt("dve", sm, sv(cn), sm, sv("t0"), sm, sv("t1"), ALU.subtract)
            p.ts("dve", sm, sv(sn), sm, sv("t2"), 2.0, ALU.mult)

        for _ in range(5):
            csquare("c", "s")
        p.tt("dve", sm, sv("cr"), sm, sv("r"), sm, sv("c"), ALU.mult)
        p.ts("dve", sm, sv("cr"), sm, sv("cr"), -1.0, ALU.add)
        p.tt("dve", sm, sv("ci"), sm, sv("r"), sm, sv("s"), ALU.mult)
        p.tt("dve", sm, sv("t0"), spk, lre, spk, lre, ALU.mult)
        p.tt("dve", sm, sv("t1"), spk, lim, spk, lim, ALU.mult)
        p.tt("dve", sm, sv("den"), sm, sv("t0"), sm, sv("t1"), ALU.add)
        p.recip(sm, sv("den"), sm, sv("den"))
        p.tt("dve", sm, sv("t0"), sm, sv("cr"), spk, lre, ALU.mult)
        p.tt("dve", sm, sv("t1"), sm, sv("ci"), spk, lim, ALU.mult)
        p.tt("dve", sm, sv("t0"), sm, sv("t0"), sm, sv("t1"), ALU.add)
        p.tt("dve", sm, sv("cor"), sm, sv("t0"), sm, sv("den"), ALU.mult)
        p.tt("dve", sm, sv("t0"), sm, sv("ci"), spk, lre, ALU.mult)
        p.tt("dve", sm, sv("t1"), sm, sv("cr"), spk, lim, ALU.mult)
        p.tt("dve", sm, sv("t0"), sm, sv("t0"), sm, sv("t1"), ALU.subtract)
        p.tt("dve", sm, sv("coi"), sm, sv("t0"), sm, sv("den"), ALU.mult)
        Er = ph.sb([128, 8, T])
        Ei = ph.sb([128, 8, T])
        Fr = ph.sb([128, 8, T], BF16)
        Fi = ph.sb([128, 8, T], BF16)
        Rt = ph.sb([128, 8, T])
        tA = ph.sb([128, 8, T])
        tB = ph.sb([128, 8, T])
        p.memset("dve", Er, Er[:, :, 0:1], 1.0)
        p.memset("dve", Ei, Ei[:, :, 0:1], 0.0)
        p.copy("dve", sm, sv("pr"), sm, sv("c"))
        p.copy("dve", sm, sv("pi"), sm, sv("s"))
        w = 1
        while w < T:
            prb = sm[:, SM["pr"], :].rearrange("p (j o) -> p j o", o=1).broadcast_to([128, 8, w])
            pib = sm[:, SM["pi"], :].rearrange("p (j o) -> p j o", o=1).broadcast_to([128, 8, w])
            p.tt("dve", tA, tA[:, :, 0:w], Er, Er[:, :, 0:w], sm, prb, ALU.mult)
            p.tt("dve", tB, tB[:, :, 0:w], Ei, Ei[:, :, 0:w], sm, pib, ALU.mult)
            p.tt("dve", Er, Er[:, :, w:2 * w], tA, tA[:, :, 0:w], tB, tB[:, :, 0:w], ALU.subtract)
            p.tt("dve", tA, tA[:, :, 0:w], Er, Er[:, :, 0:w], sm, pib, ALU.mult)
            p.tt("dve", tB, tB[:, :, 0:w], Ei, Ei[:, :, 0:w], sm, prb, ALU.mult)
            p.tt("dve", Ei, Ei[:, :, w:2 * w], tA, tA[:, :, 0:w], tB, tB[:, :, 0:w], ALU.add)
            csquare("pr", "pi")
            w *= 2
        p.copy("dve", sm, sv("etr"), sm, sv("pr"))
        p.copy("dve", sm, sv("eti"), sm, sv("pi"))
        corb = sm[:, SM["cor"], :].rearrange("p (j o) -> p j o", o=1).broadcast_to([128, 8, T])
        coib = sm[:, SM["coi"], :].rearrange("p (j o) -> p j o", o=1).broadcast_to([128, 8, T])
        rb = sm[:, SM["r"], :].rearrange("p (j o) -> p j o", o=1).broadcast_to([128, 8, T])
        p.tt("dve", tA, tA[:], Er, Er[:], sm, corb, ALU.mult)
        p.tt("dve", tB, tB[:], Ei, Ei[:], sm, coib, ALU.mult)
        p.tt("dve", Fr, Fr[:], tA, tA[:], tB, tB[:], ALU.add)
        p.tt("dve", tA, tA[:], Er, Er[:], sm, coib, ALU.mult)
        p.tt("dve", tB, tB[:], Ei, Ei[:], sm, corb, ALU.mult)
        p.tt("dve", Fi, Fi[:], tA, tA[:], tB, tB[:], ALU.subtract)
        p.copy("dve", Rt, Rt[:], sm, rb)
        p.memset("dve", sm, sv("zir"), 0.0)
        p.memset("dve", sm, sv("zii"), 0.0)

        u32s = [ph.sb([128, 2, T]) for _ in range(2)]
        ubs = [ph.sb([128, 2, T], BF16) for _ in range(2)]
        wk = [ph.sb([128, T], BF16) for _ in range(10)]
        abs_ = [ph.sb([128, T], BF16) for _ in range(4)]
        Ecb = ph.sb([128, 8, T], BF16)
        Esb = ph.sb([128, 8, T], BF16)
        p.copy("act", Ecb, Ecb[:], Er, Er[:])
        p.copy("act", Esb, Esb[:], Ei, Ei[:])
        xrs = [ph.sb([128, T], BF16) for _ in range(2)]
        xis = [ph.sb([128, T], BF16) for _ in range(2)]
        yv = [ph.sb([128, T]) for _ in range(4)]
        gl32 = ph.sb([128, 2, T])
        glb = ph.sb([128, 2, T], BF16)
        sq = ph.sb([128, T], BF16)
        rr = ph.sb([128, T])
        outb = [ph.sb([128, T], BF16) for _ in range(2)]
        uv = UT[:].rearrange("(c p) t -> p c t", p=128)
        for tb in range(S // T):
            tsl = slice(tb * T, (tb + 1) * T)
            u32, ub = u32s[tb % 2], ubs[tb % 2]
            p.dma("sp", u32[:], uv[:, :, tsl], reads=(UT,), writes=(u32,))
            cast_load(ub, ub[:], UT, uv[:, :, tsl])
            for j in range(8):
                A, B = PS[(2 * j) % 4], PS[(2 * j + 1) % 4]
                p.mm(A, A[:], bc["bpr"], bc["bpr"][:, j, :], ub, ub[:, j // 4, :], start=True, stop=True)
                p.mm(B, B[:], bc["bpi"], bc["bpi"][:, j, :], ub, ub[:, j // 4, :], start=True, stop=True)
                t1, t2, t3, t4, wre, wim, zre, zim, t5, t6 = wk
                Ab, Bb = abs_[(2 * j) % 4], abs_[(2 * j + 1) % 4]
                p.copy("act", Ab, Ab[:], A, A[:])
                p.copy("act", Bb, Bb[:], B, B[:])
                p.tt("dve", t1, t1[:], Fr, Fr[:, j, :], Ab, Ab[:], ALU.mult)
                p.tt("dve", t2, t2[:], Fi, Fi[:, j, :], Bb, Bb[:], ALU.mult)
                p.tt("dve", wre, wre[:], t1, t1[:], t2, t2[:], ALU.subtract)
                p.tt("dve", t3, t3[:], Fr, Fr[:, j, :], Bb, Bb[:], ALU.mult)
                p.tt("dve", t4, t4[:], Fi, Fi[:, j, :], Ab, Ab[:], ALU.mult)
                p.tt("dve", wim, wim[:], t3, t3[:], t4, t4[:], ALU.add)
                p.op("dve", lambda en: en.tensor_tensor_scan(out=zre[:], data0=Rt[:, j, :], data1=wre[:],
                                                             initial=sm[:, SM["zir"], j:j + 1],
                                                             op0=ALU.mult, op1=ALU.add),
                     reads=(Rt, wre, sm), writes=(zre,))
                p.op("dve", lambda en: en.tensor_tensor_scan(out=zim[:], data0=Rt[:, j, :], data1=wim[:],
                                                             initial=sm[:, SM["zii"], j:j + 1],
                                                             op0=ALU.mult, op1=ALU.add),
                     reads=(Rt, wim, sm), writes=(zim,))
                xr, xi = xrs[j % 2], xis[j % 2]
                p.tt("dve", t1, t1[:], Ecb, Ecb[:, j, :], zre, zre[:], ALU.mult)
                p.tt("dve", t2, t2[:], Esb, Esb[:, j, :], zim, zim[:], ALU.mult)
                p.tt("dve", xr, xr[:], t1, t1[:], t2, t2[:], ALU.subtract)
                p.tt("dve", t5, t5[:], Ecb, Ecb[:, j, :], zim, zim[:], ALU.mult)
                p.tt("dve", t6, t6[:], Esb, Esb[:, j, :], zre, zre[:], ALU.mult)
                p.tt("dve", xi, xi[:], t5, t5[:], t6, t6[:], ALU.add)
                p.tt("dve", sm, sm[:, SM["u0"], j:j + 1], sm, sm[:, SM["eti"], j:j + 1], zim, zim[:, T - 1:T], ALU.mult)
                p.tt("dve", sm, sm[:, SM["u1"], j:j + 1], sm, sm[:, SM["eti"], j:j + 1], zre, zre[:, T - 1:T], ALU.mult)
                p.stt(sm, sm[:, SM["zir"], j:j + 1], zre, zre[:, T - 1:T], sm[:, SM["etr"], j:j + 1],
                      sm, sm[:, SM["u0"], j:j + 1], ALU.mult, ALU.subtract)
                p.stt(sm, sm[:, SM["zii"], j:j + 1], zim, zim[:, T - 1:T], sm[:, SM["etr"], j:j + 1],
                      sm, sm[:, SM["u1"], j:j + 1], ALU.mult, ALU.add)
                Y = PS[4 + j // 4]
                p.mm(Y, Y[:], bc["cpr"], bc["cpr"][:, j, :], xr, xr[:], start=(j % 4 == 0), stop=False)
                p.mm(Y, Y[:], bc["cpi"], bc["cpi"][:, j, :], xi, xi[:], start=False, stop=(j % 4 == 3))
            for ft in range(2):
                Y = PS[4 + ft]
                y0, y1, y2, y3 = yv
                p.stt(y0, y0[:], u32, u32[:, ft, :], spk[:, 24 + ft:25 + ft], Y, Y[:], ALU.mult, ALU.add,
                      extra_reads=(spk,))
                p.tt("dve", y1, y1[:], y0, y0[:], y0, y0[:], ALU.mult)
                p.ts("dve", y1, y1[:], y1, y1[:], 0.044715, ALU.mult, 1.0, ALU.add)
                p.tt("dve", y2, y2[:], y1, y1[:], y0, y0[:], ALU.mult)
                p.actf(y3, y3[:], y2, y2[:], AF.Sigmoid, scale=2.0 * math.sqrt(2.0 / math.pi))
                p.tt("dve", gl32, gl32[:, ft, :], y0, y0[:], y3, y3[:], ALU.mult)
                p.copy("act", glb, glb[:, ft, :], gl32, gl32[:, ft, :])
            for ft in range(2):
                G = PS[6]
                for k2 in range(2):
                    p.mm(G, G[:], wg, wg[:, k2, ft * 128:(ft + 1) * 128], glb, glb[:, k2, :],
                         start=(k2 == 0), stop=(k2 == 1))
                y0, y1, y2, y3 = yv
                p.actf(y0, y0[:], G, G[:], AF.Sigmoid, bias=spk[:, 26 + ft:27 + ft], extra_reads=(spk,))
                p.tt("dve", y1, y1[:], gl32, gl32[:, ft, :], y0, y0[:], ALU.mult)
                ob = outb[ft]
                group_norm(y1, y1[:], 128, T, "bd64", 64.0, spk[:, 16 + ft:17 + ft], spk, ob, ob[:], sq, rr, PS[7])
                p.dma("sp", MIX[ft * 128:(ft + 1) * 128, tsl], ob[:], reads=(ob,), writes=(MIX,))
        ph.close()

    def phase_sb(l):
        ph = Phase()
        spk = ph.sb([128, SPN])
        spd = wd(f"sp{l}", [128, SPN])
        p.dma("sp", spk[:], spd[:], reads=(spd,), writes=(spk,))
        qTs = [ph.sb([128, S], BF16) for _ in range(2)]
        kTs = [ph.sb([128, S], BF16) for _ in range(2)]
        nkTs = [ph.sb([128, S], BF16) for _ in range(2)]
        vvs = [ph.sb([128, NKT, 128], BF16) for _ in range(2)]
        for t_ in qTs + kTs + vvs:
            p.memset("dve", t_, t_[:], 0.0)
        Es = [ph.sb([128, 2, 512]) for _ in range(2)]
        SPb = [ph.sb([128, 2, 512], BF16) for _ in range(3)]
        Wb = [ph.sb([128, 2, 512], BF16) for _ in range(2)]
        SPsum = ph.sb([128, 512], BF16)
        sq = ph.sb([128, 512], BF16)
        rr = ph.sb([128, 512])
        ob = [ph.sb([64, 512], BF16) for _ in range(2)]
        iters = []
        for h in range(4):
            for qb in range(NB):
                for a in range(4 * qb + 3, 0, -2):
                    iters.append((h, qb, a))
        n = len(iters)

        def load_head(h):
            hs = slice(h * 64, (h + 1) * 64)
            qT, kT, nkT, vv = qTs[h % 2], kTs[h % 2], nkTs[h % 2], vvs[h % 2]
            p.dma("sp", qT[0:64, :], QT["sb"][hs, :], reads=(QT["sb"],), writes=(qT,))
            p.dma("sp", kT[0:64, :], KT["sb"][hs, :], reads=(KT["sb"],), writes=(kT,))
            p.dma("sp", vv[:, :, 0:64], VV["sb"][:, hs].rearrange("(a p) d -> p a d", p=128), reads=(VV["sb"],),
                  writes=(vv,))
            p.actf(nkT, nkT[:], kT, kT[:], AF.Copy, scale=-1.0)

        def stage1(it):
            h, qb, a = iters[it]
            if qb == 0 and a == 3 and h == 0:
                load_head(0)
            if qb == 1 and a == 7 and h + 1 < 4:
                load_head(h + 1)
            qT, kT = qTs[h % 2], kTs[h % 2]
            qsl = slice(qb * 512, (qb + 1) * 512)
            b0i = 2 * (it % 2)
            E, sp_ = Es[it % 2], SPb[it % 3]
            for t in range(2):
                A = PS[b0i + t]
                ksl = slice((a - t) * 128, (a - t + 1) * 128)
                p.mm(A, A[:], kT, kT[:, ksl], qT, qT[:, qsl], start=True, stop=True)
            p.actf(E, E[:], PS[b0i], ps2(b0i), AF.Exp, extra_reads=(PS[b0i + 1],))
            p.actf(sp_, sp_[:], E, E[:], AF.Ln, bias=1.0)
            for t in range(2):
                diag = a - t - 4 * qb
                if diag >= 0:
                    p.tt("dve", sp_, sp_[:, t, :], sp_, sp_[:, t, :], cbt, cbs(f"m{diag}"), ALU.mult)

        def stage2(it):
            h, qb, a = iters[it]
            qT, nkT = qTs[h % 2], nkTs[h % 2]
            qsl = slice(qb * 512, (qb + 1) * 512)
            sp_, W = SPb[it % 3], Wb[it % 2]
            first = (a == 4 * qb + 3)
            for t in range(2):
                Bp = PS[4 + t]
                ksl = slice((a - t) * 128, (a - t + 1) * 128)
                p.mm(Bp, Bp[:], nkT, nkT[:, ksl], qT, qT[:, qsl], start=True, stop=False)
                lastmm = first and t == 0
                p.mm(Bp, Bp[:], cbt, cbs("tincl"), sp_, sp_[:, t, :], start=False, stop=lastmm)
                if t == 1:
                    p.mm(Bp, Bp[:], cbt, cbs("ones"), sp_, sp_[:, 0, :], start=False, stop=first)
                if not first:
                    p.mm(Bp, Bp[:], cbt, cbs("ones"), SPsum, SPsum[:], start=False, stop=True)
            p.actf(W, W[:], PS[4], ps2(4), AF.Exp, scale=-1.0, extra_reads=(PS[5],))
            for t in range(2):
                diag = a - t - 4 * qb
                if diag >= 0:
                    p.tt("dve", W, W[:, t, :], W, W[:, t, :], cbt, cbs(f"m{diag}"), ALU.mult)
            if a - 1 > 0:
                if first:
                    p.tt("dve", SPsum, SPsum[:], sp_, sp_[:, 0, :], sp_, sp_[:, 1, :], ALU.add)
                else:
                    p.tt("dve", SPsum, SPsum[:], SPsum, SPsum[:], sp_, sp_[:, 0, :], ALU.add)
                    p.tt("dve", SPsum, SPsum[:], SPsum, SPsum[:], sp_, sp_[:, 1, :], ALU.add)

        def stage3(it):
            h, qb, a = iters[it]
            vv, W = vvs[h % 2], Wb[it % 2]
            O = PS[6 + qb % 2]
            for t in range(2):
                p.mm(O, O[:, :], vv, vv[:, a - t, :], W, W[:, t, :], start=(a == 4 * qb + 3 and t == 0),
                     stop=(a - t == 0))
            if a - 1 == 0:
                qsl = slice(qb * 512, (qb + 1) * 512)

                def finA(O=O):
                    p.actf(sq, sq[0:64, :], O, O[0:64, :], AF.Square)

                def finB(O=O, h=h, qb=qb, qsl=qsl):
                    o_ = ob[qb % 2]
                    G = PS[4]
                    p.mm(G, G[0:64, :], cbt, cbs("bd64", rows=64, c1=64), sq, sq[0:64, :], start=True, stop=True)
                    p.actf(rr, rr[0:64, :], G, G[0:64, :], AF.Ln, bias=EPS, scale=1.0 / 64.0)
                    p.actf(rr, rr[0:64, :], rr, rr[0:64, :], AF.Exp, scale=-0.5)
                    p.stt(o_, o_[:], O, O[0:64, :], spk[0:64, 188 + h:189 + h], rr, rr[0:64, :], ALU.mult, ALU.mult,
                          extra_reads=(spk,))
                    p.dma("sp", MIX[256 + h * 64:256 + (h + 1) * 64, qsl], o_[:], reads=(o_,), writes=(MIX,))

                pend.append((cur_step[0] + 1, finA))
                pend.append((cur_step[0] + 2, finB))

        pend = []
        cur_step = [0]
        step = 0
        while step < n + 2 or pend:
            cur_step[0] = step
            if step < n:
                stage1(step)
            if 0 <= step - 1 < n:
                stage2(step - 1)
            if 0 <= step - 2 < n:
                stage3(step - 2)
            due = [f for (d, f) in pend if d <= step]
            pend[:] = [(d, f) for (d, f) in pend if d > step]
            for f in due:
                f()
            step += 1
        ph.close()

    def phase_ch(l):
        ph = Phase()
        spk = ph.sb([128, SPN])
        spd = wd(f"sp{l}", [128, SPN])
        p.dma("sp", spk[:], spd[:], reads=(spd,), writes=(spk,))
        chd = wd(f"chb{l}", [128, 4, 5, 128])
        bt32 = ph.sb([128, 4, 5, 128])
        btb = ph.sb([128, 4, 5, 128], BF16)
        p.dma("sp", bt32[:], chd[:], reads=(chd,), writes=(bt32,))
        for h in range(4):
            p.tt("dve", bt32, bt32[:, h, 0, :], bt32, bt32[:, h, 0, :], cft, cfs("chm0"), ALU.add)
            p.tt("dve", bt32, bt32[:, h, 4, :], bt32, bt32[:, h, 4, :], cft, cfs("chm4"), ALU.add)
        p.copy("dve", btb, btb[:], bt32, bt32[:])
        qTs = [ph.sb([128, S], BF16) for _ in range(2)]
        kTs = [ph.sb([128, S], BF16) for _ in range(2)]
        vas = [ph.sb([128, NKT, 128], BF16) for _ in range(2)]
        for t_ in qTs + kTs:
            p.memset("dve", t_, t_[:], 0.0)
        for va in vas:
            p.memset("dve", va, va[:, :, 64:128], 0.0)
            p.memset("dve", va, va[:, :, 64:65], 1.0)
        Pt = [ph.sb([128, 640], BF16) for _ in range(2)]
        sq = ph.sb([65, 512], BF16)
        rr = ph.sb([64, 512])
        ob = [ph.sb([64, 512], BF16) for _ in range(2)]
        iters = [(h, qt) for h in range(4) for qt in range(NKT)]
        n = len(iters)

        def load_head(h):
            hs = slice(h * 64, (h + 1) * 64)
            p.dma("sp", qTs[h % 2][0:64, :], QT["ch"][hs, :], reads=(QT["ch"],), writes=(qTs[h % 2],))
            p.dma("sp", kTs[h % 2][0:64, :], KT["ch"][hs, :], reads=(KT["ch"],), writes=(kTs[h % 2],))
            p.dma("sp", vas[h % 2][:, :, 0:64], VV["ch"][:, hs].rearrange("(a p) d -> p a d", p=128),
                  reads=(VV["ch"],), writes=(vas[h % 2],))

        def stage1(it):
            h, qt = iters[it]
            if h == 0 and qt == 0:
                load_head(0)
            if qt == 2 and h + 1 < 4:
                load_head(h + 1)
            qT, kT = qTs[h % 2], kTs[h % 2]
            q128 = slice(qt * 128, (qt + 1) * 128)
            nd = min(4, qt) + 1
            X, Y = PS[it % 2], PS[2 + it % 2]
            for d in range(nd):
                a = qt - d
                dst_b = X if d < 4 else Y
                dst = X[:, d * 128:(d + 1) * 128] if d < 4 else Y[:, 0:128]
                p.mm(dst_b, dst, kT, kT[:, a * 128:(a + 1) * 128], qT, qT[:, q128], start=True, stop=False)
                p.mm(dst_b, dst, btb, btb[:, h, d, :], cbt, cbs("ident"), start=False, stop=True)

        def stage2(it):
            h, qt = iters[it]
            va = vas[h % 2]
            nd = min(4, qt) + 1
            X, Y = PS[it % 2], PS[2 + it % 2]
            P_ = Pt[it % 2]
            O = PS[4 + (qt // 4) % 2]
            ocol = slice((qt % 4) * 128, (qt % 4 + 1) * 128)
            n4 = min(nd, 4)
            p.actf(P_, P_[:, 0:n4 * 128], X, X[:, 0:n4 * 128], AF.Exp)
            if nd == 5:
                p.actf(P_, P_[:, 512:640], Y, Y[:, 0:128], AF.Exp)
            for d in range(nd):
                a = qt - d
                p.mm(O, O[:, ocol], va, va[:, a, :], P_, P_[:, d * 128:(d + 1) * 128],
                     start=(d == 0), stop=(d == nd - 1))
            if qt % 4 == 3:
                qb = qt // 4
                qsl = slice(qb * 512, (qb + 1) * 512)
                o_ = ob[qb % 2]
                p.actf(sq, sq[:], O, O[0:65, :], AF.Square, scale=cfs("sclrow", r0=0, r1=65), extra_reads=(cft,))
                SSB = PS[6]
                p.mm(SSB, SSB[0:64, :], cbt, cbs("ones", rows=65, c1=64), sq, sq[:], start=True, stop=True)
                p.actf(rr, rr[:], SSB, SSB[0:64, :], AF.Ln, scale=1.0 / 64.0)
                p.actf(rr, rr[:], rr, rr[:], AF.Exp, scale=-0.5)
                p.stt(o_, o_[:], O, O[0:64, :], spk[0:64, 192 + h:193 + h], rr, rr[:], ALU.mult, ALU.mult,
                      extra_reads=(spk,))
                p.dma("sp", MIX[512 + h * 64:512 + (h + 1) * 64, qsl], o_[:], reads=(o_,), writes=(MIX,))

        for step in range(n + 1):
            if step < n:
                stage1(step)
            if 0 <= step - 1 < n:
                stage2(step - 1)
        ph.close()

    def phase_df(l):
        ph = Phase()
        lam_init = 0.8 - 0.6 * math.exp(-0.3 * l)
        spk = ph.sb([128, SPN])
        spd = wd(f"sp{l}", [128, SPN])
        p.dma("sp", spk[:], spd[:], reads=(spd,), writes=(spk,))
        AX = mybir.AxisListType.X
        sc = ph.sb([128, 8])
        pr = ph.sb([128, 2, 32])
        p.tt("dve", pr, pr[:, 0, :], spk, spk[:, 56:88], spk, spk[:, 88:120], ALU.mult)
        p.tt("dve", pr, pr[:, 1, :], spk, spk[:, 120:152], spk, spk[:, 152:184], ALU.mult)
        p.op("dve", lambda en: en.tensor_reduce(out=sc[:, 0:2], in_=pr[:], axis=AX, op=ALU.add), reads=(pr,), writes=(sc,))
        p.actf(sc, sc[:, 2:4], sc, sc[:, 0:2], AF.Exp)
        p.tt("dve", sc, sc[:, 4:5], sc, sc[:, 3:4], sc, sc[:, 2:3], ALU.subtract)
        p.ts("dve", sc, sc[:, 5:6], sc, sc[:, 4:5], -lam_init, ALU.add)
        lrow = ph.sb([128, 64])
        p.ts("dve", lrow, lrow[:], cft, cfs("ones", c1=64), sc[:, 5:6], ALU.mult, extra_reads=(sc,))
        gdf = ph.sb([64, 4])
        p.ts("dve", gdf, gdf[:], spk, spk[0:64, 196:200], 1.0 - lam_init, ALU.mult)
        qT = ph.sb([128, S], BF16)
        kT = ph.sb([128, 2, S], BF16)
        p.memset("dve", qT, qT[:], 0.0)
        p.memset("dve", kT, kT[:], 0.0)
        va = ph.sb([128, NKT, 128], BF16)
        p.memset("dve", va, va[:, :, 64:128], 0.0)
        p.memset("dve", va, va[:, :, 64:65], 1.0)
        Pt2 = [ph.sb([128, 2, 512], BF16) for _ in range(2)]
        sd2 = [ph.sb([128, 2, 128]) for _ in range(2)]
        rc = ph.sb([128, 1024])
        b0 = ph.sb([64, 512])
        b1 = ph.sb([64, 512])
        t0 = ph.sb([64, 512])
        t1 = ph.sb([64, 512])
        sq = ph.sb([64, 512], BF16)
        rr = ph.sb([64, 512])
        ob = [ph.sb([64, 512], BF16) for _ in range(2)]
        qTs = [qT, ph.sb([128, S], BF16)]
        kTs = [kT, ph.sb([128, 2, S], BF16)]
        p.memset("dve", qTs[1], qTs[1][:], 0.0)
        p.memset("dve", kTs[1], kTs[1][:], 0.0)
        vas = [va, ph.sb([128, NKT, 128], BF16)]
        p.memset("dve", vas[1], vas[1][:, :, 64:128], 0.0)
        p.memset("dve", vas[1], vas[1][:, :, 64:65], 1.0)
        iters = []
        for h in range(4):
            for qb in range(NB):
                for a in range(4 * qb + 4):
                    iters.append((h, qb, a))
        n = len(iters)

        def load_head(h):
            hs = slice(h * 64, (h + 1) * 64)
            p.dma("sp", qTs[h % 2][0:64, :], QT["df"][hs, :], reads=(QT["df"],), writes=(qTs[h % 2],))
            for c_ in range(2):
                p.dma("sp", kTs[h % 2][32 * c_:32 * c_ + 32, c_, :], KT["df"][h * 64 + 32 * c_:h * 64 + 32 * c_ + 32, :],
                      reads=(KT["df"],), writes=(kTs[h % 2],))
            p.dma("sp", vas[h % 2][:, :, 0:64], VV["df"][:, hs].rearrange("(a p) d -> p a d", p=128),
                  reads=(VV["df"],), writes=(vas[h % 2],))

        def stage1(it):
            h, qb, a = iters[it]
            if qb == 0 and a == 0 and h == 0:
                load_head(0)
            if qb == 0 and a == 2 and h + 1 < 4:
                load_head(h + 1)
            qT_, kT_ = qTs[h % 2], kTs[h % 2]
            qsl = slice(qb * 512, (qb + 1) * 512)
            ksl = slice(a * 128, (a + 1) * 128)
            for c in range(2):
                Sc = PS[c + 2 * (it % 2)]
                p.mm(Sc, Sc[:], kT_, kT_[:, c, ksl], qT_, qT_[:, qsl], start=True, stop=True)

        def stage2(it):
            h, qb, a = iters[it]
            sl = SLOPES[h]
            nsub = 2 if h == 0 else 1
            SW = 512 // nsub
            va_ = vas[h % 2]
            qsl = slice(qb * 512, (qb + 1) * 512)
            Oc = (PS[4 + 2 * (qb % 2)], PS[5 + 2 * (qb % 2)])
            amax = 4 * qb + 3
            i = a - 4 * qb
            b0i = 2 * (it % 2)
            S0, S1 = PS[b0i], PS[b0i + 1]
            S2 = ps2(b0i)
            P2 = Pt2[it % 2]
            c0 = 0
            if i >= 0:
                j = i
                r = (128 * j) // SW
                off = 128 * j - r * SW
                sdt = sd2[it % 2]
                for c in range(2):
                    Sc = PS[b0i + c]
                    p.tt("dve", sdt, sdt[:, c, :], Sc, Sc[:, 128 * j:128 * j + 128], cft, cfs(f"dfd{h}"), ALU.add)
                p.actf(P2, P2[:, :, 128 * j:128 * j + 128], sdt, sdt[:], AF.Exp, bias=bias_const(sl * off),
                       extra_reads=(bct,))
                c0 = 128 * (i + 1)
            for r in range(nsub):
                lo = max(r * SW, c0)
                hi = (r + 1) * SW
                if lo >= hi:
                    continue
                m = (qb * 512 + r * SW - 128 * a) // 128
                p.actf(P2, P2[:, :, lo:hi], S0, S2[:, :, lo:hi], AF.Exp,
                       bias=cfs(f"dfb{h}", c0=m + 3, c1=m + 4), extra_reads=(cft, S1))
            w0 = 128 * max(i, 0)
            for c in range(2):
                p.mm(Oc[c], Oc[c][:, w0:512], va_, va_[:, a, :], P2, P2[:, c, w0:512],
                     start=(a == 0), stop=(a == amax))
            c = 1
            if a == amax:
                rcq = rcs[qb % 2]

                def finA(Oc=Oc, rcq=rcq):
                    p.copy("act", rcq, rcq[64:65, 0:512], Oc[0], Oc[0][64:65, :])
                    p.copy("act", rcq, rcq[64:65, 512:1024], Oc[1], Oc[1][64:65, :])
                    p.recip(rcq, rcq[64:65, :], rcq, rcq[64:65, :])

                def finB(Oc=Oc, rcq=rcq, h=h, qb=qb, qsl=qsl):
                    bi = 2 * ((cur_step[0] + 1) % 2)
                    B0, B1 = PS[bi], PS[bi + 1]
                    p.mm(B0, B0[0:64, :], cft, cfs("ones", r0=64, r1=65, c1=64), rcq, rcq[64:65, 0:512],
                         start=True, stop=True)
                    p.mm(B1, B1[0:64, :], lrow, lrow[64:65, :], rcq, rcq[64:65, 512:1024], start=True, stop=True)
                    p.copy("act", b0, b0[:], B0, B0[0:64, :])
                    p.copy("act", b1, b1[:], B1, B1[0:64, :])
                    p.tt("dve", t0, t0[:], Oc[0], Oc[0][0:64, :], b0, b0[:], ALU.mult)
                    p.tt("dve", t1, t1[:], Oc[1], Oc[1][0:64, :], b1, b1[:], ALU.mult)
                    p.tt("dve", t0, t0[:], t0, t0[:], t1, t1[:], ALU.add)

                def finC(h=h, qb=qb, qsl=qsl):
                    bi = 2 * ((cur_step[0] + 1) % 2)
                    o_ = ob[qb % 2]
                    group_norm(t0, t0[:], 64, 512, "bd64", 64.0, gdf[:, h:h + 1], gdf, o_, o_[:], sq, rr, PS[bi])
                    p.dma("sp", MIX[768 + h * 64:768 + (h + 1) * 64, qsl], o_[:], reads=(o_,), writes=(MIX,))

                pend.append((cur_step[0] + 1, finA))
                pend.append((cur_step[0] + 2, finB))
                pend.append((cur_step[0] + 3, finC))

        rcs = [rc, ph.sb([128, 1024])]
        pend = []
        cur_step = [0]
        step = 0
        while step < n + 1 or pend:
            cur_step[0] = step
            if step < n:
                stage1(step)
            if 0 <= step - 1 < n:
                stage2(step - 1)
            due = [f for (d, f) in pend if d <= step]
            pend[:] = [(d, f) for (d, f) in pend if d > step]
            for f in due:
                f()
            step += 1
        ph.close()

    mixers_local = {"ssm": phase_ssm, "sb": phase_sb, "ch": phase_ch, "df": phase_df}

    mixers = mixers_local

    def run_layers():
        nl = len(layer_list)
        for li, l in enumerate(layer_list):
            xsrc = xT if (li == 0 and first_layer_from_x) else XR
            moe = (l % 2 == 1)
            moe_idx = l // 2 if moe else None
            dense_idx = l // 2 if not moe else None
            if "n1" in phases:
                phase_n1(l, xsrc)
            for m in ("ssm", "sb", "ch", "df"):
                if m in phases:
                    mixers[m](l)
            if "op" in phases:
                phase_op(l, xsrc, moe_idx)
            if "ffn" in phases:
                dst = yT if (li == nl - 1 and last_to_y) else XR
                phase_ffn(l, moe_idx, dense_idx, dst)

    return nc, p, run_layers, mixers, locals()


S_FULL = 4096
LAUNCH_GROUPS = [[0, 1, 2, 3]]


def _layer_inputs(inp, l):
    m = {f"sp{l}": pack_small(inp, l),
         f"w_in{l}": np.ascontiguousarray(inp["w_in"][l]),
         f"w_out{l}": np.ascontiguousarray(inp["w_out"][l]),
         f"wglu{l}": np.ascontiguousarray(inp["ssm_w_glu"][l]),
         f"chb{l}": pack_chb(inp, l)}
    bpr, bpi, cpr, cpi = pack_ssm_bc(inp, l)
    m.update({f"bpr{l}": bpr, f"bpi{l}": bpi, f"cpr{l}": cpr, f"cpi{l}": cpi})
    i = l // 2
    if l % 2 == 0:
        m.update({f"w1_{i}": np.ascontiguousarray(inp["ffn_w1"][i]), f"w3_{i}": np.ascontiguousarray(inp["ffn_w3"][i]),
                  f"w2_{i}": np.ascontiguousarray(inp["ffn_w2"][i])})
    else:
        m.update({f"mw1_{i}": np.ascontiguousarray(inp["moe_w1"][i]), f"mw3_{i}": np.ascontiguousarray(inp["moe_w3"][i]),
                  f"mw2_{i}": np.ascontiguousarray(inp["moe_w2"][i]), f"mr{i}": np.ascontiguousarray(inp["moe_router"][i])})
    return m


def kernel(**inputs):
    inp = {k: np.asarray(v) for k, v in inputs.items()}
    x = inp["x"].astype(np.float32, copy=False)
    B, S, _ = x.shape
    cb, cf = make_consts()
    cur = [np.ascontiguousarray(x[b].T) for b in range(B)]
    for grp in LAUNCH_GROUPS:
        nc, p, run_layers, mixers, loc = build(S, grp)
        run_layers()
        p.finish()
        used = set(loc["used_inputs"])
        shared = {"cb": cb, "cf": cf}
        for l in grp:
            shared.update(_layer_inputs(inp, l))
        shared = {k: v for k, v in shared.items() if k in used}
        in_maps = []
        for b in range(B):
            m = dict(shared)
            m["xT"] = cur[b]
            in_maps.append(m)
        res = run_bass_kernel_spmd(nc, in_maps, core_ids=list(range(B)))
        cur = [np.ascontiguousarray(res.results[b]["yT"]) for b in range(B)]
    out = np.stack([cur[b].T for b in range(B)], axis=0)
    return np.ascontiguousarray(out.astype(np.float32, copy=False))
```

```python
import math
import numpy as np
import ml_dtypes
import concourse.bass as bass
import concourse.mybir as mybir
from concourse.bass_utils import run_bass_kernel_spmd

F32 = mybir.dt.float32
BF16 = mybir.dt.bfloat16
AF = mybir.ActivationFunctionType
ALU = mybir.AluOpType

D = 1024
DEPTH = 4
NCORES = 8
EPS = 1e-6
IN_COLS = 2560
D_FF = 2816
D_FFE = 3584
NEXP = 8
GATE_ENG = "dve"
H_DF = 4
SLOPES = [2.0 ** (-8.0 * (h + 1) / H_DF) for h in range(H_DF)]


class Buf:
    __slots__ = ("h", "lw", "rd", "name")

    def __init__(self, h, name=""):
        self.h = h
        self.lw = None
        self.rd = {}
        self.name = name

    def __getitem__(self, idx):
        return self.h[idx]

    def view(self, idx):
        return Buf(self.h[idx], self.name)


class Prog:
    SEM_ROT = 30000

    def __init__(self, nc):
        self.nc = nc
        self.eng = {"pe": nc.tensor, "act": nc.scalar, "dve": nc.vector, "pool": nc.gpsimd, "sp": nc.sync}
        self.sem = {}
        self.cnt = {}
        self.nsem = 0
        for e in self.eng:
            self._newsem(e)
        self.seen = {e: {} for e in self.eng}
        self.ndsem = 16
        self.dsem = [nc.alloc_semaphore(f"dq{i}") for i in range(self.ndsem)]
        self.dcnt = [0] * self.ndsem
        self.dnext = 0
        self.dnext_sw = 0
        self.ninst = 0
        self._uid = 0
        self.pending = {}

    def _newsem(self, e):
        self.nsem += 1
        self.sem[e] = self.nc.alloc_semaphore(f"s{e}{self.nsem}")
        self.cnt[e] = 0

    def uid(self, p="t"):
        self._uid += 1
        return f"{p}{self._uid}"

    def sb(self, shape, dt=F32, name=None):
        return Buf(self.nc.alloc_sbuf_tensor(name or self.uid("sb"), list(shape), dt), name or "")

    def ps(self, shape, dt=F32, name=None):
        return Buf(self.nc.alloc_psum_tensor(name or self.uid("ps"), list(shape), dt), name or "")

    def dram(self, name, shape, dt, kind="Internal"):
        return Buf(self.nc.dram_tensor(name, list(shape), dt, kind=kind).ap(), name)

    def _collect(self, reads, writes):
        t = {}

        def add(k, v):
            if t.get(k, 0) < v:
                t[k] = v

        for b in reads:
            if b.lw is not None:
                add(*b.lw)
        for b in writes:
            if b.lw is not None:
                add(*b.lw)
            for k, v in b.rd.items():
                add(k, v)
        return t

    def _wait(self, e, tickets, skip_own=False):
        own = self.sem[e]
        seen = self.seen[e]
        for s, v in tickets.items():
            if skip_own and s is own:
                continue
            if seen.get(s, 0) < v:
                self.eng[e].wait_ge(s, v)
                seen[s] = v

    def _mark(self, reads, writes, tk):
        s, v = tk
        for b in reads:
            if b.rd.get(s, 0) < v:
                b.rd[s] = v
        for b in writes:
            b.lw = tk
            b.rd = {}

    def op(self, e, fn, reads=(), writes=(), sig=True):
        self._wait(e, self._collect(reads, writes), skip_own=(e == "pe"))
        inst = fn(self.eng[e])
        self.ninst += 1
        if sig:
            if self.cnt[e] >= self.SEM_ROT:
                self._newsem(e)
            self.cnt[e] += 1
            inst.then_inc(self.sem[e], 1)
            tk = (self.sem[e], self.cnt[e])
        else:
            assert self.cnt[e] < self.SEM_ROT + 10000
            tk = (self.sem[e], self.cnt[e] + 1)
        self._mark(reads, writes, tk)
        return inst

    def dma(self, q, out_ap, in_ap, reads=(), writes=(), **kw):
        t = self._collect(reads, writes)
        if q == "pool":
            k = 10 + self.dnext_sw
            self.dnext_sw = (self.dnext_sw + 1) % 6
        else:
            k = self.dnext
            self.dnext = (self.dnext + 1) % 10
        if self.dcnt[k] > 0:
            s = self.dsem[k]
            if t.get(s, 0) < self.dcnt[k]:
                t[s] = self.dcnt[k]
        self._wait(q, t)
        inst = self.eng[q].dma_start(out=out_ap, in_=in_ap, **kw)
        self.ninst += 1
        self.dcnt[k] += 16
        inst.then_inc(self.dsem[k], 16)
        self._mark(reads, writes, (self.dsem[k], self.dcnt[k]))
        return inst

    def finish(self):
        t = {}
        for k in range(self.ndsem):
            if self.dcnt[k] > 0:
                t[self.dsem[k]] = self.dcnt[k]
        for e in ("pe", "act", "dve", "pool"):
            if self.cnt[e] > 0:
                t[self.sem[e]] = self.cnt[e]
        self._wait("sp", t)

    def mm(self, out_b, out_ap, l_b, l_ap, r_b, r_ap, start, stop, sig=None):
        if sig is None:
            sig = stop
        return self.op("pe", lambda en: en.matmul(out_ap, lhsT=l_ap, rhs=r_ap, start=start, stop=stop),
                       reads=(l_b, r_b) if start else (l_b, r_b), writes=(out_b,), sig=sig)

    def actf(self, out_b, out_ap, in_b, in_ap, func, bias=None, scale=None, extra_reads=()):
        kw = {}
        if bias is not None:
            kw["bias"] = bias
        if scale is not None:
            kw["scale"] = scale
        return self.op("act", lambda en: en.activation(out=out_ap, in_=in_ap, func=func, **kw),
                       reads=(in_b,) + tuple(extra_reads), writes=(out_b,))

    def tt(self, e, out_b, out_ap, a_b, a_ap, b_b, b_ap, op):
        return self.op(e, lambda en: en.tensor_tensor(out=out_ap, in0=a_ap, in1=b_ap, op=op),
                       reads=(a_b, b_b), writes=(out_b,))

    def ts(self, e, out_b, out_ap, a_b, a_ap, s1, op0, s2=None, op1=None, extra_reads=()):
        if op1 is None:
            return self.op(e, lambda en: en.tensor_scalar(out=out_ap, in0=a_ap, scalar1=s1, scalar2=None, op0=op0),
                           reads=(a_b,) + tuple(extra_reads), writes=(out_b,))
        return self.op(e, lambda en: en.tensor_scalar(out=out_ap, in0=a_ap, scalar1=s1, scalar2=s2, op0=op0, op1=op1),
                       reads=(a_b,) + tuple(extra_reads), writes=(out_b,))

    def stt(self, out_b, out_ap, a_b, a_ap, scalar, b_b, b_ap, op0, op1, extra_reads=()):
        return self.op("dve", lambda en: en.scalar_tensor_tensor(out=out_ap, in0=a_ap, scalar=scalar, in1=b_ap,
                                                                 op0=op0, op1=op1),
                       reads=(a_b, b_b) + tuple(extra_reads), writes=(out_b,))

    def copy(self, e, out_b, out_ap, in_b, in_ap):
        if e == "act":
            return self.op("act", lambda en: en.copy(out=out_ap, in_=in_ap), reads=(in_b,), writes=(out_b,))
        return self.op(e, lambda en: en.tensor_copy(out=out_ap, in_=in_ap), reads=(in_b,), writes=(out_b,))

    def memset(self, e, out_b, out_ap, val):
        return self.op(e, lambda en: en.memset(out_ap, val), reads=(), writes=(out_b,))

    def recip(self, out_b, out_ap, in_b, in_ap):
        return self.op("dve", lambda en: en.reciprocal(out=out_ap, in_=in_ap), reads=(in_b,), writes=(out_b,))


def _prog_patch():
    def op(self, e, fn, reads=(), writes=(), sig=True):
        self._wait(e, self._collect(reads, writes), skip_own=(e == "pe"))
        inst = fn(self.eng[e])
        self.ninst += 1
        pend = self.pending.get(e, False)
        if sig:
            if self.cnt[e] >= self.SEM_ROT and not pend:
                self._newsem(e)
            self.cnt[e] += 1
            inst.then_inc(self.sem[e], 1)
            tk = (self.sem[e], self.cnt[e])
            self.pending[e] = False
        else:
            tk = (self.sem[e], self.cnt[e] + 1)
            self.pending[e] = True
        self._mark(reads, writes, tk)
        return inst

    def barrier(self):
        t = {}
        for k in range(self.ndsem):
            if self.dcnt[k] > 0:
                t[self.dsem[k]] = self.dcnt[k]
        for e in ("pe", "act", "dve", "pool", "sp"):
            assert not self.pending.get(e, False)
            if self.cnt[e] > 0:
                t[self.sem[e]] = self.cnt[e]
        for e in ("pe", "act", "dve", "pool", "sp"):
            self._wait(e, t)

    Prog.op = op
    Prog.barrier = barrier


_prog_patch()


CB = {}
CF = {}


def _layout_consts():
    off = 0
    for name, w in (("ones", 128), ("ident", 128), ("bd64", 128), ("bd32", 128), ("tincl", 128),
                    ("m0", 512), ("m1", 512), ("m2", 512), ("m3", 512)):
        CB[name] = (off, w)
        off += w
    CB["_n"] = off
    off = 0
    for name, w in (("ident", 128), ("chm0", 128), ("chm4", 128), ("dfd0", 128), ("dfd1", 128), ("dfd2", 128),
                    ("dfd3", 128), ("dfb0", 36), ("dfb1", 36), ("dfb2", 36), ("dfb3", 36), ("sel", 8 * 128),
                    ("sclrow", 1), ("ones", 128)):
        CF[name] = (off, w)
        off += w
    CF["_n"] = off


_layout_consts()


def make_consts():
    cb = np.zeros((128, CB["_n"]), np.float32)
    cf = np.zeros((128, CF["_n"]), np.float32)
    i = np.arange(128)

    def setb(n, a):
        o, w = CB[n]
        cb[:, o:o + w] = a

    def setf(n, a):
        o, w = CF[n]
        cf[:a.shape[0], o:o + w] = a

    setb("ones", np.ones((128, 128)))
    setb("ident", np.eye(128))
    setb("bd64", (i[:, None] // 64 == i[None, :] // 64).astype(np.float32))
    setb("bd32", (i[:, None] // 32 == i[None, :] // 32).astype(np.float32))
    setb("tincl", (i[:, None] >= i[None, :]).astype(np.float32))
    q = np.arange(512)
    for m in range(4):
        setb(f"m{m}", ((128 * m + i[:, None]) < q[None, :]).astype(np.float32))
    setf("ident", np.eye(128, dtype=np.float32))
    qq = i[:, None]
    kk = i[None, :]
    setf("chm0", np.where((kk >= 64) & (qq < 64), -1e30, 0.0).astype(np.float32))
    setf("chm4", np.where((kk < 64) & (qq >= 64), -1e30, 0.0).astype(np.float32))
    kk2 = i[:, None]
    qq2 = i[None, :]
    for h in range(4):
        sl = SLOPES[h]
        t = -sl * np.abs(qq2 - kk2) + sl * qq2
        t = np.where((kk2 // 64) <= (qq2 // 64), t, -1e30)
        setf(f"dfd{h}", t.astype(np.float32))
        setf(f"dfb{h}", (sl * (i[:, None] - 128.0 * (np.arange(36)[None, :] - 3.0))).astype(np.float32))
    sel = np.zeros((128, 8, 128), np.float32)
    for e in range(8):
        sel[e, e, :] = 1.0
    setf("sel", sel.reshape(128, 8 * 128))
    scl = np.ones((128, 1), np.float32)
    scl[64, 0] = math.sqrt(64.0 * EPS)
    setf("sclrow", scl)
    setf("ones", np.ones((128, 128), np.float32))
    return cb.astype(ml_dtypes.bfloat16), cf


SP = {"g1": (0, 8), "g2": (8, 8), "go": (16, 8), "d": (24, 2), "bglu": (26, 2), "chq": (28, 1), "chk": (29, 1),
      "dfq": (30, 1), "dfk": (31, 1), "lre": (32, 8), "lim": (40, 8), "ldt": (48, 8), "dfl": (56, 128), "goh": (184, 16)}
SPN = 200


def pack_small(inp, l):
    a = np.zeros((128, SPN), np.float32)

    def fm(v):
        return np.ascontiguousarray(v.reshape(-1, 128).T)

    a[:, 0:8] = fm(inp["norm_mix_g"][l])
    a[:, 8:16] = fm(inp["norm_ffn_g"][l])
    a[:, 16:24] = fm(inp["out_norm_g"][l])
    a[:, 24:26] = fm(inp["ssm_d"][l])
    a[:, 26:28] = fm(inp["ssm_b_glu"][l])
    a[:, 28] = np.tile(inp["ch_q_norm_g"][l], 2)
    a[:, 29] = np.tile(inp["ch_k_norm_g"][l], 2)
    a[:, 30] = np.tile(inp["df_q_norm_g"][l].reshape(-1), 2)
    a[:, 31] = np.tile(inp["df_k_norm_g"][l].reshape(-1), 2)
    for nm, key in (("lre", "ssm_lam_re"), ("lim", "ssm_lam_im")):
        o = SP[nm][0]
        v = inp[key][l].reshape(8, 2, 64)
        a[:, o:o + 8] = v.transpose(1, 2, 0).reshape(128, 8)
    o = SP["ldt"][0]
    v = np.repeat(inp["ssm_log_dt"][l].reshape(8, 2, 1), 64, axis=2)
    a[:, o:o + 8] = v.transpose(1, 2, 0).reshape(128, 8)
    o = SP["dfl"][0]
    a[:, o:o + 128] = inp["df_lambda"][l].reshape(1, 128)
    a[0:64, 184:200] = inp["out_norm_g"][l].reshape(16, 64).T
    return a


def pack_ssm_bc(inp, l):
    b_re, b_im = inp["ssm_b_re"][l], inp["ssm_b_im"][l]
    c_re, c_im = inp["ssm_c_re"][l], inp["ssm_c_im"][l]
    outs = []
    for b in (b_re, b_im):
        pad = np.zeros((8, 128, 128), np.float32)
        for g in range(16):
            j, g2 = g // 2, g % 2
            k0 = (g % 8) * 16
            pad[j, k0:k0 + 16, g2 * 64:(g2 + 1) * 64] = b[g].T
        outs.append(np.ascontiguousarray(pad.transpose(1, 0, 2)))
    for c in (c_re, c_im):
        pad = np.zeros((8, 128, 128), np.float32)
        for g in range(16):
            j, g2 = g // 2, g % 2
            m0 = (g % 8) * 16
            pad[j, g2 * 64:(g2 + 1) * 64, m0:m0 + 16] = c[g].T
        outs.append(np.ascontiguousarray(pad.transpose(1, 0, 2)))
    return outs


def pack_chb(inp, l):
    rb = inp["ch_rel_bias"][l]
    q = np.arange(128)[:, None]
    k = np.arange(128)[None, :]
    out = np.zeros((128, 4, 5, 128), np.float32)
    for d in range(5):
        idx = np.clip(128 * d + q - k, -128, 128) + 128
        out[:, :, d, :] = rb[:, idx].transpose(1, 0, 2)
    return out


from contextlib import ExitStack

ALL_PHASES = ("n1", "ssm", "sb", "ch", "df", "op", "ffn")


def build(S, layer_list, phases=ALL_PHASES, io=None, first_layer_from_x=True, last_to_y=True):
    nc = bass.Bass("TRN2", target_bir_lowering=False)
    p = Prog(nc)
    NB = S // 512
    NKT = S // 128
    io = io or {}
    used_inputs = []

    def dr(name, shape, dt, kind="Internal"):
        if name in io:
            kind = "ExternalInput" if io[name] == "in" else "ExternalOutput"
        if kind == "ExternalInput":
            used_inputs.append(name)
        return p.dram(name, shape, dt, kind)

    xT = dr("xT", [1024, S], F32, "ExternalInput")
    yT = dr("yT", [1024, S], F32, "ExternalOutput")
    cb_d = dr("cb", [128, CB["_n"]], BF16, "ExternalInput")
    cf_d = dr("cf", [128, CF["_n"]], F32, "ExternalInput")
    XR = dr("XR", [1024, S], F32)
    HT = dr("HT", [1024, S], BF16)
    UT = dr("UT", [256, S], F32)
    QT = {g: dr(f"QT{g}", [256, S], BF16) for g in ("sb", "ch", "df")}
    KT = {g: dr(f"KT{g}", [256, S], BF16) for g in ("sb", "ch", "df")}
    VV = {g: dr(f"VV{g}", [S, 256], BF16) for g in ("sb", "ch", "df")}
    MIX = dr("MIX", [1024, S], BF16)
    GT = dr("GT", [8, S], F32)

    PSALL = nc.alloc_psum_tensor("psall", [128, 8, 512], F32)
    PS = [Buf(PSALL[:, i, :], f"psb{i}") for i in range(8)]

    def ps2(i):
        return PSALL[:, i:i + 2, :]
    cbt = p.sb([128, CB["_n"]], BF16, name="cbt")
    cft = p.sb([128, CF["_n"]], F32, name="cft")
    p.dma("sp", cbt[:], cb_d[:], reads=(cb_d,), writes=(cbt,))
    p.dma("sp", cft[:], cf_d[:], reads=(cf_d,), writes=(cft,))

    def cbs(name, rows=128, c0=0, c1=None):
        o, w = CB[name]
        c1 = w if c1 is None else c1
        return cbt[0:rows, o + c0:o + c1]

    def cfs(name, r0=0, r1=128, c0=0, c1=None):
        o, w = CF[name]
        c1 = w if c1 is None else c1
        return cft[r0:r1, o + c0:o + c1]

    bct = p.sb([128, 16], F32, name="bct")
    _bias_cols = {}
    for _h in range(4):
        for _o in range(4):
            _v = float(SLOPES[_h] * 128 * _o)
            p.memset("pool", bct, bct[:, _h * 4 + _o:_h * 4 + _o + 1], _v)
            _bias_cols[round(_v, 6)] = _h * 4 + _o

    def bias_const(v):
        c = _bias_cols[round(float(v), 6)]
        return bct[:, c:c + 1]

    class Phase:
        def __init__(self):
            self.stk = ExitStack()

        def sb(self, shape, dt=F32):
            h = self.stk.enter_context(nc.sbuf_tensor(p.uid("t"), list(shape), dt))
            return Buf(h)

        def close(self):
            p.barrier()
            self.stk.close()

    def ld_w(ph, name, shape_dram, pattern, sb_shape, **kw):
        d = dr(name, shape_dram, F32, "ExternalInput")
        t = ph.sb(sb_shape, BF16)
        return d, t

    wdecl = {}

    def wd(name, shape):
        if name not in wdecl:
            wdecl[name] = dr(name, shape, F32, "ExternalInput")
        return wdecl[name]

    def cast_load(dst_b, dst_ap, src_b, src_ap):
        p.dma("pool", dst_ap, src_ap, reads=(src_b,), writes=(dst_b,), max_dma_last_dim=4096)

    def rms_block(ph, xt, spk, gofs, hT, tmp_sq, tmp_r, psb, h32=None):
        p.actf(tmp_sq, tmp_sq[:], xt, xt[:], AF.Square)
        for c in range(8):
            p.mm(psb, psb[:], cbt, cbs("ones"), tmp_sq, tmp_sq[:, c, :], start=(c == 0), stop=(c == 7))
        p.actf(tmp_r, tmp_r[:], psb, psb[:], AF.Ln, bias=EPS, scale=1.0 / 1024.0)
        p.actf(tmp_r, tmp_r[:], tmp_r, tmp_r[:], AF.Exp, scale=-0.5)
        for c in range(8):
            p.stt(hT, hT[:, c, :], xt, xt[:, c, :], spk[:, gofs + c:gofs + c + 1], tmp_r, tmp_r[:],
                  ALU.mult, ALU.mult, extra_reads=(spk,))
            if h32 is not None:
                p.stt(h32, h32[:, c, :], xt, xt[:, c, :], spk[:, gofs + c:gofs + c + 1], tmp_r, tmp_r[:],
                      ALU.mult, ALU.mult, extra_reads=(spk,))

    def group_norm(src_b, src_ap, rows, N, bdname, gsz, gain_ap, gain_b, out_b, out_ap, sq, rr, psb, gscale=None):
        p.actf(sq, sq[0:rows, 0:N], src_b, src_ap, AF.Square)
        p.mm(psb, psb[0:rows, 0:N], cbt, cbs(bdname, rows=rows, c1=rows), sq, sq[0:rows, 0:N], start=True, stop=True)
        p.actf(rr, rr[0:rows, 0:N], psb, psb[0:rows, 0:N], AF.Ln, bias=EPS, scale=1.0 / gsz)
        p.actf(rr, rr[0:rows, 0:N], rr, rr[0:rows, 0:N], AF.Exp, scale=-0.5)
        p.stt(out_b, out_ap, src_b, src_ap, gain_ap, rr, rr[0:rows, 0:N], ALU.mult, ALU.mult, extra_reads=(gain_b,))

    def phase_n1(l, xsrc):
        ph = Phase()
        spk = ph.sb([128, SPN])
        spd = wd(f"sp{l}", [128, SPN])
        p.dma("sp", spk[:], spd[:], reads=(spd,), writes=(spk,))
        wind = wd(f"w_in{l}", [1024, IN_COLS])
        win = ph.sb([128, 8, IN_COLS], BF16)
        wv = wind[:].rearrange("(c p) n -> p c n", p=128)
        for c in range(8):
            cast_load(win, win[:, c, :], wind, wv[:, c, :])
        gq = ph.sb([128, 4])
        p.ts("dve", gq, gq[:, 0:1], spk, spk[:, 28:29], 0.125, ALU.mult)
        p.copy("dve", gq, gq[:, 1:2], spk, spk[:, 29:30])
        p.ts("dve", gq, gq[:, 2:3], spk, spk[:, 30:31], 32.0 ** -0.5, ALU.mult)
        p.copy("dve", gq, gq[:, 3:4], spk, spk[:, 31:32])
        xts = [ph.sb([128, 8, 512]) for _ in range(3)]
        sqs = [ph.sb([128, 8, 512], BF16) for _ in range(2)]
        hTs = [ph.sb([128, 8, 512], BF16) for _ in range(2)]
        rrs = [ph.sb([128, 512]) for _ in range(2)]
        evs = [ph.sb([128, 512], BF16) for _ in range(4)]
        evf = [ph.sb([128, 512]) for _ in range(2)]
        sq2 = [ph.sb([128, 512], BF16) for _ in range(2)]
        rr2 = [ph.sb([128, 512]) for _ in range(2)]
        vts = [ph.sb([128, 768], BF16) for _ in range(2)]
        xv = xsrc[:].rearrange("(c p) t -> p c t", p=128)
        cnt = {"ev": 0, "ps": 0, "nm": 0}
        fm = [("u", 0, 0), ("u", 128, 1), ("qsb", 256, 0), ("qsb", 384, 1), ("ksb", 512, 0), ("ksb", 640, 1),
              ("qch", 1024, 0), ("qch", 1152, 1), ("kch", 1280, 0), ("kch", 1408, 1),
              ("qdf", 1792, 0), ("qdf", 1920, 1), ("kdf", 2048, 0), ("kdf", 2176, 1)]

        def stageL(tb):
            xt = xts[tb % 3]
            p.dma("sp", xt[:], xv[:, :, tb * 512:(tb + 1) * 512], reads=(xsrc,), writes=(xt,))

        def stageA(tb):
            rms_block(ph, xts[tb % 3], spk, SP["g1"][0], hTs[tb % 2], sqs[tb % 2], rrs[tb % 2], PS[7])

        def stageB(tb):
            hT = hTs[tb % 2]
            tsl = slice(tb * 512, (tb + 1) * 512)
            pending = [None]

            def finish_norm():
                if pending[0] is None:
                    return
                kind, rows, psb, ev, sq_, rr_, pn = pending[0]
                pending[0] = None
                gi = {"qch": 0, "kch": 1, "qdf": 2, "kdf": 3}[kind]
                ch = kind.endswith("ch")
                p.mm(pn, pn[:], cbt, cbs("bd64" if ch else "bd32"), sq_, sq_[:], start=True, stop=True)
                p.actf(rr_, rr_[:], pn, pn[:], AF.Ln, bias=EPS, scale=1.0 / (64.0 if ch else 32.0))
                p.actf(rr_, rr_[:], rr_, rr_[:], AF.Exp, scale=-0.5)
                p.stt(ev, ev[:], psb, psb[:], gq[:, gi:gi + 1], rr_, rr_[:], ALU.mult, ALU.mult, extra_reads=(gq,))
                dst = (QT if kind[0] == "q" else KT)["ch" if ch else "df"]
                p.dma("sp", dst[rows, tsl], ev[:], reads=(ev,), writes=(dst,))

            for kind, col, half in fm:
                psb = PS[cnt["ps"] % 4]
                cnt["ps"] += 1
                for c in range(8):
                    p.mm(psb, psb[:], win, win[:, c, col:col + 128], hT, hT[:, c, :], start=(c == 0), stop=(c == 7))
                finish_norm()
                rows = slice(half * 128, (half + 1) * 128)
                if kind == "u":
                    ev = evf[cnt["ev"] % 2]
                    cnt["ev"] += 1
                    p.copy("act", ev, ev[:], psb, psb[:])
                    p.dma("sp", UT[rows, tsl], ev[:], reads=(ev,), writes=(UT,))
                elif kind == "qsb":
                    ev = evs[cnt["ev"] % 4]
                    cnt["ev"] += 1
                    p.actf(ev, ev[:], psb, psb[:], AF.Copy, scale=0.125)
                    p.dma("sp", QT["sb"][rows, tsl], ev[:], reads=(ev,), writes=(QT["sb"],))
                elif kind == "ksb":
                    ev = evs[cnt["ev"] % 4]
                    cnt["ev"] += 1
                    p.copy("act", ev, ev[:], psb, psb[:])
                    p.dma("sp", KT["sb"][rows, tsl], ev[:], reads=(ev,), writes=(KT["sb"],))
                else:
                    ev = evs[cnt["ev"] % 4]
                    cnt["ev"] += 1
                    k2 = cnt["nm"] % 2
                    cnt["nm"] += 1
                    p.actf(sq2[k2], sq2[k2][:], psb, psb[:], AF.Square)
                    pending[0] = (kind, rows, psb, ev, sq2[k2], rr2[k2], PS[4 + k2])
            for tt_ in range(4):
                vt = vts[tt_ % 2]
                tok = slice(tt_ * 128, (tt_ + 1) * 128)
                for gi, (g, col) in enumerate((("sb", 768), ("ch", 1536), ("df", 2304))):
                    psb = PS[cnt["ps"] % 4]
                    cnt["ps"] += 1
                    for c in range(8):
                        p.mm(psb, psb[:, 0:256], hT, hT[:, c, tok], win, win[:, c, col:col + 256],
                             start=(c == 0), stop=(c == 7))
                    if tt_ == 0 and gi == 0:
                        finish_norm()
                    p.copy("act" if gi != 1 else "dve", vt, vt[:, gi * 256:(gi + 1) * 256], psb, psb[:, 0:256])
                t0 = tb * 512 + tt_ * 128
                for gi, g in enumerate(("sb", "ch", "df")):
                    p.dma("sp", VV[g][t0:t0 + 128, :], vt[:, gi * 256:(gi + 1) * 256], reads=(vt,), writes=(VV[g],))

        stageL(0)
        if NB > 1:
            stageL(1)
        stageA(0)
        for tb in range(NB):
            if tb + 2 < NB:
                stageL(tb + 2)
            if tb + 1 < NB:
                stageA(tb + 1)
            stageB(tb)
        ph.close()

    def phase_op(l, xsrc, moe_idx):
        ph = Phase()
        spk = ph.sb([128, SPN])
        spd = wd(f"sp{l}", [128, SPN])
        p.dma("sp", spk[:], spd[:], reads=(spd,), writes=(spk,))
        wod = wd(f"w_out{l}", [1024, 1024])
        wo = ph.sb([128, 8, 1024], BF16)
        wv = wod[:].rearrange("(c p) n -> p c n", p=128)
        for c in range(8):
            cast_load(wo, wo[:, c, :], wod, wv[:, c, :])
        if moe_idx is not None:
            wrd = wd(f"mr{moe_idx}", [1024, 8])
            wr = ph.sb([128, 8, 8])
            p.dma("sp", wr[:], wrd[:].rearrange("(c p) e -> p c e", p=128), reads=(wrd,), writes=(wr,))
            h32s = [ph.sb([128, 8, 512]) for _ in range(2)]
            lg = ph.sb([128, 4, 8])
            wk = [ph.sb([128, 4, 8]) for _ in range(4)]
            mx = [ph.sb([128, 4, 1]) for _ in range(3)]
            gts = ph.sb([8, 512])
        xts = [ph.sb([128, 8, 512]) for _ in range(2)]
        xns = [ph.sb([128, 8, 512]) for _ in range(2)]
        mxs = [ph.sb([128, 8, 512], BF16) for _ in range(2)]
        sqs = [ph.sb([128, 8, 512], BF16) for _ in range(2)]
        hTs = [ph.sb([128, 8, 512], BF16) for _ in range(2)]
        rrs = [ph.sb([128, 512]) for _ in range(2)]
        xv = xsrc[:].rearrange("(c p) t -> p c t", p=128)
        xo = XR[:].rearrange("(c p) t -> p c t", p=128)
        mv = MIX[:].rearrange("(c p) t -> p c t", p=128)
        hv = HT[:].rearrange("(c p) t -> p c t", p=128)
        cnt = {"ps": 0}

        def stageL(tb):
            tsl = slice(tb * 512, (tb + 1) * 512)
            xt, mt = xts[tb % 2], mxs[tb % 2]
            p.dma("sp", xt[:], xv[:, :, tsl], reads=(xsrc,), writes=(xt,))
            p.dma("sp", mt[:], mv[:, :, tsl], reads=(MIX,), writes=(mt,))

        def stageA(tb):
            xt, xn, mt, sq = xts[tb % 2], xns[tb % 2], mxs[tb % 2], sqs[tb % 2]
            tsl = slice(tb * 512, (tb + 1) * 512)
            for ft in range(8):
                psb = PS[cnt["ps"] % 4]
                cnt["ps"] += 1
                for c in range(8):
                    p.mm(psb, psb[:], wo, wo[:, c, ft * 128:(ft + 1) * 128], mt, mt[:, c, :],
                         start=(c == 0), stop=(c == 7))
                p.tt("dve", xn, xn[:, ft, :], xt, xt[:, ft, :], psb, psb[:], ALU.add)
            p.dma("sp", xo[:, :, tsl], xn[:], reads=(xn,), writes=(XR,))
            p.actf(sq, sq[:], xn, xn[:], AF.Square)

        def stageB(tb):
            xn, sq, hT, rr = xns[tb % 2], sqs[tb % 2], hTs[tb % 2], rrs[tb % 2]
            tsl = slice(tb * 512, (tb + 1) * 512)
            h32 = h32s[tb % 2] if moe_idx is not None else None
            psb = PS[7]
            gofs = SP["g2"][0]
            for c in range(8):
                p.mm(psb, psb[:], cbt, cbs("ones"), sq, sq[:, c, :], start=(c == 0), stop=(c == 7))
            p.actf(rr, rr[:], psb, psb[:], AF.Ln, bias=EPS, scale=1.0 / 1024.0)
            p.actf(rr, rr[:], rr, rr[:], AF.Exp, scale=-0.5)
            for c in range(8):
                p.stt(hT, hT[:, c, :], xn, xn[:, c, :], spk[:, gofs + c:gofs + c + 1], rr, rr[:],
                      ALU.mult, ALU.mult, extra_reads=(spk,))
                if h32 is not None:
                    p.stt(h32, h32[:, c, :], xn, xn[:, c, :], spk[:, gofs + c:gofs + c + 1], rr, rr[:],
                          ALU.mult, ALU.mult, extra_reads=(spk,))
            p.dma("sp", hv[:, :, tsl], hT[:], reads=(hT,), writes=(HT,))
            if moe_idx is not None:
                pl = PS[6]
                for tt_ in range(4):
                    for c in range(8):
                        p.mm(pl, pl[:, tt_ * 8:(tt_ + 1) * 8], h32, h32[:, c, tt_ * 128:(tt_ + 1) * 128],
                             wr, wr[:, c, :], start=(c == 0), stop=(c == 7))
                p.copy("dve", lg, lg[:], pl, pl[:, 0:32].rearrange("p (a e) -> p a e", e=8))
                m1, m2, ssum = mx
                AX = mybir.AxisListType.X
                p.op("dve", lambda en: en.tensor_reduce(out=m1[:], in_=lg[:], axis=AX, op=ALU.max),
                     reads=(lg,), writes=(m1,))
                p.tt("dve", wk[0], wk[0][:], lg, lg[:], m1, m1[:].broadcast_to([128, 4, 8]), ALU.is_equal)
                p.stt(wk[1], wk[1][:].rearrange("p a e -> p (a e)"), wk[0], wk[0][:].rearrange("p a e -> p (a e)"),
                      -1e30, lg, lg[:].rearrange("p a e -> p (a e)"), ALU.mult, ALU.add)
                p.op("dve", lambda en: en.tensor_reduce(out=m2[:], in_=wk[1][:], axis=AX, op=ALU.max),
                     reads=(wk[1],), writes=(m2,))
                p.tt("dve", wk[0], wk[0][:], lg, lg[:], m2, m2[:].broadcast_to([128, 4, 8]), ALU.is_ge)
                p.tt("dve", wk[1], wk[1][:], lg, lg[:], m1, m1[:].broadcast_to([128, 4, 8]), ALU.subtract)
                p.actf(wk[2], wk[2][:], wk[1], wk[1][:], AF.Exp)
                p.tt("dve", wk[2], wk[2][:], wk[2], wk[2][:], wk[0], wk[0][:], ALU.mult)
                p.op("dve", lambda en: en.tensor_reduce(out=ssum[:], in_=wk[2][:], axis=AX, op=ALU.add),
                     reads=(wk[2],), writes=(ssum,))
                p.recip(ssum, ssum[:], ssum, ssum[:])
                p.tt("dve", wk[3], wk[3][:], wk[2], wk[2][:], ssum, ssum[:].broadcast_to([128, 4, 8]), ALU.mult)
                pt = PS[5]
                for tt_ in range(4):
                    p.op("pe", lambda en: en.transpose(pt[0:8, tt_ * 128:(tt_ + 1) * 128], wk[3][:, tt_, :],
                                                       cfs("ident")),
                         reads=(wk[3], cft), writes=(pt,), sig=(tt_ == 3))
                p.copy("act", gts, gts[:], pt, pt[0:8, :])
                p.dma("sp", GT[:, tsl], gts[:], reads=(gts,), writes=(GT,))

        stageL(0)
        if NB > 1:
            stageL(1)
        stageA(0)
        for tb in range(NB):
            if tb + 1 < NB:
                stageA(tb + 1)
            if tb + 2 < NB:
                stageL(tb + 2)
            stageB(tb)
        ph.close()

    def phase_ffn(l, moe_idx, dense_idx, dst):
        ph = Phase()
        moe = moe_idx is not None
        FC = 512 if moe else 256
        nfi = FC // 128
        dff = D_FFE if moe else D_FF
        nchunk = dff // FC
        nexp = NEXP if moe else 1
        if moe:
            w1d = wd(f"mw1_{moe_idx}", [NEXP, 1024, D_FFE])
            w3d = wd(f"mw3_{moe_idx}", [NEXP, 1024, D_FFE])
            w2d = wd(f"mw2_{moe_idx}", [NEXP, D_FFE, 1024])
        else:
            w1d = wd(f"w1_{dense_idx}", [1024, D_FF])
            w3d = wd(f"w3_{dense_idx}", [1024, D_FF])
            w2d = wd(f"w2_{dense_idx}", [D_FF, 1024])
        HTOK = min(2048, S)
        nhalf = S // HTOK
        ntb = HTOK // 512
        acc = ph.sb([128, 8, HTOK])
        h2 = ph.sb([128, 8, HTOK], BF16)
        w1s = [ph.sb([128, 8, FC], BF16) for _ in range(2)]
        w3s = [ph.sb([128, 8, FC], BF16) for _ in range(2)]
        w2s = [ph.sb([128, nfi, 1024], BF16) for _ in range(2)]
        sas = [ph.sb([128, 512]) for _ in range(3)]
        gs = [ph.sb([128, nfi, 512], BF16) for _ in range(2)]
        if moe:
            gtile = ph.sb([8, HTOK])
            gbhs = [ph.sb([128, ntb, 512]) for _ in range(2)]
        xo = XR[:].rearrange("(c p) t -> p c t", p=128)
        do = dst[:].rearrange("(c p) t -> p c t", p=128)
        hv = HT[:].rearrange("(c p) t -> p c t", p=128)
        accv = [[acc.view((slice(None), ft, slice(tb * 512, (tb + 1) * 512))) for tb in range(ntb)] for ft in range(8)]
        chunks = [(e, fc) for e in range(nexp) for fc in range(nchunk)]
        nck = len(chunks)

        def load_chunk(ci):
            e, fc = chunks[ci]
            w1, w3, w2 = w1s[ci % 2], w3s[ci % 2], w2s[ci % 2]
            fsl = slice(fc * FC, (fc + 1) * FC)
            if moe:
                s1 = w1d[e].rearrange("(c p) n -> p c n", p=128)
                s3 = w3d[e].rearrange("(c p) n -> p c n", p=128)
                s2 = w2d[e, fsl, :].rearrange("(i p) n -> p i n", p=128)
            else:
                s1 = w1d[:].rearrange("(c p) n -> p c n", p=128)
                s3 = w3d[:].rearrange("(c p) n -> p c n", p=128)
                s2 = w2d[fsl, :].rearrange("(i p) n -> p i n", p=128)
            cast_load(w1, w1[:], w1d, s1[:, :, fsl])
            cast_load(w3, w3[:], w3d, s3[:, :, fsl])
            cast_load(w2, w2[:], w2d, s2)

        nsa = [0]

        def stage1(u):
            ci, tb = divmod(u, ntb)
            e, fc = chunks[ci]
            w1, w3 = w1s[ci % 2], w3s[ci % 2]
            tsl = slice(tb * 512, (tb + 1) * 512)
            g = gs[u % 2]
            if moe:
                gbh = gbhs[e % 2]
                if fc == 0 and tb == 0:
                    for tb2 in range(ntb):
                        pg = PS[6 + tb2 % 2]
                        p.mm(pg, pg[:], cft, cfs("sel", r0=0, r1=8, c0=e * 128, c1=(e + 1) * 128),
                             gtile, gtile[:, tb2 * 512:(tb2 + 1) * 512], start=True, stop=True)
                        p.copy("act", gbh, gbh[:, tb2, :], pg, pg[:])
                gb_ap = gbh[:, tb, :]
            for i in range(nfi):
                pa, pb = PS[(2 * i) % 4], PS[(2 * i + 1) % 4]
                for c in range(8):
                    p.mm(pa, pa[:], w1, w1[:, c, i * 128:(i + 1) * 128], h2, h2[:, c, tsl],
                         start=(c == 0), stop=(c == 7))
                for c in range(8):
                    p.mm(pb, pb[:], w3, w3[:, c, i * 128:(i + 1) * 128], h2, h2[:, c, tsl],
                         start=(c == 0), stop=(c == 7))
                sa = sas[nsa[0] % 3]
                nsa[0] += 1
                p.actf(sa, sa[:], pa, pa[:], AF.Silu)
                if moe:
                    p.tt(GATE_ENG, sa, sa[:], sa, sa[:], gbh, gb_ap, ALU.mult)
                p.tt("dve", g, g[:, i, :], sa, sa[:], pb, pb[:], ALU.mult)

        def stage2(u):
            ci, tb = divmod(u, ntb)
            w2 = w2s[ci % 2]
            g = gs[u % 2]
            for ft in range(8):
                po = PS[4 + ft % (2 if moe else 4)]
                for i in range(nfi):
                    p.mm(po, po[:], w2, w2[:, i, ft * 128:(ft + 1) * 128], g, g[:, i, :],
                         start=(i == 0), stop=(i == nfi - 1))
                av = accv[ft][tb]
                p.tt("dve", av, av[:], av, av[:], po, po[:], ALU.add)

        for hf in range(nhalf):
            hsl = slice(hf * HTOK, (hf + 1) * HTOK)
            for c in range(8):
                p.dma("sp", acc[:, c, :], xo[:, c, hsl], reads=(XR,), writes=tuple(accv[c]))
            p.dma("sp", h2[:], hv[:, :, hsl], reads=(HT,), writes=(h2,))
            if moe:
                p.dma("sp", gtile[:], GT[:, hsl], reads=(GT,), writes=(gtile,))
            load_chunk(0)
            if nck > 1:
                load_chunk(1)
            nu = nck * ntb
            for step in range(nu + 1):
                if step < nu:
                    stage1(step)
                if step >= 1:
                    stage2(step - 1)
                    ci, tb = divmod(step, ntb)
                    if step < nu and tb == 0 and ci >= 1 and ci + 1 < nck:
                        load_chunk(ci + 1)
            for c in range(8):
                p.dma("sp", do[:, c, hsl], acc[:, c, :], reads=tuple(accv[c]), writes=(dst,))
        ph.close()

    def phase_ssm(l):
        ph = Phase()
        T = 512
        spk = ph.sb([128, SPN])
        spd = wd(f"sp{l}", [128, SPN])
        p.dma("sp", spk[:], spd[:], reads=(spd,), writes=(spk,))
        bc = {}
        for nm in ("bpr", "bpi", "cpr", "cpi"):
            d = wd(f"{nm}{l}", [128, 8, 128])
            t = ph.sb([128, 8, 128], BF16)
            cast_load(t, t[:], d, d[:])
            bc[nm] = t
        p.ts("dve", bc["cpi"], bc["cpi"][:], bc["cpi"], bc["cpi"][:], -1.0, ALU.mult)
        wgd = wd(f"wglu{l}", [256, 256])
        wg = ph.sb([128, 2, 256], BF16)
        cast_load(wg, wg[:], wgd, wgd[:].rearrange("(c p) n -> p c n", p=128))
        sm = ph.sb([128, 24, 8])
        SM = {n: i for i, n in enumerate(("dt", "a", "th", "r", "c", "s", "t0", "t1", "t2", "cr", "ci", "den",
                                           "cor", "coi", "pr", "pi", "etr", "eti", "zir", "zii", "u0", "u1"))}

        def sv(n):
            return sm[:, SM[n], :]

        lre = spk[:, 32:40]
        lim = spk[:, 40:48]
        p.actf(sm, sv("dt"), spk, spk[:, 48:56], AF.Exp)
        p.tt("dve", sm, sv("a"), sm, sv("dt"), spk, lre, ALU.mult)
        p.tt("dve", sm, sv("th"), sm, sv("dt"), spk, lim, ALU.mult)
        p.actf(sm, sv("r"), sm, sv("a"), AF.Exp)
        hp = ph.sb([128, 1])
        p.memset("dve", hp, hp[:], math.pi / 2)
        p.actf(sm, sv("s"), sm, sv("th"), AF.Sin, scale=1.0 / 32.0)
        p.actf(sm, sv("c"), sm, sv("th"), AF.Sin, scale=1.0 / 32.0, bias=hp[:], extra_reads=(hp,))

        def csquare(cn, sn):
            p.tt("dve", sm, sv("t0"), sm, sv(cn), sm, sv(cn), ALU.mult)
            p.tt("dve", sm, sv("t1"), sm, sv(sn), sm, sv(sn), ALU.mult)
            p.tt("dve", sm, sv("t2"), sm, sv(cn), sm, sv(sn), ALU.mult)
            p.tt("dve", sm, sv(cn), sm, sv("t0"), sm, sv("t1"), ALU.subtract)
            p.ts("dve", sm, sv(sn), sm, sv("t2"), 2.0, ALU.mult)

        for _ in range(5):
            csquare("c", "s")
        p.tt("dve", sm, sv("cr"), sm, sv("r"), sm, sv("c"), ALU.mult)
        p.ts("dve", sm, sv("cr"), sm, sv("cr"), -1.0, ALU.add)
        p.tt("dve", sm, sv("ci"), sm, sv("r"), sm, sv("s"), ALU.mult)
        p.tt("dve", sm, sv("t0"), spk, lre, spk, lre, ALU.mult)
        p.tt("dve", sm, sv("t1"), spk, lim, spk, lim, ALU.mult)
        p.tt("dve", sm, sv("den"), sm, sv("t0"), sm, sv("t1"), ALU.add)
        p.recip(sm, sv("den"), sm, sv("den"))
        p.tt("dve", sm, sv("t0"), sm, sv("cr"), spk, lre, ALU.mult)
        p.tt("dve", sm, sv("t1"), sm, sv("ci"), spk, lim, ALU.mult)
        p.tt("dve", sm, sv("t0"), sm, sv("t0"), sm, sv("t1"), ALU.add)
        p.tt("dve", sm, sv("cor"), sm, sv("t0"), sm, sv("den"), ALU.mult)
        p.tt("dve", sm, sv("t0"), sm, sv("ci"), spk, lre, ALU.mult)
        p.tt("dve", sm, sv("t1"), sm, sv("cr"), spk, lim, ALU.mult)
        p.tt("dve", sm, sv("t0"), sm, sv("t0"), sm, sv("t1"), ALU.subtract)
        p.tt("dve", sm, sv("coi"), sm, sv("t0"), sm, sv("den"), ALU.mult)
        Er = ph.sb([128, 8, T])
        Ei = ph.sb([128, 8, T])
        Fr = ph.sb([128, 8, T], BF16)
        Fi = ph.sb([128, 8, T], BF16)
        Rt = ph.sb([128, 8, T])
        tA = ph.sb([128, 8, T])
        tB = ph.sb([128, 8, T])
        p.memset("dve", Er, Er[:, :, 0:1], 1.0)
        p.memset("dve", Ei, Ei[:, :, 0:1], 0.0)
        p.copy("dve", sm, sv("pr"), sm, sv("c"))
        p.copy("dve", sm, sv("pi"), sm, sv("s"))
        w = 1
        while w < T:
            prb = sm[:, SM["pr"], :].rearrange("p (j o) -> p j o", o=1).broadcast_to([128, 8, w])
            pib = sm[:, SM["pi"], :].rearrange("p (j o) -> p j o", o=1).broadcast_to([128, 8, w])
            p.tt("dve", tA, tA[:, :, 0:w], Er, Er[:, :, 0:w], sm, prb, ALU.mult)
            p.tt("dve", tB, tB[:, :, 0:w], Ei, Ei[:, :, 0:w], sm, pib, ALU.mult)
            p.tt("dve", Er, Er[:, :, w:2 * w], tA, tA[:, :, 0:w], tB, tB[:, :, 0:w], ALU.subtract)
            p.tt("dve", tA, tA[:, :, 0:w], Er, Er[:, :, 0:w], sm, pib, ALU.mult)
            p.tt("dve", tB, tB[:, :, 0:w], Ei, Ei[:, :, 0:w], sm, prb, ALU.mult)
            p.tt("dve", Ei, Ei[:, :, w:2 * w], tA, tA[:, :, 0:w], tB, tB[:, :, 0:w], ALU.add)
            csquare("pr", "pi")
            w *= 2
        p.copy("dve", sm, sv("etr"), sm, sv("pr"))
        p.copy("dve", sm, sv("eti"), sm, sv("pi"))
        corb = sm[:, SM["cor"], :].rearrange("p (j o) -> p j o", o=1).broadcast_to([128, 8, T])
        coib = sm[:, SM["coi"], :].rearrange("p (j o) -> p j o", o=1).broadcast_to([128, 8, T])
        rb = sm[:, SM["r"], :].rearrange("p (j o) -> p j o", o=1).broadcast_to([128, 8, T])
        p.tt("dve", tA, tA[:], Er, Er[:], sm, corb, ALU.mult)
        p.tt("dve", tB, tB[:], Ei, Ei[:], sm, coib, ALU.mult)
        p.tt("dve", Fr, Fr[:], tA, tA[:], tB, tB[:], ALU.add)
        p.tt("dve", tA, tA[:], Er, Er[:], sm, coib, ALU.mult)
        p.tt("dve", tB, tB[:], Ei, Ei[:], sm, corb, ALU.mult)
        p.tt("dve", Fi, Fi[:], tA, tA[:], tB, tB[:], ALU.subtract)
        p.copy("dve", Rt, Rt[:], sm, rb)
        p.memset("dve", sm, sv("zir"), 0.0)
        p.memset("dve", sm, sv("zii"), 0.0)

        u32s = [ph.sb([128, 2, T]) for _ in range(2)]
        ubs = [ph.sb([128, 2, T], BF16) for _ in range(2)]
        wk = [ph.sb([128, T], BF16) for _ in range(10)]
        abs_ = [ph.sb([128, T], BF16) for _ in range(4)]
        Ecb = ph.sb([128, 8, T], BF16)
        Esb = ph.sb([128, 8, T], BF16)
        p.copy("act", Ecb, Ecb[:], Er, Er[:])
        p.copy("act", Esb, Esb[:], Ei, Ei[:])
        xrs = [ph.sb([128, T], BF16) for _ in range(2)]
        xis = [ph.sb([128, T], BF16) for _ in range(2)]
        yv = [ph.sb([128, T]) for _ in range(4)]
        gl32 = ph.sb([128, 2, T])
        glb = ph.sb([128, 2, T], BF16)
        sq = ph.sb([128, T], BF16)
        rr = ph.sb([128, T])
        outb = [ph.sb([128, T], BF16) for _ in range(2)]
        uv = UT[:].rearrange("(c p) t -> p c t", p=128)
        for tb in range(S // T):
            tsl = slice(tb * T, (tb + 1) * T)
            u32, ub = u32s[tb % 2], ubs[tb % 2]
            p.dma("sp", u32[:], uv[:, :, tsl], reads=(UT,), writes=(u32,))
            cast_load(ub, ub[:], UT, uv[:, :, tsl])
            for j in range(8):
                A, B = PS[(2 * j) % 4], PS[(2 * j + 1) % 4]
                p.mm(A, A[:], bc["bpr"], bc["bpr"][:, j, :], ub, ub[:, j // 4, :], start=True, stop=True)
                p.mm(B, B[:], bc["bpi"], bc["bpi"][:, j, :], ub, ub[:, j // 4, :], start=True, stop=True)
                t1, t2, t3, t4, wre, wim, zre, zim, t5, t6 = wk
                Ab, Bb = abs_[(2 * j) % 4], abs_[(2 * j + 1) % 4]
                p.copy("act", Ab, Ab[:], A, A[:])
                p.copy("act", Bb, Bb[:], B, B[:])
                p.tt("dve", t1, t1[:], Fr, Fr[:, j, :], Ab, Ab[:], ALU.mult)
                p.tt("dve", t2, t2[:], Fi, Fi[:, j, :], Bb, Bb[:], ALU.mult)
                p.tt("dve", wre, wre[:], t1, t1[:], t2, t2[:], ALU.subtract)
                p.tt("dve", t3, t3[:], Fr, Fr[:, j, :], Bb, Bb[:], ALU.mult)
                p.tt("dve", t4, t4[:], Fi, Fi[:, j, :], Ab, Ab[:], ALU.mult)
                p.tt("dve", wim, wim[:], t3, t3[:], t4, t4[:], ALU.add)
                p.op("dve", lambda en: en.tensor_tensor_scan(out=zre[:], data0=Rt[:, j, :], data1=wre[:],
                                                             initial=sm[:, SM["zir"], j:j + 1],
                                                             op0=ALU.mult, op1=ALU.add),
                     reads=(Rt, wre, sm), writes=(zre,))
                p.op("dve", lambda en: en.tensor_tensor_scan(out=zim[:], data0=Rt[:, j, :], data1=wim[:],
                                                             initial=sm[:, SM["zii"], j:j + 1],
                                                             op0=ALU.mult, op1=ALU.add),
                     reads=(Rt, wim, sm), writes=(zim,))
                xr, xi = xrs[j % 2], xis[j % 2]
                p.tt("dve", t1, t1[:], Ecb, Ecb[:, j, :], zre, zre[:], ALU.mult)
                p.tt("dve", t2, t2[:], Esb, Esb[:, j, :], zim, zim[:], ALU.mult)
                p.tt("dve", xr, xr[:], t1, t1[:], t2, t2[:], ALU.subtract)
                p.tt("dve", t5, t5[:], Ecb, Ecb[:, j, :], zim, zim[:], ALU.mult)
                p.tt("dve", t6, t6[:], Esb, Esb[:, j, :], zre, zre[:], ALU.mult)
                p.tt("dve", xi, xi[:], t5, t5[:], t6, t6[:], ALU.add)
                p.tt("dve", sm, sm[:, SM["u0"], j:j + 1], sm, sm[:, SM["eti"], j:j + 1], zim, zim[:, T - 1:T], ALU.mult)
                p.tt("dve", sm, sm[:, SM["u1"], j:j + 1], sm, sm[:, SM["eti"], j:j + 1], zre, zre[:, T - 1:T], ALU.mult)
                p.stt(sm, sm[:, SM["zir"], j:j + 1], zre, zre[:, T - 1:T], sm[:, SM["etr"], j:j + 1],
                      sm, sm[:, SM["u0"], j:j + 1], ALU.mult, ALU.subtract)
                p.stt(sm, sm[:, SM["zii"], j:j + 1], zim, zim[:, T - 1:T], sm[:, SM["etr"], j:j + 1],
                      sm, sm[:, SM["u1"], j:j + 1], ALU.mult, ALU.add)
                Y = PS[4 + j // 4]
                p.mm(Y, Y[:], bc["cpr"], bc["cpr"][:, j, :], xr, xr[:], start=(j % 4 == 0), stop=False)
                p.mm(Y, Y[:], bc["cpi"], bc["cpi"][:, j, :], xi, xi[:], start=False, stop=(j % 4 == 3))
            for ft in range(2):
                Y = PS[4 + ft]
                y0, y1, y2, y3 = yv
                p.stt(y0, y0[:], u32, u32[:, ft, :], spk[:, 24 + ft:25 + ft], Y, Y[:], ALU.mult, ALU.add,
                      extra_reads=(spk,))
                p.tt("dve", y1, y1[:], y0, y0[:], y0, y0[:], ALU.mult)
                p.ts("dve", y1, y1[:], y1, y1[:], 0.044715, ALU.mult, 1.0, ALU.add)
                p.tt("dve", y2, y2[:], y1, y1[:], y0, y0[:], ALU.mult)
                p.actf(y3, y3[:], y2, y2[:], AF.Sigmoid, scale=2.0 * math.sqrt(2.0 / math.pi))
                p.tt("dve", gl32, gl32[:, ft, :], y0, y0[:], y3, y3[:], ALU.mult)
                p.copy("act", glb, glb[:, ft, :], gl32, gl32[:, ft, :])
            for ft in range(2):
                G = PS[6]
                for k2 in range(2):
                    p.mm(G, G[:], wg, wg[:, k2, ft * 128:(ft + 1) * 128], glb, glb[:, k2, :],
                         start=(k2 == 0), stop=(k2 == 1))
                y0, y1, y2, y3 = yv
                p.actf(y0, y0[:], G, G[:], AF.Sigmoid, bias=spk[:, 26 + ft:27 + ft], extra_reads=(spk,))
                p.tt("dve", y1, y1[:], gl32, gl32[:, ft, :], y0, y0[:], ALU.mult)
                ob = outb[ft]
                group_norm(y1, y1[:], 128, T, "bd64", 64.0, spk[:, 16 + ft:17 + ft], spk, ob, ob[:], sq, rr, PS[7])
                p.dma("sp", MIX[ft * 128:(ft + 1) * 128, tsl], ob[:], reads=(ob,), writes=(MIX,))
        ph.close()

    def phase_sb(l):
        ph = Phase()
        spk = ph.sb([128, SPN])
        spd = wd(f"sp{l}", [128, SPN])
        p.dma("sp", spk[:], spd[:], reads=(spd,), writes=(spk,))
        qTs = [ph.sb([128, S], BF16) for _ in range(2)]
        kTs = [ph.sb([128, S], BF16) for _ in range(2)]
        nkTs = [ph.sb([128, S], BF16) for _ in range(2)]
        vvs = [ph.sb([128, NKT, 128], BF16) for _ in range(2)]
        for t_ in qTs + kTs + vvs:
            p.memset("dve", t_, t_[:], 0.0)
        Es = [ph.sb([128, 2, 512]) for _ in range(2)]
        SPb = [ph.sb([128, 2, 512], BF16) for _ in range(3)]
        Wb = [ph.sb([128, 2, 512], BF16) for _ in range(2)]
        SPsum = ph.sb([128, 512], BF16)
        sq = ph.sb([128, 512], BF16)
        rr = ph.sb([128, 512])
        ob = [ph.sb([64, 512], BF16) for _ in range(2)]
        iters = []
        for h in range(4):
            for qb in range(NB):
                for a in range(4 * qb + 3, 0, -2):
                    iters.append((h, qb, a))
        n = len(iters)

        def load_head(h):
            hs = slice(h * 64, (h + 1) * 64)
            qT, kT, nkT, vv = qTs[h % 2], kTs[h % 2], nkTs[h % 2], vvs[h % 2]
            p.dma("sp", qT[0:64, :], QT["sb"][hs, :], reads=(QT["sb"],), writes=(qT,))
            p.dma("sp", kT[0:64, :], KT["sb"][hs, :], reads=(KT["sb"],), writes=(kT,))
            p.dma("sp", vv[:, :, 0:64], VV["sb"][:, hs].rearrange("(a p) d -> p a d", p=128), reads=(VV["sb"],),
                  writes=(vv,))
            p.actf(nkT, nkT[:], kT, kT[:], AF.Copy, scale=-1.0)

        def stage1(it):
            h, qb, a = iters[it]
            if qb == 0 and a == 3 and h == 0:
                load_head(0)
            if qb == 1 and a == 7 and h + 1 < 4:
                load_head(h + 1)
            qT, kT = qTs[h % 2], kTs[h % 2]
            qsl = slice(qb * 512, (qb + 1) * 512)
            b0i = 2 * (it % 2)
            E, sp_ = Es[it % 2], SPb[it % 3]
            for t in range(2):
                A = PS[b0i + t]
                ksl = slice((a - t) * 128, (a - t + 1) * 128)
                p.mm(A, A[:], kT, kT[:, ksl], qT, qT[:, qsl], start=True, stop=True)
            p.actf(E, E[:], PS[b0i], ps2(b0i), AF.Exp, extra_reads=(PS[b0i + 1],))
            p.actf(sp_, sp_[:], E, E[:], AF.Ln, bias=1.0)
            for t in range(2):
                diag = a - t - 4 * qb
                if diag >= 0:
                    p.tt("dve", sp_, sp_[:, t, :], sp_, sp_[:, t, :], cbt, cbs(f"m{diag}"), ALU.mult)

        def stage2(it):
            h, qb, a = iters[it]
            qT, nkT = qTs[h % 2], nkTs[h % 2]
            qsl = slice(qb * 512, (qb + 1) * 512)
            sp_, W = SPb[it % 3], Wb[it % 2]
            first = (a == 4 * qb + 3)
            for t in range(2):
                Bp = PS[4 + t]
                ksl = slice((a - t) * 128, (a - t + 1) * 128)
                p.mm(Bp, Bp[:], nkT, nkT[:, ksl], qT, qT[:, qsl], start=True, stop=False)
                lastmm = first and t == 0
                p.mm(Bp, Bp[:], cbt, cbs("tincl"), sp_, sp_[:, t, :], start=False, stop=lastmm)
                if t == 1:
                    p.mm(Bp, Bp[:], cbt, cbs("ones"), sp_, sp_[:, 0, :], start=False, stop=first)
                if not first:
                    p.mm(Bp, Bp[:], cbt, cbs("ones"), SPsum, SPsum[:], start=False, stop=True)
            p.actf(W, W[:], PS[4], ps2(4), AF.Exp, scale=-1.0, extra_reads=(PS[5],))
            for t in range(2):
                diag = a - t - 4 * qb
                if diag >= 0:
                    p.tt("dve", W, W[:, t, :], W, W[:, t, :], cbt, cbs(f"m{diag}"), ALU.mult)
            if a - 1 > 0:
                if first:
                    p.tt("dve", SPsum, SPsum[:], sp_, sp_[:, 0, :], sp_, sp_[:, 1, :], ALU.add)
                else:
                    p.tt("dve", SPsum, SPsum[:], SPsum, SPsum[:], sp_, sp_[:, 0, :], ALU.add)
                    p.tt("dve", SPsum, SPsum[:], SPsum, SPsum[:], sp_, sp_[:, 1, :], ALU.add)

        def stage3(it):
            h, qb, a = iters[it]
            vv, W = vvs[h % 2], Wb[it % 2]
            O = PS[6 + qb % 2]
            for t in range(2):
                p.mm(O, O[:, :], vv, vv[:, a - t, :], W, W[:, t, :], start=(a == 4 * qb + 3 and t == 0),
                     stop=(a - t == 0))
            if a - 1 == 0:
                qsl = slice(qb * 512, (qb + 1) * 512)

                def finA(O=O):
                    p.actf(sq, sq[0:64, :], O, O[0:64, :], AF.Square)

                def finB(O=O, h=h, qb=qb, qsl=qsl):
                    o_ = ob[qb % 2]
                    G = PS[4]
                    p.mm(G, G[0:64, :], cbt, cbs("bd64", rows=64, c1=64), sq, sq[0:64, :], start=True, stop=True)
                    p.actf(rr, rr[0:64, :], G, G[0:64, :], AF.Ln, bias=EPS, scale=1.0 / 64.0)
                    p.actf(rr, rr[0:64, :], rr, rr[0:64, :], AF.Exp, scale=-0.5)
                    p.stt(o_, o_[:], O, O[0:64, :], spk[0:64, 188 + h:189 + h], rr, rr[0:64, :], ALU.mult, ALU.mult,
                          extra_reads=(spk,))
                    p.dma("sp", MIX[256 + h * 64:256 + (h + 1) * 64, qsl], o_[:], reads=(o_,), writes=(MIX,))

                pend.append((cur_step[0] + 1, finA))
                pend.append((cur_step[0] + 2, finB))

        pend = []
        cur_step = [0]
        step = 0
        while step < n + 2 or pend:
            cur_step[0] = step
            if step < n:
                stage1(step)
            if 0 <= step - 1 < n:
                stage2(step - 1)
            if 0 <= step - 2 < n:
                stage3(step - 2)
            due = [f for (d, f) in pend if d <= step]
            pend[:] = [(d, f) for (d, f) in pend if d > step]
            for f in due:
                f()
            step += 1
        ph.close()

    def phase_ch(l):
        ph = Phase()
        spk = ph.sb([128, SPN])
        spd = wd(f"sp{l}", [128, SPN])
        p.dma("sp", spk[:], spd[:], reads=(spd,), writes=(spk,))
        chd = wd(f"chb{l}", [128, 4, 5, 128])
        bt32 = ph.sb([128, 4, 5, 128])
        btb = ph.sb([128, 4, 5, 128], BF16)
        p.dma("sp", bt32[:], chd[:], reads=(chd,), writes=(bt32,))
        for h in range(4):
            p.tt("dve", bt32, bt32[:, h, 0, :], bt32, bt32[:, h, 0, :], cft, cfs("chm0"), ALU.add)
            p.tt("dve", bt32, bt32[:, h, 4, :], bt32, bt32[:, h, 4, :], cft, cfs("chm4"), ALU.add)
        p.copy("dve", btb, btb[:], bt32, bt32[:])
        qTs = [ph.sb([128, S], BF16) for _ in range(2)]
        kTs = [ph.sb([128, S], BF16) for _ in range(2)]
        vas = [ph.sb([128, NKT, 128], BF16) for _ in range(2)]
        for t_ in qTs + kTs:
            p.memset("dve", t_, t_[:], 0.0)
        for va in vas:
            p.memset("dve", va, va[:, :, 64:128], 0.0)
            p.memset("dve", va, va[:, :, 64:65], 1.0)
        Pt = [ph.sb([128, 640], BF16) for _ in range(2)]
        sq = ph.sb([65, 512], BF16)
        rr = ph.sb([64, 512])
        ob = [ph.sb([64, 512], BF16) for _ in range(2)]
        iters = [(h, qt) for h in range(4) for qt in range(NKT)]
        n = len(iters)

        def load_head(h):
            hs = slice(h * 64, (h + 1) * 64)
            p.dma("sp", qTs[h % 2][0:64, :], QT["ch"][hs, :], reads=(QT["ch"],), writes=(qTs[h % 2],))
            p.dma("sp", kTs[h % 2][0:64, :], KT["ch"][hs, :], reads=(KT["ch"],), writes=(kTs[h % 2],))
            p.dma("sp", vas[h % 2][:, :, 0:64], VV["ch"][:, hs].rearrange("(a p) d -> p a d", p=128),
                  reads=(VV["ch"],), writes=(vas[h % 2],))

        def stage1(it):
            h, qt = iters[it]
            if h == 0 and qt == 0:
                load_head(0)
            if qt == 2 and h + 1 < 4:
                load_head(h + 1)
            qT, kT = qTs[h % 2], kTs[h % 2]
            q128 = slice(qt * 128, (qt + 1) * 128)
            nd = min(4, qt) + 1
            X, Y = PS[it % 2], PS[2 + it % 2]
            for d in range(nd):
                a = qt - d
                dst_b = X if d < 4 else Y
                dst = X[:, d * 128:(d + 1) * 128] if d < 4 else Y[:, 0:128]
                p.mm(dst_b, dst, kT, kT[:, a * 128:(a + 1) * 128], qT, qT[:, q128], start=True, stop=False)
                p.mm(dst_b, dst, btb, btb[:, h, d, :], cbt, cbs("ident"), start=False, stop=True)

        def stage2(it):
            h, qt = iters[it]
            va = vas[h % 2]
            nd = min(4, qt) + 1
            X, Y = PS[it % 2], PS[2 + it % 2]
            P_ = Pt[it % 2]
            O = PS[4 + (qt // 4) % 2]
            ocol = slice((qt % 4) * 128, (qt % 4 + 1) * 128)
            n4 = min(nd, 4)
            p.actf(P_, P_[:, 0:n4 * 128], X, X[:, 0:n4 * 128], AF.Exp)
            if nd == 5:
                p.actf(P_, P_[:, 512:640], Y, Y[:, 0:128], AF.Exp)
            for d in range(nd):
                a = qt - d
                p.mm(O, O[:, ocol], va, va[:, a, :], P_, P_[:, d * 128:(d + 1) * 128],
                     start=(d == 0), stop=(d == nd - 1))
            if qt % 4 == 3:
                qb = qt // 4
                qsl = slice(qb * 512, (qb + 1) * 512)
                o_ = ob[qb % 2]
                p.actf(sq, sq[:], O, O[0:65, :], AF.Square, scale=cfs("sclrow", r0=0, r1=65), extra_reads=(cft,))
                SSB = PS[6]
                p.mm(SSB, SSB[0:64, :], cbt, cbs("ones", rows=65, c1=64), sq, sq[:], start=True, stop=True)
                p.actf(rr, rr[:], SSB, SSB[0:64, :], AF.Ln, scale=1.0 / 64.0)
                p.actf(rr, rr[:], rr, rr[:], AF.Exp, scale=-0.5)
                p.stt(o_, o_[:], O, O[0:64, :], spk[0:64, 192 + h:193 + h], rr, rr[:], ALU.mult, ALU.mult,
                      extra_reads=(spk,))
                p.dma("sp", MIX[512 + h * 64:512 + (h + 1) * 64, qsl], o_[:], reads=(o_,), writes=(MIX,))

        for step in range(n + 1):
            if step < n:
                stage1(step)
            if 0 <= step - 1 < n:
                stage2(step - 1)
        ph.close()

    def phase_df(l):
        ph = Phase()
        lam_init = 0.8 - 0.6 * math.exp(-0.3 * l)
        spk = ph.sb([128, SPN])
        spd = wd(f"sp{l}", [128, SPN])
        p.dma("sp", spk[:], spd[:], reads=(spd,), writes=(spk,))
        AX = mybir.AxisListType.X
        sc = ph.sb([128, 8])
        pr = ph.sb([128, 2, 32])
        p.tt("dve", pr, pr[:, 0, :], spk, spk[:, 56:88], spk, spk[:, 88:120], ALU.mult)
        p.tt("dve", pr, pr[:, 1, :], spk, spk[:, 120:152], spk, spk[:, 152:184], ALU.mult)
        p.op("dve", lambda en: en.tensor_reduce(out=sc[:, 0:2], in_=pr[:], axis=AX, op=ALU.add), reads=(pr,), writes=(sc,))
        p.actf(sc, sc[:, 2:4], sc, sc[:, 0:2], AF.Exp)
        p.tt("dve", sc, sc[:, 4:5], sc, sc[:, 3:4], sc, sc[:, 2:3], ALU.subtract)
        p.ts("dve", sc, sc[:, 5:6], sc, sc[:, 4:5], -lam_init, ALU.add)
        lrow = ph.sb([128, 64])
        p.ts("dve", lrow, lrow[:], cft, cfs("ones", c1=64), sc[:, 5:6], ALU.mult, extra_reads=(sc,))
        gdf = ph.sb([64, 4])
        p.ts("dve", gdf, gdf[:], spk, spk[0:64, 196:200], 1.0 - lam_init, ALU.mult)
        qT = ph.sb([128, S], BF16)
        kT = ph.sb([128, 2, S], BF16)
        p.memset("dve", qT, qT[:], 0.0)
        p.memset("dve", kT, kT[:], 0.0)
        va = ph.sb([128, NKT, 128], BF16)
        p.memset("dve", va, va[:, :, 64:128], 0.0)
        p.memset("dve", va, va[:, :, 64:65], 1.0)
        Pt2 = [ph.sb([128, 2, 512], BF16) for _ in range(2)]
        sd2 = [ph.sb([128, 2, 128]) for _ in range(2)]
        rc = ph.sb([128, 1024])
        b0 = ph.sb([64, 512])
        b1 = ph.sb([64, 512])
        t0 = ph.sb([64, 512])
        t1 = ph.sb([64, 512])
        sq = ph.sb([64, 512], BF16)
        rr = ph.sb([64, 512])
        ob = [ph.sb([64, 512], BF16) for _ in range(2)]
        qTs = [qT, ph.sb([128, S], BF16)]
        kTs = [kT, ph.sb([128, 2, S], BF16)]
        p.memset("dve", qTs[1], qTs[1][:], 0.0)
        p.memset("dve", kTs[1], kTs[1][:], 0.0)
        vas = [va, ph.sb([128, NKT, 128], BF16)]
        p.memset("dve", vas[1], vas[1][:, :, 64:128], 0.0)
        p.memset("dve", vas[1], vas[1][:, :, 64:65], 1.0)
        iters = []
        for h in range(4):
            for qb in range(NB):
                for a in range(4 * qb + 4):
                    iters.append((h, qb, a))
        n = len(iters)

        def load_head(h):
            hs = slice(h * 64, (h + 1) * 64)
            p.dma("sp", qTs[h % 2][0:64, :], QT["df"][hs, :], reads=(QT["df"],), writes=(qTs[h % 2],))
            for c_ in range(2):
                p.dma("sp", kTs[h % 2][32 * c_:32 * c_ + 32, c_, :], KT["df"][h * 64 + 32 * c_:h * 64 + 32 * c_ + 32, :],
                      reads=(KT["df"],), writes=(kTs[h % 2],))
            p.dma("sp", vas[h % 2][:, :, 0:64], VV["df"][:, hs].rearrange("(a p) d -> p a d", p=128),
                  reads=(VV["df"],), writes=(vas[h % 2],))

        def stage1(it):
            h, qb, a = iters[it]
            if qb == 0 and a == 0 and h == 0:
                load_head(0)
            if qb == 0 and a == 2 and h + 1 < 4:
                load_head(h + 1)
            qT_, kT_ = qTs[h % 2], kTs[h % 2]
            qsl = slice(qb * 512, (qb + 1) * 512)
            ksl = slice(a * 128, (a + 1) * 128)
            for c in range(2):
                Sc = PS[c + 2 * (it % 2)]
                p.mm(Sc, Sc[:], kT_, kT_[:, c, ksl], qT_, qT_[:, qsl], start=True, stop=True)

        def stage2(it):
            h, qb, a = iters[it]
            sl = SLOPES[h]
            nsub = 2 if h == 0 else 1
            SW = 512 // nsub
            va_ = vas[h % 2]
            qsl = slice(qb * 512, (qb + 1) * 512)
            Oc = (PS[4 + 2 * (qb % 2)], PS[5 + 2 * (qb % 2)])
            amax = 4 * qb + 3
            i = a - 4 * qb
            b0i = 2 * (it % 2)
            S0, S1 = PS[b0i], PS[b0i + 1]
            S2 = ps2(b0i)
            P2 = Pt2[it % 2]
            c0 = 0
            if i >= 0:
                j = i
                r = (128 * j) // SW
                off = 128 * j - r * SW
                sdt = sd2[it % 2]
                for c in range(2):
                    Sc = PS[b0i + c]
                    p.tt("dve", sdt, sdt[:, c, :], Sc, Sc[:, 128 * j:128 * j + 128], cft, cfs(f"dfd{h}"), ALU.add)
                p.actf(P2, P2[:, :, 128 * j:128 * j + 128], sdt, sdt[:], AF.Exp, bias=bias_const(sl * off),
                       extra_reads=(bct,))
                c0 = 128 * (i + 1)
            for r in range(nsub):
                lo = max(r * SW, c0)
                hi = (r + 1) * SW
                if lo >= hi:
                    continue
                m = (qb * 512 + r * SW - 128 * a) // 128
                p.actf(P2, P2[:, :, lo:hi], S0, S2[:, :, lo:hi], AF.Exp,
                       bias=cfs(f"dfb{h}", c0=m + 3, c1=m + 4), extra_reads=(cft, S1))
            w0 = 128 * max(i, 0)
            for c in range(2):
                p.mm(Oc[c], Oc[c][:, w0:512], va_, va_[:, a, :], P2, P2[:, c, w0:512],
                     start=(a == 0), stop=(a == amax))
            c = 1
            if a == amax:
                rcq = rcs[qb % 2]

                def finA(Oc=Oc, rcq=rcq):
                    p.copy("act", rcq, rcq[64:65, 0:512], Oc[0], Oc[0][64:65, :])
                    p.copy("act", rcq, rcq[64:65, 512:1024], Oc[1], Oc[1][64:65, :])
                    p.recip(rcq, rcq[64:65, :], rcq, rcq[64:65, :])

                def finB(Oc=Oc, rcq=rcq, h=h, qb=qb, qsl=qsl):
                    bi = 2 * ((cur_step[0] + 1) % 2)
                    B0, B1 = PS[bi], PS[bi + 1]
                    p.mm(B0, B0[0:64, :], cft, cfs("ones", r0=64, r1=65, c1=64), rcq, rcq[64:65, 0:512],
                         start=True, stop=True)
                    p.mm(B1, B1[0:64, :], lrow, lrow[64:65, :], rcq, rcq[64:65, 512:1024], start=True, stop=True)
                    p.copy("act", b0, b0[:], B0, B0[0:64, :])
                    p.copy("act", b1, b1[:], B1, B1[0:64, :])
                    p.tt("dve", t0, t0[:], Oc[0], Oc[0][0:64, :], b0, b0[:], ALU.mult)
                    p.tt("dve", t1, t1[:], Oc[1], Oc[1][0:64, :], b1, b1[:], ALU.mult)
                    p.tt("dve", t0, t0[:], t0, t0[:], t1, t1[:], ALU.add)

                def finC(h=h, qb=qb, qsl=qsl):
                    bi = 2 * ((cur_step[0] + 1) % 2)
                    o_ = ob[qb % 2]
                    group_norm(t0, t0[:], 64, 512, "bd64", 64.0, gdf[:, h:h + 1], gdf, o_, o_[:], sq, rr, PS[bi])
                    p.dma("sp", MIX[768 + h * 64:768 + (h + 1) * 64, qsl], o_[:], reads=(o_,), writes=(MIX,))

                pend.append((cur_step[0] + 1, finA))
                pend.append((cur_step[0] + 2, finB))
                pend.append((cur_step[0] + 3, finC))

        rcs = [rc, ph.sb([128, 1024])]
        pend = []
        cur_step = [0]
        step = 0
        while step < n + 1 or pend:
            cur_step[0] = step
            if step < n:
                stage1(step)
            if 0 <= step - 1 < n:
                stage2(step - 1)
            due = [f for (d, f) in pend if d <= step]
            pend[:] = [(d, f) for (d, f) in pend if d > step]
            for f in due:
                f()
            step += 1
        ph.close()

    mixers_local = {"ssm": phase_ssm, "sb": phase_sb, "ch": phase_ch, "df": phase_df}

    mixers = mixers_local

    def run_layers():
        nl = len(layer_list)
        for li, l in enumerate(layer_list):
            xsrc = xT if (li == 0 and first_layer_from_x) else XR
            moe = (l % 2 == 1)
            moe_idx = l // 2 if moe else None
            dense_idx = l // 2 if not moe else None
            if "n1" in phases:
                phase_n1(l, xsrc)
            for m in ("ssm", "sb", "ch", "df"):
                if m in phases:
                    mixers[m](l)
            if "op" in phases:
                phase_op(l, xsrc, moe_idx)
            if "ffn" in phases:
                dst = yT if (li == nl - 1 and last_to_y) else XR
                phase_ffn(l, moe_idx, dense_idx, dst)

    return nc, p, run_layers, mixers, locals()


S_FULL = 4096
LAUNCH_GROUPS = [[0, 1, 2, 3]]


def _layer_inputs(inp, l):
    m = {f"sp{l}": pack_small(inp, l),
         f"w_in{l}": np.ascontiguousarray(inp["w_in"][l]),
         f"w_out{l}": np.ascontiguousarray(inp["w_out"][l]),
         f"wglu{l}": np.ascontiguousarray(inp["ssm_w_glu"][l]),
         f"chb{l}": pack_chb(inp, l)}
    bpr, bpi, cpr, cpi = pack_ssm_bc(inp, l)
    m.update({f"bpr{l}": bpr, f"bpi{l}": bpi, f"cpr{l}": cpr, f"cpi{l}": cpi})
    i = l // 2
    if l % 2 == 0:
        m.update({f"w1_{i}": np.ascontiguousarray(inp["ffn_w1"][i]), f"w3_{i}": np.ascontiguousarray(inp["ffn_w3"][i]),
                  f"w2_{i}": np.ascontiguousarray(inp["ffn_w2"][i])})
    else:
        m.update({f"mw1_{i}": np.ascontiguousarray(inp["moe_w1"][i]), f"mw3_{i}": np.ascontiguousarray(inp["moe_w3"][i]),
                  f"mw2_{i}": np.ascontiguousarray(inp["moe_w2"][i]), f"mr{i}": np.ascontiguousarray(inp["moe_router"][i])})
    return m


def kernel(**inputs):
    inp = {k: np.asarray(v) for k, v in inputs.items()}
    x = inp["x"].astype(np.float32, copy=False)
    B, S, _ = x.shape
    cb, cf = make_consts()
    cur = [np.ascontiguousarray(x[b].T) for b in range(B)]
    for grp in LAUNCH_GROUPS:
        nc, p, run_layers, mixers, loc = build(S, grp)
        run_layers()
        p.finish()
        used = set(loc["used_inputs"])
        shared = {"cb": cb, "cf": cf}
        for l in grp:
            shared.update(_layer_inputs(inp, l))
        shared = {k: v for k, v in shared.items() if k in used}
        in_maps = []
        for b in range(B):
            m = dict(shared)
            m["xT"] = cur[b]
            in_maps.append(m)
        res = run_bass_kernel_spmd(nc, in_maps, core_ids=list(range(B)))
        cur = [np.ascontiguousarray(res.results[b]["yT"]) for b in range(B)]
    out = np.stack([cur[b].T for b in range(B)], axis=0)
    return np.ascontiguousarray(out.astype(np.float32, copy=False))
```
